# Optimizing a Trainium2 kernel written in Bass

```python
import math
import jax
import jax.numpy as jnp
from jax import lax
import numpy as np

D_MODEL = 1024
BATCH = 8
SEQ = 4096
DEPTH = 1

GRID_W = 64
CTX_LEN = 256
LRU_WIDTH = 512
LRU_BLOCKS = 8
LRU_BLOCK_W = LRU_WIDTH // LRU_BLOCKS
LRU_C = 8.0
CONV_W = 4
CONV_PAD_LO = 1
ATT_HEADS = 4
HEAD_DIM = 64
V_DIM = 2 * HEAD_DIM
QK_WIDTH = ATT_HEADS * 2 * HEAD_DIM
ATT_WIDTH = ATT_HEADS * V_DIM
MIX_WIDTH = LRU_WIDTH + ATT_WIDTH
IN_WIDTH = 2 * LRU_WIDTH + 2 * QK_WIDTH + ATT_WIDTH
ROPE_AXIS_DIM = HEAD_DIM // 2
ROPE_PAIRS = ROPE_AXIS_DIM // 2
ROPE_BASE = 10000.0
Q_BLOCK = 128
N_EXPERTS = 16
EC_FACTOR = 2
D_EXPERT = 2816
N_MOD = 6
EPS = 1e-6

kernel_name = "hybrid_rglru_diffattn_ecmoe_dit"


def rmsnorm(x, g):
    xf = x.astype(jnp.float32)
    y = xf * lax.rsqrt(jnp.mean(xf * xf, axis=-1, keepdims=True) + EPS)
    return (y * g.astype(jnp.float32)).astype(x.dtype)


def adaln(cvec, w, b, n_chunks):
    cols = n_chunks * D_MODEL
    m = jax.nn.silu(cvec) @ w[:, :cols] + b[:cols]
    return m.reshape(cvec.shape[0], n_chunks, D_MODEL)


def modulate(h, shift, scale):
    return h * (1 + scale[:, None]) + shift[:, None]


def split_cols(p):
    o1 = LRU_WIDTH
    o2 = 2 * LRU_WIDTH
    o3 = o2 + QK_WIDTH
    o4 = o3 + QK_WIDTH
    return p[..., :o1], p[..., o1:o2], p[..., o2:o3], p[..., o3:o4], p[..., o4:]


def axial_rope_tables(n):
    rows = n // GRID_W
    row = jnp.repeat(jnp.arange(rows), GRID_W).astype(jnp.float32)
    col = jnp.tile(jnp.arange(GRID_W), rows).astype(jnp.float32)
    inv = ROPE_BASE ** (-jnp.arange(ROPE_PAIRS, dtype=jnp.float32) / ROPE_PAIRS)
    ang_r = row[:, None] * inv
    ang_c = col[:, None] * inv
    return jnp.cos(ang_r), jnp.sin(ang_r), jnp.cos(ang_c), jnp.sin(ang_c)


def rope_axis(x, cos, sin):
    cos = cos[:, None, None, :].astype(x.dtype)
    sin = sin[:, None, None, :].astype(x.dtype)
    x1, x2 = x[..., :ROPE_PAIRS], x[..., ROPE_PAIRS:]
    return jnp.concatenate([x1 * cos - x2 * sin, x1 * sin + x2 * cos], axis=-1)


def apply_axial_rope(x, tabs):
    cr, sr, cc, sc = tabs
    return jnp.concatenate([rope_axis(x[..., :ROPE_AXIS_DIM], cr, sr),
                            rope_axis(x[..., ROPE_AXIS_DIM:], cc, sc)], axis=-1)


def centred_depthwise_conv(x, w, b):
    n = x.shape[1]
    xp = jnp.pad(x, ((0, 0), (CONV_PAD_LO, CONV_W - 1 - CONV_PAD_LO), (0, 0)))
    return sum(xp[:, k:k + n] * w[k] for k in range(CONV_W)) + b


def block_diag(x, w, b):
    xb = x.reshape(x.shape[:-1] + (LRU_BLOCKS, LRU_BLOCK_W))
    y = jnp.einsum("bsnw,nwv->bsnv", xb, w) + b
    return y.reshape(x.shape)


def rglru_gates(xc, wa, ba, wi, bi, lam, reset_first):
    r = jax.nn.sigmoid(block_diag(xc, wa, ba)).astype(jnp.float32)
    i = jax.nn.sigmoid(block_diag(xc, wi, bi)).astype(jnp.float32)
    log_a = -LRU_C * r * jax.nn.softplus(-lam.astype(jnp.float32))
    a = jnp.exp(log_a)
    mult = jnp.sqrt(-jnp.expm1(2.0 * log_a))
    if reset_first:
        mult = mult.at[:, 0].set(1.0)
    return a, mult * i * xc.astype(jnp.float32)


def _combine(e1, e2):
    a1, b1 = e1
    a2, b2 = e2
    return a1 * a2, a2 * b1 + b2


def linear_scan(a, u, h0=None):
    if h0 is not None:
        u = u.at[:, 0].add(a[:, 0] * h0)
    _, h = lax.associative_scan(_combine, (a, u), axis=1)
    return h


def rglru_direction(xc_ctx, xc_lat, wa, ba, wi, bi, lam, reverse):
    if reverse:
        xc_ctx = xc_ctx[:, ::-1]
        xc_lat = xc_lat[:, ::-1]
    a_c, u_c = rglru_gates(xc_ctx, wa, ba, wi, bi, lam, True)
    h_c = linear_scan(a_c, u_c)
    a_l, u_l = rglru_gates(xc_lat, wa, ba, wi, bi, lam, False)
    h_l = linear_scan(a_l, u_l, h_c[:, -1])
    if reverse:
        return h_c[:, ::-1], h_l[:, ::-1]
    return h_c, h_l


def diff_attention(q, k, v, lam):
    B, n = q.shape[:2]
    nb = n // Q_BLOCK
    scale = HEAD_DIM ** -0.5
    qb = jnp.moveaxis(q.reshape((B, nb, Q_BLOCK) + q.shape[2:]), 1, 0)

    def block(qblk):
        s = jnp.einsum("bqhcd,bkhcd->bhcqk", qblk, k).astype(jnp.float32) * scale
        p = jax.nn.softmax(s, axis=-1)
        w = (p[:, :, 0] - lam * p[:, :, 1]).astype(v.dtype)
        return jnp.einsum("bhqk,bkhe->bqhe", w, v)

    o = lax.map(block, qb)
    return jnp.moveaxis(o, 0, 1).reshape(B, n, ATT_HEADS, V_DIM)


def hybrid_mixer(hl, hc, rope, w_in, conv_w, conv_b, lru_wa, lru_ba, lru_wi, lru_bi, lru_lambda,
                 q_g, k_g, lq1, lk1, lq2, lk2, subln_g, lam_init, ctx_out):
    B, n, _ = hl.shape
    L = hc.shape[1]
    w_x, w_gate, w_q, w_k, w_v = split_cols(w_in)
    x_l, gate_l, q_l, k_l, v_l = split_cols(hl @ w_in)
    x_c, k_c, v_c = hc @ w_x, hc @ w_k, hc @ w_v

    xl_conv = centred_depthwise_conv(x_l, conv_w, conv_b)
    xc_conv = centred_depthwise_conv(x_c, conv_w, conv_b)
    hf_c, hf_l = rglru_direction(xc_conv, xl_conv, lru_wa[0], lru_ba[0], lru_wi[0], lru_bi[0],
                                 lru_lambda[0], False)
    hb_c, hb_l = rglru_direction(xc_conv, xl_conv, lru_wa[1], lru_ba[1], lru_wi[1], lru_bi[1],
                                 lru_lambda[1], True)
    lru_l = (hf_l + hb_l).astype(hl.dtype) * jax.nn.gelu(gate_l)

    f32 = jnp.float32
    lam = (jnp.exp(jnp.sum(lq1.astype(f32) * lk1.astype(f32)))
           - jnp.exp(jnp.sum(lq2.astype(f32) * lk2.astype(f32))) + lam_init)
    q_l = apply_axial_rope(rmsnorm(q_l.reshape(B, n, ATT_HEADS, 2, HEAD_DIM), q_g), rope)
    k_l = apply_axial_rope(rmsnorm(k_l.reshape(B, n, ATT_HEADS, 2, HEAD_DIM), k_g), rope)
    k_c = rmsnorm(k_c.reshape(B, L, ATT_HEADS, 2, HEAD_DIM), k_g)
    v_l = v_l.reshape(B, n, ATT_HEADS, V_DIM)
    v_c = v_c.reshape(B, L, ATT_HEADS, V_DIM)
    k_all = jnp.concatenate([k_c, k_l], axis=1)
    v_all = jnp.concatenate([v_c, v_l], axis=1)
    att_l = diff_attention(q_l, k_all, v_all, lam)
    att_l = (rmsnorm(att_l, subln_g) * (1 - lam_init)).reshape(B, n, ATT_WIDTH)
    out_l = jnp.concatenate([lru_l, att_l], axis=-1)
    if not ctx_out:
        return out_l, None

    lru_c = (hf_c + hb_c).astype(hc.dtype) * jax.nn.gelu(hc @ w_gate)
    q_c = rmsnorm((hc @ w_q).reshape(B, L, ATT_HEADS, 2, HEAD_DIM), q_g)
    att_c = diff_attention(q_c, k_c, v_c, lam)
    att_c = (rmsnorm(att_c, subln_g) * (1 - lam_init)).reshape(B, L, ATT_WIDTH)
    return out_l, jnp.concatenate([lru_c, att_c], axis=-1)


def expert_choice_ffn(h, w_router, w_gate, w_up, w_down):
    B, n, D = h.shape
    cap = EC_FACTOR * n // N_EXPERTS
    aff = jax.nn.softmax((h @ w_router).astype(jnp.float32), axis=-1)
    g, idx = lax.top_k(jnp.swapaxes(aff, 1, 2), cap)
    xe = jax.vmap(lambda hb, ib: hb[ib])(h, idx)

    def expert(args):
        xk, wg, wu, wd = args
        return (jax.nn.silu(xk @ wg) * (xk @ wu)) @ wd

    ye = lax.map(expert, (jnp.swapaxes(xe, 0, 1), w_gate, w_up, w_down))
    ye = jnp.swapaxes(ye, 0, 1) * g[..., None].astype(h.dtype)
    return jax.vmap(lambda yb, ib: jnp.zeros((n, D), yb.dtype).at[ib.reshape(-1)].add(yb.reshape(-1, D)))(ye, idx)


def setup_inputs(seed: int = 0) -> dict:
    key = jax.random.key(seed)
    ks = jax.random.split(key, 28)
    f32 = jnp.float32

    def nrm(k, shape, s):
        return jax.random.normal(k, shape, f32) * s

    a0 = jax.random.uniform(ks[14], (DEPTH, 2, LRU_WIDTH), f32, 0.9, 0.999)
    s0 = a0 ** (1.0 / LRU_C)
    return {
        "x": nrm(ks[0], (BATCH, SEQ, D_MODEL), 1.0),
        "c": nrm(ks[1], (BATCH, D_MODEL), 1.0),
        "ctx": nrm(ks[2], (BATCH, CTX_LEN, D_MODEL), 1.0),
        "c_ctx": nrm(ks[3], (D_MODEL,), 1.0),
        "w_ada": nrm(ks[4], (DEPTH, D_MODEL, N_MOD * D_MODEL), 0.5 * D_MODEL ** -0.5),
        "b_ada": nrm(ks[5], (DEPTH, N_MOD * D_MODEL), 0.02),
        "norm1_g": 1.0 + nrm(ks[6], (DEPTH, D_MODEL), 0.02),
        "norm2_g": 1.0 + nrm(ks[7], (DEPTH, D_MODEL), 0.02),
        "w_in": nrm(ks[8], (DEPTH, D_MODEL, IN_WIDTH), D_MODEL ** -0.5),
        "conv_w": nrm(ks[9], (DEPTH, CONV_W, LRU_WIDTH), CONV_W ** -0.5),
        "conv_b": nrm(ks[10], (DEPTH, LRU_WIDTH), 0.02),
        "lru_wa": nrm(ks[11], (DEPTH, 2, LRU_BLOCKS, LRU_BLOCK_W, LRU_BLOCK_W), LRU_BLOCK_W ** -0.5),
        "lru_ba": nrm(ks[12], (DEPTH, 2, LRU_BLOCKS, LRU_BLOCK_W), 0.02),
        "lru_wi": nrm(ks[13], (DEPTH, 2, LRU_BLOCKS, LRU_BLOCK_W, LRU_BLOCK_W), LRU_BLOCK_W ** -0.5),
        "lru_bi": nrm(ks[15], (DEPTH, 2, LRU_BLOCKS, LRU_BLOCK_W), 0.02),
        "lru_lambda": jnp.log(s0) - jnp.log1p(-s0),
        "q_norm_g": 1.0 + nrm(ks[16], (DEPTH, HEAD_DIM), 0.02),
        "k_norm_g": 1.0 + nrm(ks[17], (DEPTH, HEAD_DIM), 0.02),
        "lambda_q1": nrm(ks[18], (DEPTH, HEAD_DIM), 0.1),
        "lambda_k1": nrm(ks[19], (DEPTH, HEAD_DIM), 0.1),
        "lambda_q2": nrm(ks[20], (DEPTH, HEAD_DIM), 0.1),
        "lambda_k2": nrm(ks[21], (DEPTH, HEAD_DIM), 0.1),
        "subln_g": 1.0 + nrm(ks[22], (DEPTH, V_DIM), 0.02),
        "w_out": nrm(ks[23], (DEPTH, MIX_WIDTH, D_MODEL), MIX_WIDTH ** -0.5),
        "w_router": nrm(ks[24], (DEPTH, D_MODEL, N_EXPERTS), D_MODEL ** -0.5),
        "w_gate": nrm(ks[25], (DEPTH, N_EXPERTS, D_MODEL, D_EXPERT), D_MODEL ** -0.5),
        "w_up": nrm(ks[26], (DEPTH, N_EXPERTS, D_MODEL, D_EXPERT), D_MODEL ** -0.5),
        "w_down": nrm(ks[27], (DEPTH, N_EXPERTS, D_EXPERT, D_MODEL), D_EXPERT ** -0.5),
    }


def reference(x, c, ctx, c_ctx, w_ada, b_ada, norm1_g, norm2_g, w_in, conv_w, conv_b,
              lru_wa, lru_ba, lru_wi, lru_bi, lru_lambda, q_norm_g, k_norm_g,
              lambda_q1, lambda_k1, lambda_q2, lambda_k2, subln_g, w_out,
              w_router, w_gate, w_up, w_down):
    n = x.shape[1]
    rope = axial_rope_tables(n)
    for layer in range(DEPTH):
        last = layer == DEPTH - 1
        lam_init = 0.8 - 0.6 * math.exp(-0.3 * layer)
        mod_l = adaln(c, w_ada[layer], b_ada[layer], N_MOD)
        mod_c = adaln(c_ctx[None], w_ada[layer], b_ada[layer], 2 if last else N_MOD)
        hl = modulate(rmsnorm(x, norm1_g[layer]), mod_l[:, 0], mod_l[:, 1])
        hc = modulate(rmsnorm(ctx, norm1_g[layer]), mod_c[:, 0], mod_c[:, 1])
        mix_l, mix_c = hybrid_mixer(hl, hc, rope, w_in[layer], conv_w[layer], conv_b[layer],
                                    lru_wa[layer], lru_ba[layer], lru_wi[layer], lru_bi[layer],
                                    lru_lambda[layer], q_norm_g[layer], k_norm_g[layer],
                                    lambda_q1[layer], lambda_k1[layer], lambda_q2[layer],
                                    lambda_k2[layer], subln_g[layer], lam_init, not last)
        x = x + mod_l[:, 2][:, None] * (mix_l @ w_out[layer])
        h2 = modulate(rmsnorm(x, norm2_g[layer]), mod_l[:, 3], mod_l[:, 4])
        x = x + mod_l[:, 5][:, None] * expert_choice_ffn(h2, w_router[layer], w_gate[layer],
                                                         w_up[layer], w_down[layer])
        if not last:
            ctx = ctx + mod_c[:, 2][:, None] * (mix_c @ w_out[layer])
            h2c = modulate(rmsnorm(ctx, norm2_g[layer]), mod_c[:, 3], mod_c[:, 4])
            ctx = ctx + mod_c[:, 5][:, None] * expert_choice_ffn(h2c, w_router[layer], w_gate[layer],
                                                                 w_up[layer], w_down[layer])
    return x
```

```python
import math
from contextlib import ExitStack

import numpy as np
import concourse.bass as bass
import concourse.mybir as mybir
from concourse.bass_utils import run_bass_kernel_spmd

F32 = mybir.dt.float32
BF16 = mybir.dt.bfloat16
I32 = mybir.dt.int32
AF = mybir.ActivationFunctionType
ALU = mybir.AluOpType
AX = mybir.AxisListType

D = 1024
SEQ = 4096
CTX = 256
NT = 34
NE = 16
CAP = 512
DEXP = 2816
NJ = 22
EPS = 1e-6
LAM_INIT = 0.2
NCONST = 936


class Reg:
    __slots__ = ("w", "rs")

    def __init__(self):
        self.w = {}
        self.rs = {}


class Tile:
    def __init__(self, t):
        self.t = t
        self.r = Reg()

    def __getitem__(self, k):
        return self.t[k]


class Eng:
    def __init__(self, h, key, dkeys):
        self.h = h
        self.key = key
        self.n = 0
        self.seen = {}
        self.dkeys = dkeys
        self.dvals = [0] * len(dkeys)
        self.di = 0


def _reg(x):
    return x.r if isinstance(x, Tile) else x


class Sched:
    def __init__(self, nc, stack):
        self.nc = nc
        self.sems = []

        def mk(name):
            s = stack.enter_context(nc.semaphore(name))
            self.sems.append(s)
            return len(self.sems) - 1

        self.pe = Eng(nc.tensor, mk("s_pe"), [])
        self.act = Eng(nc.scalar, mk("s_act"), [])
        self.dve = Eng(nc.vector, mk("s_dve"), [])
        self.pool = Eng(nc.gpsimd, mk("s_pool"), [mk(f"d_pool{i}") for i in range(12)])
        self.sp = Eng(nc.sync, mk("s_sp"), [mk(f"d_sp{i}") for i in range(12)])
        self.engs = [self.pe, self.act, self.dve, self.pool, self.sp]

    def _deps(self, rd, wr):
        deps = {}

        def add(k, v):
            if deps.get(k, 0) < v:
                deps[k] = v

        for r in rd:
            r = _reg(r)
            for k, v in r.w.items():
                add(k, v)
        for r in wr:
            r = _reg(r)
            for k, v in r.w.items():
                add(k, v)
            for k, v in r.rs.items():
                add(k, v)
        return deps

    def _wait(self, e, deps, self_sync):
        for k, v in deps.items():
            if k == e.key and not self_sync:
                continue
            if e.seen.get(k, 0) >= v:
                continue
            e.h.wait_ge(self.sems[k], v)
            e.seen[k] = v

    def _mark(self, ev, rd, wr):
        k, v = ev
        for r in rd:
            r = _reg(r)
            if r.rs.get(k, 0) < v:
                r.rs[k] = v
        for r in wr:
            r = _reg(r)
            r.w[k] = v
            r.rs = {}

    def op(self, e, fn, rd=(), wr=(), self_sync=True):
        self._wait(e, self._deps(rd, wr), self_sync)
        ins = fn(e.h)
        e.n += 1
        ins.then_inc(self.sems[e.key], 1)
        self._mark((e.key, e.n), rd, wr)
        return ins

    def dma(self, q, fn, rd=(), wr=()):
        self._wait(q, self._deps(rd, wr), True)
        slot = q.di % len(q.dkeys)
        q.di += 1
        k = q.dkeys[slot]
        prev = q.dvals[slot]
        if q.seen.get(k, 0) < prev:
            q.h.wait_ge(self.sems[k], prev)
            q.seen[k] = prev
        ins = fn(q.h)
        q.dvals[slot] = prev + 16
        ins.then_inc(self.sems[k], 16)
        self._mark((k, prev + 16), rd, wr)
        return ins

    def barrier(self):
        for e in self.engs:
            for o in self.engs:
                if o.n > 0 and e.seen.get(o.key, 0) < o.n and o is not e:
                    e.h.wait_ge(self.sems[o.key], o.n)
                    e.seen[o.key] = o.n
                for k, v in zip(o.dkeys, o.dvals):
                    if v > 0 and e.seen.get(k, 0) < v:
                        e.h.wait_ge(self.sems[k], v)
                        e.seen[k] = v

    def finish(self):
        q = self.sp
        for e in self.engs:
            for k, v in zip(e.dkeys, e.dvals):
                if v > 0 and q.seen.get(k, 0) < v:
                    q.h.wait_ge(self.sems[k], v)
                    q.seen[k] = v
        for e in self.engs:
            if e is not q and e.n > 0:
                q.h.wait_ge(self.sems[e.key], e.n)


def build(dbg=False, stop_after=99):
    nc = bass.Bass("TRN2", target_bir_lowering=False)
    okind = "ExternalOutput" if dbg else "Internal"

    def din(name, shape, dt=F32):
        return nc.dram_tensor(name, list(shape), dt, kind="ExternalInput").ap()

    x_d = din("x", [SEQ, D])
    ctx_d = din("ctx", [CTX, D])
    cvec_d = din("cvec", [128, 16])
    wada_d = din("w_ada", [D, 6 * D])
    bada_d = din("b_ada", [1, 6 * D])
    normg_d = din("normg", [1, 2 * D])
    qkg_d = din("qkg", [1, 1024])
    lamv_d = din("lamv", [1, 256])
    subg_d = din("subg", [1, 512])
    win_d = din("w_in", [D, 2560])
    convw_d = din("convw", [128, 16])
    convb_d = din("convb", [128, 4])
    lruw_d = din("lruw", [128, 2048])
    lrub_d = din("lrub", [128, 16])
    lrul_d = din("lrul", [128, 8])
    wout_d = din("w_out", [D, D])
    wr_d = din("w_router", [D, NE])
    big = stop_after >= 5
    wg_d = din("w_gate", [NE, D, DEXP] if big else [1, 8, 8])
    wu_d = din("w_up", [NE, D, DEXP] if big else [1, 8, 8])
    wd_d = din("w_down", [NE, DEXP, D] if big else [1, 8, 8])
    rope_d = din("rope", [128, NT * 128])
    consts_d = din("consts", [128, NCONST])
    out_d = nc.dram_tensor("out", [SEQ, D], F32, kind="ExternalOutput").ap()

    xl_d = nc.dram_tensor("xl_s", [4, 128, CTX + SEQ], F32, kind=okind).ap()
    gel_d = nc.dram_tensor("gel_s", [4, 128, SEQ], BF16, kind=okind).ap()
    qT_d = nc.dram_tensor("qT_s", [4, 128, SEQ], BF16, kind=okind).ap()
    kT_d = nc.dram_tensor("kT_s", [4, 128, CTX + SEQ], BF16, kind=okind).ap()
    v_d = nc.dram_tensor("v_s", [NT, 128, 520], BF16, kind=okind).ap()
    h2_d = nc.dram_tensor("h2_s", [SEQ, D], BF16, kind=okind).ap()
    lru_d = nc.dram_tensor("lru_s", [4, 128, SEQ], BF16, kind=okind).ap()
    if dbg:
        dbg_mod = nc.dram_tensor("dbg_mod", [1, 8192], F32, kind="ExternalOutput").ap()
        dbg_aff = nc.dram_tensor("dbg_aff", [128, 32 * NE], F32, kind="ExternalOutput").ap()
        dbg_idx = nc.dram_tensor("dbg_idx", [128, NE * 4], I32, kind="ExternalOutput").ap()
        dbg_g = nc.dram_tensor("dbg_g", [128, NE * 4], F32, kind="ExternalOutput").ap()

    R_xl = [Reg() for _ in range(4)]
    R_gel, R_qT, R_kT, R_v, R_h2, R_out, R_lru = Reg(), Reg(), Reg(), Reg(), Reg(), Reg(), Reg()

    with ExitStack() as top:
        S = Sched(nc, top)
        pe, act, dve, pool, sp = S.pe, S.act, S.dve, S.pool, S.sp

        def sb(stack, name, shape, dt=F32):
            return Tile(stack.enter_context(nc.sbuf_tensor("t_" + name, list(shape), dt)))

        PS = top.enter_context(nc.psum_tensor("PS", [128, 8, 512], F32))
        bankR = [Reg() for _ in range(8)]

        def bank(b):
            return PS[:, b, :]

        def bank_bf(b):
            return PS[:, b, :].bitcast(BF16)

        cst = sb(top, "cst", [128, NCONST])
        cstb = sb(top, "cstb", [128, 384], BF16)
        bcG = sb(top, "bcG", [128, 4, D])
        aff = sb(top, "aff", [128, 32, NE])
        idx_i = sb(top, "idx_i", [128, NE, 4], I32)
        gsl = sb(top, "gsl", [128, NE, 4])
        A1, B1, A1C, B1C, G1, A2, B2, G2 = range(8)
        sc2 = sb(top, "sc2", [128, 2])
        epsT = sb(top, "epsT", [128, 1])
        S.dma(sp, lambda h: h.dma_start(out=cst[:], in_=consts_d), wr=[cst])
        S.op(dve, lambda h: h.tensor_copy(out=cstb[:], in_=cst[:, 0:384]), rd=[cst], wr=[cstb])
        S.op(dve, lambda h: h.memset(epsT[:], EPS), wr=[epsT])
        ident_f = cst[:, 0:128]
        ones_f = cst[:, 256:384]
        iota_f = cst[:, 384:896]
        ident_b = cstb[:, 0:128]
        ustr_b = cstb[:, 128:256]
        ones_b = cstb[:, 256:384]

        def rsqrt_ops(dst, src, scale, n, lo=0):
            S.op(act, lambda h: h.activation(out=dst[:, lo:n], in_=src[:, lo:n], func=AF.Sqrt,
                                             scale=scale, bias=epsT[:, 0:1]),
                 rd=[src, epsT], wr=[dst])
            S.op(dve, lambda h: h.reciprocal(out=dst[:, lo:n], in_=dst[:, lo:n]), rd=[dst], wr=[dst])

        p03 = top.enter_context(ExitStack())
        bcs = sb(p03, "bcs", [128, 512])
        p01 = p03.enter_context(ExitStack())
        bcq = sb(p01, "bcq", [128, 1024])
        bcA = sb(p01, "bcA", [128, 4, D])

        def bcr(i):
            return (bcA, i) if i < 4 else (bcG, i - 4)

        winb = sb(p01, "winb", [128, 8, 2560], BF16)
        ropeT = sb(p01, "ropeT", [128, NT, 2, 64])
        for kc in range(8):
            S.dma(pool, lambda h: h.dma_start(
                out=winb[:, kc, :].rearrange("p (a n) -> p a n", n=640),
                in_=win_d[kc * 128:(kc + 1) * 128, :].rearrange("p (a n) -> p a n", n=640)), wr=[winb])
        S.dma(sp, lambda h: h.dma_start(out=ropeT[:].rearrange("p t c d -> p (t c d)"), in_=rope_d), wr=[ropeT])

        with ExitStack() as p0:
            cv = sb(p0, "cv", [128, 16])
            scv = sb(p0, "scv", [128, 16])
            modrow = sb(p0, "modrow", [1, 8192])
            bada = sb(p0, "bada", [1, 6 * D])
            normg = sb(p0, "normg", [1, 2 * D])
            rowt = sb(p0, "rowt", [1, 3, D])
            qkg = sb(p0, "qkg", [1, 1024])
            lamv = sb(p0, "lamv", [1, 256])
            subg = sb(p0, "subg", [1, 512])
            lt = sb(p0, "lt", [1, 16])
            wb = [sb(p0, f"wadab{i}", [128, 8, 256]) for i in range(2)]
            S.dma(sp, lambda h: h.dma_start(out=cv[:], in_=cvec_d), wr=[cv])
            S.dma(sp, lambda h: h.dma_start(out=bada[:], in_=bada_d), wr=[bada])
            S.dma(sp, lambda h: h.dma_start(out=normg[:], in_=normg_d), wr=[normg])
            S.dma(sp, lambda h: h.dma_start(out=qkg[:], in_=qkg_d), wr=[qkg])
            S.dma(sp, lambda h: h.dma_start(out=lamv[:], in_=lamv_d), wr=[lamv])
            S.dma(sp, lambda h: h.dma_start(out=subg[:], in_=subg_d), wr=[subg])
            S.op(act, lambda h: h.activation(out=scv[:], in_=cv[:], func=AF.Silu), rd=[cv], wr=[scv])
            wada_v = wada_d.rearrange("(kc p) n -> p kc n", p=128)
            CW = 256
            for nb in range(6 * D // CW):
                w = wb[nb % 2]
                S.dma(sp, lambda h: h.dma_start(out=w[:], in_=wada_v[:, :, nb * CW:(nb + 1) * CW]), wr=[w])
                for kc in range(8):
                    S.op(pe, lambda h: h.matmul(bank(0)[0:1, 0:CW], lhsT=scv[:, kc:kc + 1], rhs=w[:, kc, :],
                                                start=(kc == 0), stop=(kc == 7)),
                         rd=[scv, w], wr=[bankR[0]], self_sync=False)
                S.op(dve, lambda h: h.tensor_tensor(out=modrow[0:1, nb * CW:(nb + 1) * CW], in0=bank(0)[0:1, 0:CW],
                                                    in1=bada[0:1, nb * CW:(nb + 1) * CW], op=ALU.add),
                     rd=[bankR[0], bada], wr=[modrow])
                if nb < 2 * D // CW:
                    for kc in range(8):
                        S.op(pe, lambda h: h.matmul(bank(1)[0:1, 0:CW], lhsT=scv[:, 8 + kc:9 + kc], rhs=w[:, kc, :],
                                                    start=(kc == 0), stop=(kc == 7)),
                             rd=[scv, w], wr=[bankR[1]], self_sync=False)
                    S.op(dve, lambda h: h.tensor_tensor(out=modrow[0:1, 6144 + nb * CW:6144 + (nb + 1) * CW],
                                                        in0=bank(1)[0:1, 0:CW], in1=bada[0:1, nb * CW:(nb + 1) * CW],
                                                        op=ALU.add),
                         rd=[bankR[1], bada], wr=[modrow])
            if dbg:
                S.dma(sp, lambda h: h.dma_start(out=dbg_mod, in_=modrow[:]), rd=[modrow])

            def mrow(i):
                return modrow[0:1, i * D:(i + 1) * D]

            for ti, (si, go) in enumerate([(1, 0), (7, 0), (4, D)]):
                S.op(dve, lambda h: h.scalar_tensor_tensor(out=rowt[0:1, ti, :], in0=mrow(si), scalar=1.0,
                                                           in1=normg[0:1, go:go + D], op0=ALU.add, op1=ALU.mult),
                     rd=[modrow, normg], wr=[rowt])
            rows = {A1: rowt[0:1, 0, :], B1: mrow(0), A1C: rowt[0:1, 1, :], B1C: mrow(6), G1: mrow(2),
                    A2: rowt[0:1, 2, :], B2: mrow(3), G2: mrow(5)}
            cnt = 0
            for bi, row in rows.items():
                for hf in range(2):
                    b = 2 + cnt % 2
                    cnt += 1
                    S.op(pe, lambda h: h.matmul(bank(b), lhsT=ones_f[0:1, :], rhs=row[0:1, hf * 512:(hf + 1) * 512],
                                                start=True, stop=True),
                         rd=[cst, modrow, rowt], wr=[bankR[b]], self_sync=False)
                    bt_, bi_ = bcr(bi)
                    S.op(act, lambda h: h.copy(out=bt_[:, bi_, hf * 512:(hf + 1) * 512], in_=bank(b)),
                         rd=[bankR[b]], wr=[bt_])
            for hf in range(2):
                b = 2 + hf
                S.op(pe, lambda h: h.matmul(bank(b), lhsT=ones_f[0:1, :], rhs=qkg[0:1, hf * 512:(hf + 1) * 512],
                                            start=True, stop=True), rd=[cst, qkg], wr=[bankR[b]], self_sync=False)
                S.op(act, lambda h: h.mul(out=bcq[:, hf * 512:(hf + 1) * 512], in_=bank(b),
                                          mul=(0.125 if hf == 0 else 1.0)), rd=[bankR[b]], wr=[bcq])
            S.op(pe, lambda h: h.matmul(bank(2), lhsT=ones_f[0:1, :], rhs=subg[0:1, :], start=True, stop=True),
                 rd=[cst, subg], wr=[bankR[2]], self_sync=False)
            S.op(act, lambda h: h.mul(out=bcs[:], in_=bank(2), mul=1.0 - LAM_INIT), rd=[bankR[2]], wr=[bcs])
            S.op(dve, lambda h: h.tensor_tensor(out=lamv[0:1, 0:64], in0=lamv[0:1, 0:64], in1=lamv[0:1, 64:128],
                                                op=ALU.mult), rd=[lamv], wr=[lamv])
            S.op(dve, lambda h: h.tensor_tensor(out=lamv[0:1, 128:192], in0=lamv[0:1, 128:192],
                                                in1=lamv[0:1, 192:256], op=ALU.mult), rd=[lamv], wr=[lamv])
            S.op(dve, lambda h: h.reduce_sum(out=lt[0:1, 0:1], in_=lamv[0:1, 0:64], axis=AX.X), rd=[lamv], wr=[lt])
            S.op(dve, lambda h: h.reduce_sum(out=lt[0:1, 1:2], in_=lamv[0:1, 128:192], axis=AX.X), rd=[lamv], wr=[lt])
            S.op(act, lambda h: h.activation(out=lt[0:1, 2:4], in_=lt[0:1, 0:2], func=AF.Exp), rd=[lt], wr=[lt])
            S.op(dve, lambda h: h.tensor_tensor(out=lt[0:1, 4:5], in0=lt[0:1, 3:4], in1=lt[0:1, 2:3],
                                                op=ALU.subtract), rd=[lt], wr=[lt])
            S.op(dve, lambda h: h.tensor_scalar_add(out=lt[0:1, 4:5], in0=lt[0:1, 4:5], scalar1=-LAM_INIT),
                 rd=[lt], wr=[lt])
            S.op(dve, lambda h: h.reduce_max(out=lt[0:1, 6:7], in_=qkg[0:1, 0:64], axis=AX.X,
                                             apply_absolute_value=True), rd=[qkg], wr=[lt])
            S.op(dve, lambda h: h.reduce_max(out=lt[0:1, 7:8], in_=qkg[0:1, 512:576], axis=AX.X,
                                             apply_absolute_value=True), rd=[qkg], wr=[lt])
            S.op(dve, lambda h: h.tensor_tensor(out=lt[0:1, 5:6], in0=lt[0:1, 6:7], in1=lt[0:1, 7:8], op=ALU.mult),
                 rd=[lt], wr=[lt])
            S.op(dve, lambda h: h.tensor_scalar_mul(out=lt[0:1, 5:6], in0=lt[0:1, 5:6], scalar1=-8.0),
                 rd=[lt], wr=[lt])
            S.op(pe, lambda h: h.matmul(bank(3)[:, 0:2], lhsT=ones_f[0:1, :], rhs=lt[0:1, 4:6], start=True, stop=True),
                 rd=[cst, lt], wr=[bankR[3]], self_sync=False)
            S.op(act, lambda h: h.copy(out=sc2[:], in_=bank(3)[:, 0:2]), rd=[bankR[3]], wr=[sc2])
        S.barrier()
        if stop_after < 1:
            S.finish()
            return nc

        with ExitStack() as p1:
            hlT = [sb(p1, f"hlT{i}", [128, 8, 512], BF16) for i in range(2)]
            xb = [sb(p1, f"xb{i}", [128, D]) for i in range(4)]
            junk = sb(p1, "junk", [128, D], BF16)
            ss4 = [sb(p1, f"ss4{i}", [128, 4]) for i in range(2)]
            rs4 = [sb(p1, f"rs4{i}", [128, 4]) for i in range(2)]
            t1 = sb(p1, "t1_0", [128, D])
            hl = [sb(p1, f"hl{i}", [128, D], BF16) for i in range(2)]
            xlst = [sb(p1, f"xlst{i}", [128, 512]) for i in range(2)]
            gst = sb(p1, "gst0", [128, 4, 512], BF16)
            vst = [sb(p1, f"vst{i}", [128, 4, 4, 130], BF16) for i in range(2)]
            sq = [sb(p1, f"sq{i}", [128, D]) for i in range(2)]
            ss16 = [sb(p1, f"ss16{i}", [128, 16]) for i in range(2)]
            rs16 = [sb(p1, f"rs16{i}", [128, 16]) for i in range(2)]
            tq = [sb(p1, f"tq{i}", [128, D]) for i in range(2)]
            r1 = [sb(p1, f"r1{i}", [128, D]) for i in range(2)]
            qkr = [sb(p1, f"qkr{i}", [128, D], BF16) for i in range(2)]
            qkst = [sb(p1, f"qkst{i}", [128, 8, 512], BF16) for i in range(2)]
            for v in vst:
                S.op(pool, lambda h: h.memset(v[:], 1.0), wr=[v])

            def blk_tiles(blk):
                return [0, 1] if blk == 0 else [2 + 4 * (blk - 1) + i for i in range(4)]

            cnts = {"x": 0, "f": 0, "t": 0}

            hl_state = {}

            def hl_chain(blk, i):
                xts, r4 = hl_state[blk]
                ai, bi = (A1C, B1C) if blk == 0 else (A1, B1)
                xt, hh = xts[i], hl[i % 2]
                S.op(dve, lambda h: h.scalar_tensor_tensor(out=t1[:], in0=xt[:], scalar=r4[:, i:i + 1],
                                                           in1=bcA[:, ai, :], op0=ALU.mult, op1=ALU.mult),
                     rd=[xt, r4, bcA], wr=[t1])
                S.op(dve, lambda h: h.tensor_tensor(out=hh[:], in0=t1[:], in1=bcA[:, bi, :], op=ALU.add),
                     rd=[t1, bcA], wr=[hh])

            def hl_T(blk, i):
                hT, hh = hlT[blk % 2], hl[i % 2]
                for kc in range(8):
                    S.op(pe, lambda h: h.transpose(out=bank_bf(0)[:, kc * 128:(kc + 1) * 128],
                                                   in_=hh[:, kc * 128:(kc + 1) * 128], identity=ident_b),
                         rd=[hh, cstb], wr=[bankR[0]], self_sync=False)
                S.op(act, lambda h: h.copy(out=hT[:, :, i * 128:(i + 1) * 128],
                                           in_=bank_bf(0).rearrange("p (k t) -> p k t", t=128)),
                     rd=[bankR[0]], wr=[hT])

            def hl_front(blk):
                tiles = blk_tiles(blk)
                nt = len(tiles)
                s4, r4 = ss4[blk % 2], rs4[blk % 2]
                xts = []
                for i, T in enumerate(tiles):
                    xt = xb[cnts["x"] % 4]
                    cnts["x"] += 1
                    src = ctx_d[T * 128:(T + 1) * 128, :] if T < 2 else x_d[(T - 2) * 128:(T - 1) * 128, :]
                    S.dma(sp, lambda h: h.dma_start(out=xt[:], in_=src), wr=[xt])
                    S.op(act, lambda h: h.activation(out=junk[:], in_=xt[:], func=AF.Square,
                                                     accum_out=s4[:, i:i + 1]), rd=[xt], wr=[junk, s4])
                    xts.append(xt)
                rsqrt_ops(r4, s4, 1.0 / D, nt)
                hl_state[blk] = (xts, r4)
                for i in range(min(2, nt)):
                    hl_chain(blk, i)

            def hl_back(blk):
                nt = len(blk_tiles(blk))
                for i in range(nt):
                    if i >= 2:
                        hl_chain(blk, i)
                    hl_T(blk, i)

            def hlstage(blk):
                hl_front(blk)
                hl_back(blk)

            def fmstage(blk):
                tiles = blk_tiles(blk)
                W = 128 * len(tiles)
                toff = 0 if blk == 0 else CTX + (blk - 1) * 512
                loff = (blk - 1) * 512
                hT = hlT[blk % 2]
                for oc in range(4 if blk == 0 else 8):
                    b = 2 + cnts["f"] % 2
                    cnts["f"] += 1
                    for kc in range(8):
                        S.op(pe, lambda h: h.matmul(bank(b)[:, 0:W], lhsT=winb[:, kc, oc * 128:(oc + 1) * 128],
                                                    rhs=hT[:, kc, 0:W], start=(kc == 0), stop=(kc == 7)),
                             rd=[winb, hT], wr=[bankR[b]], self_sync=False)
                    if oc < 4:
                        st = xlst[oc % 2]
                        S.op(dve, lambda h: h.tensor_copy(out=st[:, 0:W], in_=bank(b)[:, 0:W]),
                             rd=[bankR[b]], wr=[st])
                        S.dma(sp, lambda h: h.dma_start(out=xl_d[oc, :, toff:toff + W], in_=st[:, 0:W]),
                              rd=[st], wr=[R_xl[oc]])
                    else:
                        S.op(act, lambda h: h.activation(out=gst[:, oc - 4, :], in_=bank(b), func=AF.Gelu_apprx_tanh),
                             rd=[bankR[b]], wr=[gst])
                if blk > 0:
                    S.dma(sp, lambda h: h.dma_start(out=gel_d.rearrange("c p t -> p c t")[:, :, loff:loff + 512],
                                                    in_=gst[:]), rd=[gst], wr=[R_gel])

            def stage_a(blk, i):
                T = blk_tiles(blk)[i]
                hT = hlT[blk % 2]
                vs = vst[blk % 2]
                g0 = 8 if blk == 0 else 0
                c0 = g0 * 64
                u = cnts["t"] % 2
                qb_ = 4 + 2 * u
                for (b, col) in ([(qb_, 1024)] if blk > 0 else []) + [(qb_ + 1, 1536), (1, 2048)]:
                    for kc in range(8):
                        S.op(pe, lambda h: h.matmul(bank(b), lhsT=hT[:, kc, i * 128:(i + 1) * 128],
                                                    rhs=winb[:, kc, col:col + 512], start=(kc == 0), stop=(kc == 7)),
                             rd=[winb, hT], wr=[bankR[b]], self_sync=False)
                S.op(act, lambda h: h.copy(out=vs[:, i, :, 0:128],
                                           in_=bank(1).rearrange("p (a e) -> p a e", e=128)),
                     rd=[bankR[1]], wr=[vs])
                pqk = PS[:, qb_:qb_ + 2, :].rearrange("p a n -> p (a n)")
                S.op(act, lambda h: h.activation(out=sq[u][:, c0:], in_=pqk[:, c0:], func=AF.Square),
                     rd=[bankR[qb_], bankR[qb_ + 1]], wr=[sq[u]])
                cnts["t"] += 1
                return u

            def stage_a2(blk, i, u):
                g0 = 8 if blk == 0 else 0
                c0 = g0 * 64
                qb_ = 4 + 2 * u
                pqk = PS[:, qb_:qb_ + 2, :].rearrange("p a n -> p (a n)")
                S.op(dve, lambda h: h.tensor_reduce(out=ss16[u][:, g0:], in_=sq[u][:, c0:].rearrange("p (g d) -> p g d", d=64),
                                                    axis=AX.X, op=ALU.add), rd=[sq[u]], wr=[ss16[u]])
                rsqrt_ops(rs16[u], ss16[u], 1.0 / 64, 16, g0)
                S.op(dve, lambda h: h.tensor_tensor(
                    out=tq[u][:, c0:].rearrange("p (g d) -> p g d", d=64),
                    in0=pqk[:, c0:].rearrange("p (g d) -> p g d", d=64),
                    in1=rs16[u][:, g0:].unsqueeze(2).broadcast_to([128, 16 - g0, 64]), op=ALU.mult),
                    rd=[bankR[qb_], bankR[qb_ + 1], rs16[u]], wr=[tq[u]])
                S.op(dve, lambda h: h.tensor_tensor(out=tq[u][:, c0:], in0=tq[u][:, c0:], in1=bcq[:, c0:], op=ALU.mult),
                     rd=[tq[u], bcq], wr=[tq[u]])

            def stage_b(blk, i, u):
                T = blk_tiles(blk)[i]
                qs = qkst[blk % 2]
                g0 = 8 if blk == 0 else 0
                c0 = g0 * 64
                ng = 16 - g0
                r2 = sq[u]
                S.op(pool, lambda h: h.tensor_tensor(
                    out=r1[u][:, c0:].rearrange("p (g d) -> p g d", d=64),
                    in0=tq[u][:, c0:].rearrange("p (g d) -> p g d", d=64),
                    in1=ropeT[:, T, 0, :].unsqueeze(1).broadcast_to([128, ng, 64]), op=ALU.mult),
                    rd=[tq[u], ropeT], wr=[r1[u]])
                tq5 = tq[u][:, c0:].rearrange("p (g t h w) -> p g t h w", t=2, h=2, w=16)
                r25 = r2[:, c0:].rearrange("p (g t h w) -> p g t h w", t=2, h=2, w=16)
                sn4 = ropeT[:, T, 1, :].rearrange("p (t h w) -> p t h w", t=2, h=2)
                for hv in range(2):
                    S.op(pool, lambda h: h.tensor_tensor(
                        out=r25[:, :, :, hv, :], in0=tq5[:, :, :, 1 - hv, :],
                        in1=sn4[:, :, hv, :].unsqueeze(1).broadcast_to([128, ng, 2, 16]), op=ALU.mult),
                        rd=[tq[u], ropeT], wr=[r2])

            def stage_b2(blk, i, u):
                qs = qkst[blk % 2]
                g0 = 8 if blk == 0 else 0
                c0 = g0 * 64
                r2 = sq[u]
                qq = qkr[u]
                S.op(pool, lambda h: h.tensor_tensor(out=qq[:, c0:], in0=r1[u][:, c0:], in1=r2[:, c0:], op=ALU.add),
                     rd=[r1[u], r2], wr=[qq])
                k0 = g0 // 2
                for kc in range(k0, 8):
                    S.op(pe, lambda h: h.transpose(out=bank_bf(0)[:, kc * 128:(kc + 1) * 128],
                                                   in_=qq[:, kc * 128:(kc + 1) * 128], identity=ident_b),
                         rd=[qq, cstb], wr=[bankR[0]], self_sync=False)
                S.op(act, lambda h: h.copy(out=qs[:, k0:8, i * 128:(i + 1) * 128],
                                           in_=bank_bf(0).rearrange("p (k t) -> p k t", t=128)[:, k0:8, :]),
                     rd=[bankR[0]], wr=[qs])

            def outstage(blk):
                tiles = blk_tiles(blk)
                nt = len(tiles)
                W = 128 * nt
                toff = 0 if blk == 0 else CTX + (blk - 1) * 512
                loff = (blk - 1) * 512
                qs = qkst[blk % 2]
                vs = vst[blk % 2]
                if blk > 0:
                    S.dma(sp, lambda h: h.dma_start(out=qT_d.rearrange("c p t -> p c t")[:, :, loff:loff + 512],
                                                    in_=qs[:, 0:4, :]), rd=[qs], wr=[R_qT])
                S.dma(sp, lambda h: h.dma_start(out=kT_d.rearrange("c p t -> p c t")[:, :, toff:toff + W],
                                                in_=qs[:, 4:8, 0:W]), rd=[qs], wr=[R_kT])
                S.dma(sp, lambda h: h.dma_start(
                    out=v_d[tiles[0]:tiles[0] + nt, :, :].rearrange("t p f -> p t f"),
                    in_=vs[:, 0:nt, :, :].rearrange("p t a e -> p t (a e)")), rd=[vs], wr=[R_v])

            flat = [(blk, i) for blk in range(9) for i in range(len(blk_tiles(blk)))]
            hlstage(0)
            fmstage(0)
            hlstage(1)
            prev = None
            for (blk, i) in flat:
                u = stage_a(blk, i)
                last = (i == len(blk_tiles(blk)) - 1)
                if i == len(blk_tiles(blk)) - 2 and blk + 2 < 9:
                    hl_front(blk + 2)
                if last and blk + 1 < 9:
                    fmstage(blk + 1)
                    if blk + 2 < 9:
                        hl_back(blk + 2)
                if prev is not None:
                    stage_b(*prev)
                stage_a2(blk, i, u)
                if prev is not None:
                    stage_b2(*prev)
                    if prev[1] == len(blk_tiles(prev[0])) - 1:
                        outstage(prev[0])
                prev = (blk, i, u)
            stage_b(*prev)
            stage_b2(*prev)
            outstage(prev[0])
        p01.close()
        S.barrier()
        if stop_after < 2:
            S.finish()
            return nc


        with ExitStack() as p2:
            TT = CTX + SEQ
            HL = SEQ // 2
            convw = sb(p2, "convw", [128, 16])
            convb = sb(p2, "convb", [128, 4])
            lrub = sb(p2, "lrub", [128, 16])
            lrul = sb(p2, "lrul", [128, 8])
            cL = sb(p2, "cL", [128, 8])
            cL2 = sb(p2, "cL2", [128, 8])
            onesT = sb(p2, "onesT", [128, 1])
            lruwb = sb(p2, "lruwb", [128, 16, 128], BF16)
            XP = sb(p2, "XP", [128, TT + 8])
            xcs = [sb(p2, f"xc{i}", [128, TT]) for i in range(2)]
            xcbs = [sb(p2, f"xcb{i}", [128, TT], BF16) for i in range(2)]
            Rs = [sb(p2, f"Rr{i}", [128, HL]) for i in range(2)]
            As = [sb(p2, f"A2_{i}", [128, HL]) for i in range(2)]
            Is = [sb(p2, f"Ii{i}", [128, HL]) for i in range(2)]
            Hf = sb(p2, "Hf", [128, TT])
            Hb = sb(p2, "Hb", [128, TT])
            gl = sb(p2, "gl", [128, SEQ], BF16)
            lst = sb(p2, "lst", [128, SEQ], BF16)
            S.dma(sp, lambda h: h.dma_start(out=convw[:], in_=convw_d), wr=[convw])
            S.dma(sp, lambda h: h.dma_start(out=convb[:], in_=convb_d), wr=[convb])
            S.dma(sp, lambda h: h.dma_start(out=lrub[:], in_=lrub_d), wr=[lrub])
            S.dma(sp, lambda h: h.dma_start(out=lrul[:], in_=lrul_d), wr=[lrul])
            S.dma(pool, lambda h: h.dma_start(out=lruwb[:], in_=lruw_d.rearrange("p (a n) -> p a n", n=128)),
                  wr=[lruwb])
            S.op(act, lambda h: h.activation(out=cL[:], in_=lrul[:], func=AF.Exp, scale=-1.0), rd=[lrul], wr=[cL])
            S.op(act, lambda h: h.activation(out=cL[:], in_=cL[:], func=AF.Ln, bias=1.0), rd=[cL], wr=[cL])
            S.op(dve, lambda h: h.tensor_scalar_mul(out=cL2[:], in0=cL[:], scalar1=-16.0), rd=[cL], wr=[cL2])
            S.op(dve, lambda h: h.tensor_scalar_mul(out=cL[:], in0=cL[:], scalar1=-8.0), rd=[cL, cL2], wr=[cL])
            S.op(dve, lambda h: h.memset(onesT[:], 1.0), wr=[onesT])
            S.op(dve, lambda h: h.memset(XP[:], 0.0), wr=[XP])
            segs = [(1, 0, CTX), (260, CTX, SEQ)]

            def conv(j):
                xc, xcb = xcs[j % 2], xcbs[j % 2]
                S.dma(sp, lambda h: h.dma_start(out=XP[:, 1:1 + CTX], in_=xl_d[j, :, 0:CTX]), rd=[R_xl[j]], wr=[XP])
                S.dma(sp, lambda h: h.dma_start(out=XP[:, 260:260 + SEQ], in_=xl_d[j, :, CTX:TT]),
                      rd=[R_xl[j]], wr=[XP])
                for (xo, to, ln) in segs:
                    S.op(dve, lambda h: h.tensor_scalar(out=xc[:, to:to + ln], in0=XP[:, xo - 1:xo - 1 + ln],
                                                         scalar1=convw[:, j * 4:j * 4 + 1], scalar2=convb[:, j:j + 1],
                                                         op0=ALU.mult, op1=ALU.add),
                         rd=[XP, convw, convb], wr=[xc])
                    for k in range(1, 4):
                        S.op(dve, lambda h: h.scalar_tensor_tensor(
                            out=xc[:, to:to + ln], in0=XP[:, xo - 1 + k:xo - 1 + k + ln],
                            scalar=convw[:, j * 4 + k:j * 4 + k + 1], in1=xc[:, to:to + ln],
                            op0=ALU.mult, op1=ALU.add), rd=[XP, convw, xc], wr=[xc])
                S.op(dve, lambda h: h.tensor_copy(out=xcb[:], in_=xc[:]), rd=[xc], wr=[xcb])

            pc = {"n": 0, "b": 0}

            def piece(j, d, t0, ln, first_col, init, rev):
                xc, xcb = xcs[j % 2], xcbs[j % 2]
                s_ = pc["n"] % 2
                pc["n"] += 1
                Rr, A2_, Ii = Rs[s_], As[s_], Is[s_]
                H = Hf if d == 0 else Hb
                for o in range(0, ln, 512):
                    w_ = min(512, ln - o)
                    for gi, dst in enumerate([Rr, Ii]):
                        b = 2 + pc["b"] % 4
                        pc["b"] += 1
                        S.op(pe, lambda h: h.matmul(bank(b)[:, 0:w_], lhsT=lruwb[:, (d * 2 + gi) * 4 + j, :],
                                                    rhs=xcb[:, t0 + o:t0 + o + w_], start=True, stop=True),
                             rd=[lruwb, xcb], wr=[bankR[b]], self_sync=False)
                        bi_ = (d * 2 + gi) * 4 + j
                        S.op(act, lambda h: h.activation(out=dst[:, o:o + w_], in_=bank(b)[:, 0:w_],
                                                         func=AF.Sigmoid, bias=lrub[:, bi_:bi_ + 1]),
                             rd=[bankR[b], lrub], wr=[dst])
                ci = d * 4 + j
                S.op(act, lambda h: h.activation(out=Rr[:, 0:ln], in_=Rr[:, 0:ln], func=AF.Exp, scale=cL[:, ci:ci + 1]),
                     rd=[Rr, cL], wr=[Rr])
                S.op(dve, lambda h: h.scalar_tensor_tensor(out=A2_[:, 0:ln], in0=Rr[:, 0:ln], scalar=1.0, in1=Rr[:, 0:ln],
                                                           op0=ALU.min, op1=ALU.mult), rd=[Rr], wr=[A2_])
                S.op(act, lambda h: h.activation(out=A2_[:, 0:ln], in_=A2_[:, 0:ln], func=AF.Sqrt, scale=-1.0,
                                                 bias=onesT[:, 0:1]), rd=[A2_, onesT], wr=[A2_])
                if first_col is not None:
                    S.op(dve, lambda h: h.memset(A2_[:, first_col:first_col + 1], 1.0), wr=[A2_])
                S.op(dve, lambda h: h.tensor_tensor(out=Ii[:, 0:ln], in0=Ii[:, 0:ln], in1=A2_[:, 0:ln], op=ALU.mult),
                     rd=[Ii, A2_], wr=[Ii])
                S.op(dve, lambda h: h.tensor_tensor(out=Ii[:, 0:ln], in0=Ii[:, 0:ln], in1=xc[:, t0:t0 + ln], op=ALU.mult),
                     rd=[Ii, xc], wr=[Ii])
                hv, av, uv = H[:, t0:t0 + ln], Rr[:, 0:ln], Ii[:, 0:ln]
                if rev:
                    hv, av, uv = hv[:, ::-1], av[:, ::-1], uv[:, ::-1]
                S.op(dve, lambda h: h.tensor_tensor_scan(out=hv, data0=av, data1=uv, initial=init,
                                                         op0=ALU.mult, op1=ALU.add), rd=[Rr, Ii, H], wr=[H])

            conv(0)
            for j in range(4):
                S.dma(sp, lambda h: h.dma_start(out=gl[:], in_=gel_d[j, :, :]), rd=[R_gel], wr=[gl])
                if j + 1 < 4:
                    conv(j + 1)
                piece(j, 0, 0, CTX, 0, 0.0, False)
                piece(j, 0, CTX, HL, None, Hf[:, CTX - 1:CTX], False)
                piece(j, 0, CTX + HL, HL, None, Hf[:, CTX + HL - 1:CTX + HL], False)
                piece(j, 1, 0, CTX, CTX - 1, 0.0, True)
                piece(j, 1, CTX + HL, HL, None, Hb[:, 0:1], True)
                piece(j, 1, CTX, HL, None, Hb[:, CTX + HL:CTX + HL + 1], True)
                S.op(dve, lambda h: h.tensor_tensor(out=Hf[:, CTX:TT], in0=Hf[:, CTX:TT], in1=Hb[:, CTX:TT], op=ALU.add),
                     rd=[Hf, Hb], wr=[Hf])
                S.op(dve, lambda h: h.tensor_tensor(out=lst[:], in0=Hf[:, CTX:TT], in1=gl[:], op=ALU.mult),
                     rd=[Hf, gl], wr=[lst])
                S.dma(sp, lambda h: h.dma_start(out=lru_d[j, :, :], in_=lst[:]), rd=[lst], wr=[R_lru])
        S.barrier()
        if stop_after < 3:
            S.finish()
            return nc


        with ExitStack() as p3:
            kT = sb(p3, "kT", [128, 4, CTX + SEQ], BF16)
            v1 = sb(p3, "v1", [128, NT, 520], BF16)
            woutb = sb(p3, "woutb", [128, 8, D], BF16)
            wrt = sb(p3, "wrt", [128, 8, NE])
            qz = [[sb(p3, f"qz{i}_{c}", [128, 4, 512], BF16) for c in range(2)] for i in range(2)]
            for i in range(2):
                for c in range(2):
                    S.op(pool, lambda h: h.memset(qz[i][c][:], 0.0), wr=[qz[i][c]])
            lruB = [sb(p3, f"lruB{i}", [128, 4, 512], BF16) for i in range(2)]
            Eb = [sb(p3, f"Eb{i}", [128, 1024], BF16) for i in range(3)]
            osb = [sb(p3, f"osb{i}", [128, 4, 128]) for i in range(2)]
            rl = sb(p3, "rl", [128, 8])
            ssn = sb(p3, "ssn", [128, 8])
            rsn = sb(p3, "rsn", [128, 8])
            junk3 = sb(p3, "junk3", [128, D], BF16)
            att = sb(p3, "att", [128, 4, 512], BF16)
            attT = sb(p3, "attT", [128, 4, 512], BF16)
            xres = [sb(p3, f"xres{i}", [128, D]) for i in range(2)]
            x1t = [sb(p3, f"x1t{i}", [128, D]) for i in range(2)]
            tmp3s = [sb(p3, f"tmp3{i}", [128, D]) for i in range(2)]
            h2fs = [sb(p3, f"h2f{i}", [128, D]) for i in range(2)]
            h2b = [sb(p3, f"h2b{i}", [128, D], BF16) for i in range(2)]
            h2Ts = [sb(p3, f"h2T{i}", [128, 8, 128]) for i in range(2)]
            lg = sb(p3, "lg", [128, NE])
            mx = sb(p3, "mx", [128, 4])
            S.dma(sp, lambda h: h.dma_start(out=kT[:], in_=kT_d.rearrange("c p t -> p c t")), rd=[R_kT], wr=[kT])
            S.dma(sp, lambda h: h.dma_start(out=v1[:], in_=v_d.rearrange("t p f -> p t f")), rd=[R_v], wr=[v1])
            S.dma(sp, lambda h: h.dma_start(out=wrt[:], in_=wr_d.rearrange("(kc p) n -> p kc n", p=128)), wr=[wrt])
            for kc in range(8):
                S.dma(pool, lambda h: h.dma_start(out=woutb[:, kc, :], in_=wout_d[kc * 128:(kc + 1) * 128, :]),
                      wr=[woutb])
            ssn2 = sb(p3, "ssn2", [128, 2])
            rsn2 = sb(p3, "rsn2", [128, 2])
            junk4 = sb(p3, "junk4", [128, D], BF16)

            def tail_steps(qb, lb, B0, B1):
                steps = []
                for s_ in range(4):
                    def st_t(s_=s_):
                        for hd in range(4):
                            S.op(pe, lambda h: h.transpose(out=bank_bf(B0)[:, hd * 128:(hd + 1) * 128],
                                                           in_=att[:, s_, hd * 128:(hd + 1) * 128], identity=ident_b),
                                 rd=[att, cstb], wr=[bankR[B0]], self_sync=False)
                        S.op(act, lambda h: h.copy(out=attT[:, :, s_ * 128:(s_ + 1) * 128],
                                                   in_=bank_bf(B0)[:, 0:512].rearrange("p (k t) -> p k t", t=128)),
                             rd=[bankR[B0]], wr=[attT])
                    steps.append(st_t)
                per_tile = []
                for s_ in range(4):
                    tok0 = qb * 512 + s_ * 128
                    tl = qb * 4 + s_
                    xr, x1 = xres[s_ % 2], x1t[s_ % 2]
                    hb_ = h2b[s_ % 2]
                    tmp3, h2f, h2T = tmp3s[s_ % 2], h2fs[s_ % 2], h2Ts[s_ % 2]

                    def st_w(fh, s_=s_, tok0=tok0, xr=xr, tmp3=tmp3):
                        bk = B0 if fh == 0 else B1
                        if fh == 0:
                            S.dma(sp, lambda h: h.dma_start(out=xr[:], in_=x_d[tok0:tok0 + 128, :]), wr=[xr])
                        for kc in range(8):
                            lhs = (lb[:, kc, s_ * 128:(s_ + 1) * 128] if kc < 4 else attT[:, kc - 4, s_ * 128:(s_ + 1) * 128])
                            S.op(pe, lambda h: h.matmul(bank(bk), lhsT=lhs, rhs=woutb[:, kc, fh * 512:(fh + 1) * 512],
                                                        start=(kc == 0), stop=(kc == 7)),
                                 rd=[lb, attT, woutb], wr=[bankR[bk]], self_sync=False)
                        S.op(dve, lambda h: h.tensor_tensor(out=tmp3[:, fh * 512:(fh + 1) * 512], in0=bank(bk),
                                                            in1=bcG[:, 0, fh * 512:(fh + 1) * 512], op=ALU.mult),
                             rd=[bankR[bk], bcG], wr=[tmp3])
                    tile_steps = {}
                    tile_steps["w0"] = (lambda st_w=st_w: st_w(0))

                    def st_n(st_w=st_w, tok0=tok0, xr=xr, x1=x1, hb_=hb_, tmp3=tmp3, h2f=h2f):
                        st_w(1)
                        S.op(pool, lambda h: h.tensor_tensor(out=x1[:], in0=tmp3[:], in1=xr[:], op=ALU.add),
                             rd=[tmp3, xr], wr=[x1])
                        S.dma(sp, lambda h: h.dma_start(out=out_d[tok0:tok0 + 128, :], in_=x1[:]), rd=[x1], wr=[R_out])
                        S.op(act, lambda h: h.activation(out=junk4[:], in_=x1[:], func=AF.Square, accum_out=ssn2[:, 0:1]),
                             rd=[x1], wr=[junk4, ssn2])
                        S.op(act, lambda h: h.activation(out=rsn2[:, 0:1], in_=ssn2[:, 0:1], func=AF.Ln, scale=1.0 / D,
                                                         bias=epsT[:, 0:1]), rd=[ssn2, epsT], wr=[rsn2])
                        S.op(act, lambda h: h.activation(out=rsn2[:, 0:1], in_=rsn2[:, 0:1], func=AF.Exp, scale=-0.5),
                             rd=[rsn2], wr=[rsn2])
                        S.op(dve, lambda h: h.scalar_tensor_tensor(out=tmp3[:], in0=x1[:], scalar=rsn2[:, 0:1], in1=bcG[:, 1, :],
                                                                   op0=ALU.mult, op1=ALU.mult), rd=[x1, rsn2, bcG], wr=[tmp3])
                        S.op(pool, lambda h: h.tensor_tensor(out=h2f[:], in0=tmp3[:], in1=bcG[:, 2, :], op=ALU.add),
                             rd=[tmp3, bcG], wr=[h2f])
                        S.op(act, lambda h: h.copy(out=hb_[:], in_=h2f[:]), rd=[h2f], wr=[hb_])
                        S.dma(sp, lambda h: h.dma_start(out=h2_d[tok0:tok0 + 128, :], in_=hb_[:]), rd=[hb_], wr=[R_h2])
                    tile_steps["n"] = st_n

                    def st_r(half, h2f=h2f, h2T=h2T):
                        for k4 in range(4):
                            kc = half * 4 + k4
                            S.op(pe, lambda h: h.transpose(out=bank(B0)[:, k4 * 128:(k4 + 1) * 128],
                                                           in_=h2f[:, kc * 128:(kc + 1) * 128], identity=ident_f),
                                 rd=[h2f, cst], wr=[bankR[B0]], self_sync=False)
                        S.op(dve, lambda h: h.tensor_copy(out=h2T[:, half * 4:half * 4 + 4, :],
                                                          in_=bank(B0).rearrange("p (k t) -> p k t", t=128)),
                             rd=[bankR[B0]], wr=[h2T])
                    tile_steps["r0"] = (lambda st_r=st_r: st_r(0))
                    tile_steps["r1"] = (lambda st_r=st_r: st_r(1))

                    def st_s(tl=tl, h2T=h2T):
                        for kc in range(8):
                            S.op(pe, lambda h: h.matmul(bank(B0)[:, 0:NE], lhsT=h2T[:, kc, :], rhs=wrt[:, kc, :],
                                                        start=(kc == 0), stop=(kc == 7)),
                                 rd=[h2T, wrt], wr=[bankR[B0]], self_sync=False)
                        S.op(dve, lambda h: h.reduce_max(out=mx[:, 0:1], in_=bank(B0)[:, 0:NE], axis=AX.X),
                             rd=[bankR[B0]], wr=[mx])
                        S.op(dve, lambda h: h.tensor_scalar_mul(out=mx[:, 1:2], in0=mx[:, 0:1], scalar1=-1.0), rd=[mx], wr=[mx])
                        S.op(act, lambda h: h.activation(out=lg[:], in_=bank(B0)[:, 0:NE], func=AF.Exp, bias=mx[:, 1:2],
                                                         accum_out=mx[:, 2:3]), rd=[bankR[B0], mx], wr=[lg, mx])
                        S.op(dve, lambda h: h.reciprocal(out=mx[:, 3:4], in_=mx[:, 2:3]), rd=[mx], wr=[mx])
                        S.op(dve, lambda h: h.tensor_scalar_mul(out=aff[:, tl, :], in0=lg[:], scalar1=mx[:, 3:4]),
                             rd=[lg, mx], wr=[aff])
                    tile_steps["sm"] = st_s
                    per_tile.append(tile_steps)
                order = [("w0", 0), ("n", 0), ("w0", 1), ("n", 1), ("r0", 0), ("r1", 0), ("w0", 2), ("n", 2), ("sm", 0),
                         ("r0", 1), ("r1", 1), ("w0", 3), ("n", 3), ("sm", 1), ("r0", 2), ("r1", 2), ("sm", 2),
                         ("r0", 3), ("r1", 3), ("sm", 3)]
                for (k_, t_) in order:
                    steps.append(per_tile[t_][k_])
                return steps

            pending = []
            deferred = []
            gcnt = {"g": 0}
            for qb in range(8):
                qt = qz[qb % 2]
                for c in range(2):
                    S.dma(sp, lambda h: h.dma_start(
                        out=qt[c][c * 64:(c + 1) * 64, :, :],
                        in_=qT_d.rearrange("c p t -> p c t")[c * 64:(c + 1) * 64, :, qb * 512:(qb + 1) * 512]),
                        rd=[R_qT], wr=[qt[c]])
                lb = lruB[qb % 2]
                S.dma(sp, lambda h: h.dma_start(out=lb[:], in_=lru_d.rearrange("c p t -> p c t")[:, :, qb * 512:(qb + 1) * 512]),
                      rd=[R_lru], wr=[lb])
                NP = NT // 2
                items = [(hd, c, kp) for hd in range(4) for c in range(2) for kp in range(NP)]

                def emit_S(i):
                    hd, c, kp = items[i]
                    pb = (i % 2) * 2
                    for u in range(2):
                        kt = kp * 2 + u
                        S.op(pe, lambda h: h.matmul(bank(pb + u), lhsT=kT[:, hd, kt * 128:(kt + 1) * 128],
                                                    rhs=qt[c][:, hd, :], start=True, stop=True),
                             rd=[kT, qt[c]], wr=[bankR[pb + u]], self_sync=False)

                emit_S(0)
                emit_S(1)
                for i, (hd, c, kp) in enumerate(items):
                    os_ = osb[hd % 2]
                    pb = (i % 2) * 2
                    ab = 4 + 2 * ((hd * 2 + c) % 2)
                    E = Eb[i % 3]
                    S.op(act, lambda h: h.activation(out=E[:], in_=PS[:, pb:pb + 2, :].rearrange("p a n -> p (a n)"),
                                                     func=AF.Exp, bias=sc2[:, 1:2]),
                         rd=[bankR[pb], bankR[pb + 1], sc2], wr=[E])
                    if i + 2 < len(items):
                        emit_S(i + 2)
                    for u in range(2):
                        kt = kp * 2 + u
                        for s_ in range(4):
                            bb = ab + s_ // 2
                            co = (s_ % 2) * 256
                            S.op(pe, lambda h: h.matmul(bank(bb)[:, co:co + 129],
                                                        lhsT=E[:, u * 512 + s_ * 128:u * 512 + (s_ + 1) * 128],
                                                        rhs=v1[:, kt, hd * 130:hd * 130 + 129],
                                                        start=(kt == 0 and s_ % 2 == 0), stop=(kt == NT - 1),
                                                        skip_group_check=True),
                                 rd=[E, v1], wr=[bankR[bb]], self_sync=False)
                    gcnt["g"] += 1
                    while deferred and deferred[0][0] <= gcnt["g"]:
                        deferred.pop(0)[1]()
                    if not deferred and i < NP - 1:
                        for _ in range(2):
                            if pending:
                                pending.pop(0)()
                    elif i == NP - 1:
                        while deferred:
                            deferred.pop(0)[1]()
                        while pending:
                            pending.pop(0)()
                    if kp < NP - 1:
                        continue
                    for s_ in range(4):
                        bb = ab + s_ // 2
                        co = (s_ % 2) * 256
                        S.op(dve, lambda h: h.reciprocal(out=rl[:, s_:s_ + 1], in_=bank(bb)[:, co + 128:co + 129]),
                             rd=[bankR[bb]], wr=[rl])
                        if c == 0:
                            S.op(dve, lambda h: h.tensor_scalar_mul(out=os_[:, s_, :], in0=bank(bb)[:, co:co + 128],
                                                                    scalar1=rl[:, s_:s_ + 1]),
                                 rd=[bankR[bb], rl], wr=[os_])
                        else:
                            S.op(dve, lambda h: h.tensor_tensor(out=rl[:, 4 + s_:5 + s_], in0=rl[:, s_:s_ + 1],
                                                                in1=sc2[:, 0:1], op=ALU.mult), rd=[rl, sc2], wr=[rl])
                            S.op(dve, lambda h: h.scalar_tensor_tensor(out=os_[:, s_, :], in0=bank(bb)[:, co:co + 128],
                                                                       scalar=rl[:, 4 + s_:5 + s_], in1=os_[:, s_, :],
                                                                       op0=ALU.mult, op1=ALU.add),
                                 rd=[bankR[bb], rl, os_], wr=[os_])
                    if c == 0:
                        continue
                    def subln(hd=hd, os_=os_):
                        for s_ in range(4):
                            S.op(act, lambda h: h.activation(out=junk3[:, 0:128], in_=os_[:, s_, :], func=AF.Square,
                                                             accum_out=ssn[:, s_:s_ + 1]), rd=[os_], wr=[junk3, ssn])
                        S.op(act, lambda h: h.activation(out=rsn[:, 0:4], in_=ssn[:, 0:4], func=AF.Ln, scale=1.0 / 128,
                                                         bias=epsT[:, 0:1]), rd=[ssn, epsT], wr=[rsn])
                        S.op(act, lambda h: h.activation(out=rsn[:, 0:4], in_=rsn[:, 0:4], func=AF.Exp, scale=-0.5),
                             rd=[rsn], wr=[rsn])
                        for s_ in range(4):
                            S.op(dve, lambda h: h.scalar_tensor_tensor(out=att[:, s_, hd * 128:(hd + 1) * 128], in0=os_[:, s_, :],
                                                                       scalar=rsn[:, s_:s_ + 1], in1=bcs[:, hd * 128:(hd + 1) * 128],
                                                                       op0=ALU.mult, op1=ALU.mult),
                                 rd=[os_, rsn, bcs], wr=[att])
                    deferred.append((gcnt["g"] + 3, subln))
                pending = tail_steps(qb, lb, 6, 7)
            while deferred:
                deferred.pop(0)[1]()
            for st in pending:
                st()
            if dbg:
                S.dma(sp, lambda h: h.dma_start(out=dbg_aff, in_=aff[:].rearrange("p t e -> p (t e)")), rd=[aff])
        p03.close()
        S.barrier()
        if stop_after < 4:
            S.finish()
            return nc

        p45 = top.enter_context(ExitStack())
        NGU = 4
        NB = 11
        wgb = [sb(p45, f"wgb{i}", [128, 8, 256], BF16) for i in range(NGU)]
        wub = [sb(p45, f"wub{i}", [128, 8, 256], BF16) for i in range(NGU)]
        wdb = [sb(p45, f"wdb{i}", [128, NJ, 512], BF16) for i in range(2)]

        def load_gu(e, b):
            i = (e * NB + b) % NGU
            for (dst, srcw) in ((wgb[i], wg_d), (wub[i], wu_d)):
                S.dma(pool, lambda h: h.dma_start(
                    out=dst[:], in_=srcw[e].rearrange("(kc p) n -> p kc n", p=128)[:, :, b * 256:(b + 1) * 256]),
                    wr=[dst])

        def load_d(e, fh):
            dst = wdb[fh]
            S.dma(pool, lambda h: h.dma_start(
                out=dst[:], in_=wd_d[e].rearrange("(j p) f -> p j f", p=128)[:, :, fh * 512:(fh + 1) * 512]),
                wr=[dst])

        if stop_after >= 5:
            for b_ in range(NGU):
                load_gu(0, b_)
            load_d(0, 0)
            load_d(0, 1)

        with ExitStack() as p4:
            lo = sb(p4, "lo", [128, NE])
            hi = sb(p4, "hi", [128, NE])
            mid = sb(p4, "mid", [128, NE])
            ta = sb(p4, "ta", [128, NE])
            cmpb = sb(p4, "cmpb", [128, 32, NE], BF16)
            maskb = sb(p4, "maskb", [128, 32, NE], BF16)
            cum = sb(p4, "cum", [128, 32, NE], BF16)
            posm = sb(p4, "posm", [128, 32, NE])
            TI = sb(p4, "TI", [128, NE, 32, 8], BF16)
            rres = sb(p4, "rres", [128, 32, NE])
            Sb = [sb(p4, f"Sb{i}", [128, 512], BF16) for i in range(4)]
            idxf = sb(p4, "idxf", [128, 4])
            pvs = sb(p4, "pvs", [128, 32])
            S.op(dve, lambda h: h.memset(lo[:], 0.0), wr=[lo])
            S.op(dve, lambda h: h.memset(hi[:], 1.0), wr=[hi])
            S.op(dve, lambda h: h.memset(mid[:], 0.5), wr=[mid])
            for it in range(32):
                S.op(dve, lambda h: h.tensor_tensor(out=cmpb[:], in0=aff[:],
                                                    in1=mid[:].unsqueeze(1).broadcast_to([128, 32, NE]), op=ALU.is_ge),
                     rd=[aff, mid], wr=[cmpb])
                for t in range(32):
                    S.op(pe, lambda h: h.matmul(bank(0)[:, 0:NE], lhsT=ones_b, rhs=cmpb[:, t, :],
                                                start=(t == 0), stop=(t == 31)),
                         rd=[cmpb, cstb], wr=[bankR[0]], self_sync=False)
                S.op(dve, lambda h: h.scalar_tensor_tensor(out=ta[:], in0=bank(0)[:, 0:NE], scalar=float(CAP) - 0.5,
                                                           in1=mid[:], op0=ALU.is_ge, op1=ALU.mult),
                     rd=[bankR[0], mid], wr=[ta])
                S.op(dve, lambda h: h.tensor_tensor(out=lo[:], in0=lo[:], in1=ta[:], op=ALU.max), rd=[lo, ta], wr=[lo])
                S.op(dve, lambda h: h.scalar_tensor_tensor(out=ta[:], in0=bank(0)[:, 0:NE], scalar=float(CAP) - 0.5,
                                                           in1=mid[:], op0=ALU.is_ge, op1=ALU.add),
                     rd=[bankR[0], mid], wr=[ta])
                S.op(dve, lambda h: h.tensor_tensor(out=hi[:], in0=hi[:], in1=ta[:], op=ALU.min), rd=[hi, ta], wr=[hi])
                S.op(dve, lambda h: h.tensor_tensor(out=mid[:], in0=lo[:], in1=hi[:], op=ALU.add), rd=[lo, hi], wr=[mid])
                S.op(dve, lambda h: h.tensor_scalar_mul(out=mid[:], in0=mid[:], scalar1=0.5), rd=[mid], wr=[mid])
            S.op(dve, lambda h: h.tensor_tensor(out=maskb[:], in0=aff[:],
                                                in1=lo[:].unsqueeze(1).broadcast_to([128, 32, NE]), op=ALU.is_ge),
                 rd=[aff, lo], wr=[maskb])
            S.op(dve, lambda h: h.memset(cum[:, 0, :], 0.0), wr=[cum])
            for t in range(1, 32):
                S.op(dve, lambda h: h.tensor_tensor(out=cum[:, t, :], in0=cum[:, t - 1, :], in1=maskb[:, t - 1, :],
                                                    op=ALU.add), rd=[cum, maskb], wr=[cum])
            S.op(pe, lambda h: h.matmul(bank(1), lhsT=ustr_b, rhs=maskb[:].rearrange("p t e -> p (t e)"),
                                        start=True, stop=False), rd=[maskb, cstb], wr=[bankR[1]], self_sync=False)
            S.op(pe, lambda h: h.matmul(bank(1), lhsT=ones_b, rhs=cum[:].rearrange("p t e -> p (t e)"),
                                        start=False, stop=True), rd=[cum, cstb], wr=[bankR[1]], self_sync=False)
            S.op(dve, lambda h: h.scalar_tensor_tensor(out=posm[:].rearrange("p t e -> p (t e)"), in0=bank(1), scalar=1.0,
                                                       in1=maskb[:].rearrange("p t e -> p (t e)"),
                                                       op0=ALU.add, op1=ALU.mult), rd=[bankR[1], maskb], wr=[posm])
            S.op(dve, lambda h: h.tensor_scalar_add(out=posm[:], in0=posm[:], scalar1=-1.0), rd=[posm], wr=[posm])
            S.op(pool, lambda h: h.memset(TI[:], 0.0), wr=[TI])
            S.op(dve, lambda h: h.tensor_copy(out=TI[:, :, :, 0],
                                              in_=cst[:, 897:929].unsqueeze(1).broadcast_to([128, NE, 32])),
                 rd=[cst, TI], wr=[TI])
            S.op(dve, lambda h: h.tensor_copy(out=TI[:, :, :, 1],
                                              in_=cst[:, 896:897].unsqueeze(1).broadcast_to([128, NE, 32])),
                 rd=[cst, TI], wr=[TI])
            affv = aff[:].rearrange("p t e -> p e t")
            rresv = rres[:].rearrange("p t e -> p e t")
            S.op(dve, lambda h: h.tensor_copy(out=TI[:, :, :, 2], in_=affv), rd=[aff, TI], wr=[TI])
            S.op(dve, lambda h: h.tensor_tensor(out=rresv, in0=affv, in1=TI[:, :, :, 2], op=ALU.subtract),
                 rd=[aff, TI], wr=[rres])
            S.op(dve, lambda h: h.tensor_copy(out=TI[:, :, :, 3], in_=rresv), rd=[rres, TI], wr=[TI])
            S.op(dve, lambda h: h.tensor_tensor(out=rresv, in0=rresv, in1=TI[:, :, :, 3], op=ALU.subtract),
                 rd=[rres, TI], wr=[rres])
            S.op(dve, lambda h: h.tensor_copy(out=TI[:, :, :, 4], in_=rresv), rd=[rres, TI], wr=[TI])
            scn = 0
            for e in range(NE):
                b = 2 + e % 2
                for t in range(32):
                    Sx = Sb[scn % 4]
                    scn += 1
                    S.op(dve, lambda h: h.tensor_scalar(out=Sx[:], in0=iota_f, scalar1=posm[:, t, e:e + 1], scalar2=None,
                                                        op0=ALU.is_equal), rd=[cst, posm], wr=[Sx])
                    for sc in range(4):
                        S.op(pe, lambda h: h.matmul(bank(b)[:, sc * 8:sc * 8 + 8], lhsT=Sx[:, sc * 128:(sc + 1) * 128],
                                                    rhs=TI[:, e, t, :], start=(t == 0 and sc == 0), stop=(t == 31),
                                                    skip_group_check=True),
                             rd=[Sx, TI], wr=[bankR[b]], self_sync=False)
                S.op(dve, lambda h: h.tensor_copy(out=pvs[:], in_=bank(b)[:, 0:32]), rd=[bankR[b]], wr=[pvs])
                pv = pvs[:].rearrange("p (s c) -> p s c", c=8)
                S.op(dve, lambda h: h.scalar_tensor_tensor(out=idxf[:], in0=pv[:, :, 0], scalar=128.0, in1=pv[:, :, 1],
                                                           op0=ALU.mult, op1=ALU.add), rd=[pvs], wr=[idxf])
                S.op(dve, lambda h: h.tensor_copy(out=idx_i[:, e, :], in_=idxf[:]), rd=[idxf], wr=[idx_i])
                S.op(dve, lambda h: h.tensor_tensor(out=gsl[:, e, :], in0=pv[:, :, 2], in1=pv[:, :, 3], op=ALU.add),
                     rd=[pvs], wr=[gsl])
                S.op(dve, lambda h: h.tensor_tensor(out=gsl[:, e, :], in0=gsl[:, e, :], in1=pv[:, :, 4], op=ALU.add),
                     rd=[pvs, gsl], wr=[gsl])
            if dbg:
                S.dma(sp, lambda h: h.dma_start(out=dbg_idx, in_=idx_i[:].rearrange("p e s -> p (e s)")), rd=[idx_i])
                S.dma(sp, lambda h: h.dma_start(out=dbg_g, in_=gsl[:].rearrange("p e s -> p (e s)")), rd=[gsl])
        S.barrier()
        if stop_after < 5:
            S.finish()
            return nc

        with ExitStack() as p5:
            xe = [sb(p5, f"xe{i}", [128, 4, D], BF16) for i in range(2)]
            xeT = [sb(p5, f"xeT{i}", [128, 8, 512], BF16) for i in range(2)]
            hT = [sb(p5, f"hTe{i}", [128, NJ, 512], BF16) for i in range(2)]
            old = [sb(p5, f"old{i}", [128, D]) for i in range(4)]
            sg = [sb(p5, f"sg{i}", [128, 512]) for i in range(2)]
            tmp5 = [sb(p5, f"tmp5{i}", [128, 512]) for i in range(2)]
            xe_r = [[Reg() for _ in range(4)] for _ in range(2)]
            R_osc = [Reg() for _ in range(4)]

            def gather_xe(e, scs=range(4)):
                xg = xe[e % 2]
                for sc in scs:
                    S.dma(pool, lambda h: h.indirect_dma_start(
                        out=xg[:, sc, :], out_offset=None, in_=h2_d[:, :],
                        in_offset=bass.IndirectOffsetOnAxis(ap=idx_i[:, e, sc:sc + 1], axis=0)),
                        rd=[R_h2, idx_i], wr=[xg])

            def gather_old(e, scs=range(4)):
                for sc in scs:
                    S.dma(pool, lambda h: h.indirect_dma_start(
                        out=old[sc][:], out_offset=None, in_=out_d[:, :],
                        in_offset=bass.IndirectOffsetOnAxis(ap=idx_i[:, e, sc:sc + 1], axis=0)),
                        rd=[R_out, idx_i], wr=[old[sc]])

            def make_xeT(e):
                xg, xt_ = xe[e % 2], xeT[e % 2]
                for kc in range(8):
                    for sc in range(4):
                        S.op(pe, lambda h: h.transpose(out=bank_bf(0)[:, sc * 128:(sc + 1) * 128],
                                                       in_=xg[:, sc, kc * 128:(kc + 1) * 128], identity=ident_b),
                             rd=[xg, cstb], wr=[bankR[0]], self_sync=False)
                    S.op(act, lambda h: h.copy(out=xt_[:, kc, :], in_=bank_bf(0)[:, 0:512]), rd=[bankR[0]], wr=[xt_])

            def scatter_new(e, scs=range(4)):
                for sc in scs:
                    S.dma(pool, lambda h: h.indirect_dma_start(
                        out=out_d[:, :], out_offset=bass.IndirectOffsetOnAxis(ap=idx_i[:, e, sc:sc + 1], axis=0),
                        in_=old[sc][:], in_offset=None), rd=[old[sc], idx_i], wr=[R_out])

            gather_xe(0)
            gather_old(0)
            make_xeT(0)
            pcnt = 0
            dcnt = 0
            for e in range(NE):
                xt_ = xeT[e % 2]
                hT_ = hT[e % 2]
                for b in range(NB):
                    i = (e * NB + b) % NGU
                    if b < 4 and e + 1 < NE:
                        gather_xe(e + 1, [b])
                    if 4 <= b < 8 and e > 0:
                        scatter_new(e - 1, [b - 4])
                    if b >= 8 and e > 0:
                        gather_old(e, [b - 8])
                    for jj in range(2):
                        j = b * 2 + jj
                        bg = 1 + (pcnt % 2) * 2
                        pcnt += 1
                        for (bk, wt) in ((bg, wgb[i]), (bg + 1, wub[i])):
                            for kc in range(8):
                                S.op(pe, lambda h: h.matmul(bank(bk), lhsT=wt[:, kc, jj * 128:(jj + 1) * 128], rhs=xt_[:, kc, :],
                                                            start=(kc == 0), stop=(kc == 7)),
                                     rd=[wt, xt_], wr=[bankR[bk]], self_sync=False)
                        sg_ = sg[j % 2]
                        S.op(act, lambda h: h.activation(out=sg_[:], in_=bank(bg), func=AF.Silu), rd=[bankR[bg]], wr=[sg_])
                        S.op(dve, lambda h: h.tensor_tensor(out=hT_[:, j, :], in0=sg_[:], in1=bank(bg + 1), op=ALU.mult),
                             rd=[sg_, bankR[bg + 1]], wr=[hT_])
                    nb_ = b + NGU
                    if nb_ < NB:
                        load_gu(e, nb_)
                    elif e + 1 < NE:
                        load_gu(e + 1, nb_ - NB)
                if e > 0:
                    gather_old(e, [3])
                if e + 1 < NE:
                    make_xeT(e + 1)
                for fh in range(2):
                    wd_ = wdb[fh]
                    for sc in range(4):
                        bk = 5 + dcnt % 3
                        dcnt += 1
                        for j in range(NJ):
                            S.op(pe, lambda h: h.matmul(bank(bk), lhsT=hT_[:, j, sc * 128:(sc + 1) * 128], rhs=wd_[:, j, :],
                                                        start=(j == 0), stop=(j == NJ - 1)),
                                 rd=[hT_, wd_], wr=[bankR[bk]], self_sync=False)
                        t5 = tmp5[sc % 2]
                        S.op(dve, lambda h: h.scalar_tensor_tensor(out=t5[:], in0=bank(bk), scalar=gsl[:, e, sc:sc + 1],
                                                                   in1=bcG[:, 3, fh * 512:(fh + 1) * 512],
                                                                   op0=ALU.mult, op1=ALU.mult),
                             rd=[bankR[bk], gsl, bcG], wr=[t5])
                        S.op(dve, lambda h: h.tensor_tensor(out=old[sc][:, fh * 512:(fh + 1) * 512],
                                                             in0=old[sc][:, fh * 512:(fh + 1) * 512], in1=t5[:], op=ALU.add),
                             rd=[old[sc], t5], wr=[old[sc]])
                    if e + 1 < NE:
                        load_d(e + 1, fh)
            scatter_new(NE - 1)
        S.finish()
    return nc


def _consts():
    c = np.zeros((128, NCONST), np.float32)
    c[:, 0:128] = np.eye(128, dtype=np.float32)
    p = np.arange(128)
    c[:, 128:256] = (p[:, None] < p[None, :]).astype(np.float32)
    c[:, 256:384] = 1.0
    c[:, 384:896] = np.arange(512, dtype=np.float32)[None, :]
    c[:, 896] = p.astype(np.float32)
    c[:, 897:929] = np.arange(32, dtype=np.float32)[None, :]
    return c


def _rope_table16():
    tab = np.zeros((128, NT, 2, 64), np.float32)
    tab[:, :, 0, :] = 1.0
    inv = (np.float32(10000.0) ** (-np.arange(16, dtype=np.float32) / np.float32(16))).astype(np.float32)
    for T in range(2, NT):
        tok = (T - 2) * 128 + np.arange(128)
        row = (tok // 64).astype(np.float32)
        col = (tok % 64).astype(np.float32)
        ar = (row[:, None] * inv[None, :]).astype(np.float32)
        ac = (col[:, None] * inv[None, :]).astype(np.float32)
        cr, sr, cc, sc_ = np.cos(ar), np.sin(ar), np.cos(ac), np.sin(ac)
        tab[:, T, 0, 0:16] = cr
        tab[:, T, 0, 16:32] = cr
        tab[:, T, 0, 32:48] = cc
        tab[:, T, 0, 48:64] = cc
        tab[:, T, 1, 0:16] = -sr
        tab[:, T, 1, 16:32] = sr
        tab[:, T, 1, 32:48] = -sc_
        tab[:, T, 1, 48:64] = sc_
    return tab.reshape(128, NT * 128)


def make_in_maps(inp, cores):
    f = lambda a: np.ascontiguousarray(np.asarray(a, dtype=np.float32))
    L = 0
    c_ctx = f(inp["c_ctx"])
    conv_w = f(inp["conv_w"][L])
    convw = np.zeros((128, 16), np.float32)
    for j in range(4):
        for k in range(4):
            convw[:, j * 4 + k] = conv_w[k, j * 128:(j + 1) * 128]
    convb = f(inp["conv_b"][L]).reshape(4, 128).T
    lruw = np.zeros((128, 2, 2, 4, 128), np.float32)
    lrub = np.zeros((128, 2, 2, 4), np.float32)
    for d in range(2):
        for gi, (wn, bn) in enumerate((("lru_wa", "lru_ba"), ("lru_wi", "lru_bi"))):
            w = f(inp[wn][L][d])
            bb = f(inp[bn][L][d])
            for j in range(4):
                for hh in range(2):
                    lruw[hh * 64:(hh + 1) * 64, d, gi, j, hh * 64:(hh + 1) * 64] = w[2 * j + hh]
                    lrub[hh * 64:(hh + 1) * 64, d, gi, j] = bb[2 * j + hh]
    lrul = np.zeros((128, 2, 4), np.float32)
    lam = f(inp["lru_lambda"][L])
    for d in range(2):
        lrul[:, d, :] = lam[d].reshape(4, 128).T
    shared = {
        "w_ada": f(inp["w_ada"][L]),
        "b_ada": f(inp["b_ada"][L]).reshape(1, -1),
        "normg": np.concatenate([f(inp["norm1_g"][L]), f(inp["norm2_g"][L])]).reshape(1, -1),
        "qkg": np.concatenate([np.tile(f(inp["q_norm_g"][L]), 8), np.tile(f(inp["k_norm_g"][L]), 8)]).reshape(1, -1),
        "lamv": np.concatenate([f(inp["lambda_q1"][L]), f(inp["lambda_k1"][L]),
                                f(inp["lambda_q2"][L]), f(inp["lambda_k2"][L])]).reshape(1, -1),
        "subg": np.tile(f(inp["subln_g"][L]), 4).reshape(1, -1),
        "w_in": f(inp["w_in"][L]),
        "convw": convw,
        "convb": np.ascontiguousarray(convb),
        "lruw": np.ascontiguousarray(lruw.reshape(128, 2048)),
        "lrub": np.ascontiguousarray(lrub.reshape(128, 16)),
        "lrul": np.ascontiguousarray(lrul.reshape(128, 8)),
        "w_out": f(inp["w_out"][L]),
        "w_router": f(inp["w_router"][L]),
        "w_gate": f(inp["w_gate"][L]),
        "w_up": f(inp["w_up"][L]),
        "w_down": f(inp["w_down"][L]),
        "rope": _rope_table16(),
        "consts": _consts(),
    }
    maps = []
    for b in cores:
        cvec = np.zeros((128, 16), np.float32)
        cvec[:, 0:8] = f(inp["c"][b]).reshape(8, 128).T
        cvec[:, 8:16] = c_ctx.reshape(8, 128).T
        m = dict(shared)
        m["x"] = f(inp["x"][b])
        m["ctx"] = f(inp["ctx"][b])
        m["cvec"] = cvec
        maps.append(m)
    return maps


def kernel(**inputs):
    nc = build()
    maps = make_in_maps(inputs, list(range(8)))
    res = run_bass_kernel_spmd(nc, maps, core_ids=list(range(8)))
    return np.stack([np.asarray(r["out"], dtype=np.float32) for r in res.results], axis=0)
```

```python
import math
from contextlib import ExitStack

import numpy as np
import concourse.bass as bass
import concourse.mybir as mybir
from concourse.bass_utils import run_bass_kernel_spmd

F32 = mybir.dt.float32
BF16 = mybir.dt.bfloat16
I32 = mybir.dt.int32
AF = mybir.ActivationFunctionType
ALU = mybir.AluOpType
AX = mybir.AxisListType

D = 1024
SEQ = 4096
CTX = 256
NT = 34
NE = 16
CAP = 512
DEXP = 2816
NJ = 22
EPS = 1e-6
LAM_INIT = 0.2
NCONST = 936


class Reg:
    __slots__ = ("w", "rs")

    def __init__(self):
        self.w = {}
        self.rs = {}


class Tile:
    def __init__(self, t):
        self.t = t
        self.r = Reg()

    def __getitem__(self, k):
        return self.t[k]


class Eng:
    def __init__(self, h, key, dkeys):
        self.h = h
        self.key = key
        self.n = 0
        self.seen = {}
        self.dkeys = dkeys
        self.dvals = [0] * len(dkeys)
        self.di = 0


def _reg(x):
    return x.r if isinstance(x, Tile) else x


class Sched:
    def __init__(self, nc, stack):
        self.nc = nc
        self.sems = []

        def mk(name):
            s = stack.enter_context(nc.semaphore(name))
            self.sems.append(s)
            return len(self.sems) - 1

        self.pe = Eng(nc.tensor, mk("s_pe"), [])
        self.act = Eng(nc.scalar, mk("s_act"), [])
        self.dve = Eng(nc.vector, mk("s_dve"), [])
        self.pool = Eng(nc.gpsimd, mk("s_pool"), [mk(f"d_pool{i}") for i in range(12)])
        self.sp = Eng(nc.sync, mk("s_sp"), [mk(f"d_sp{i}") for i in range(12)])
        self.engs = [self.pe, self.act, self.dve, self.pool, self.sp]

    def _deps(self, rd, wr):
        deps = {}

        def add(k, v):
            if deps.get(k, 0) < v:
                deps[k] = v

        for r in rd:
            r = _reg(r)
            for k, v in r.w.items():
                add(k, v)
        for r in wr:
            r = _reg(r)
            for k, v in r.w.items():
                add(k, v)
            for k, v in r.rs.items():
                add(k, v)
        return deps

    def _wait(self, e, deps, self_sync):
        for k, v in deps.items():
            if k == e.key and not self_sync:
                continue
            if e.seen.get(k, 0) >= v:
                continue
            e.h.wait_ge(self.sems[k], v)
            e.seen[k] = v

    def _mark(self, ev, rd, wr):
        k, v = ev
        for r in rd:
            r = _reg(r)
            if r.rs.get(k, 0) < v:
                r.rs[k] = v
        for r in wr:
            r = _reg(r)
            r.w[k] = v
            r.rs = {}

    def op(self, e, fn, rd=(), wr=(), self_sync=True):
        self._wait(e, self._deps(rd, wr), self_sync)
        ins = fn(e.h)
        e.n += 1
        ins.then_inc(self.sems[e.key], 1)
        self._mark((e.key, e.n), rd, wr)
        return ins

    def dma(self, q, fn, rd=(), wr=()):
        self._wait(q, self._deps(rd, wr), True)
        slot = q.di % len(q.dkeys)
        q.di += 1
        k = q.dkeys[slot]
        prev = q.dvals[slot]
        if q.seen.get(k, 0) < prev:
            q.h.wait_ge(self.sems[k], prev)
            q.seen[k] = prev
        ins = fn(q.h)
        q.dvals[slot] = prev + 16
        ins.then_inc(self.sems[k], 16)
        self._mark((k, prev + 16), rd, wr)
        return ins

    def barrier(self):
        for e in self.engs:
            for o in self.engs:
                if o.n > 0 and e.seen.get(o.key, 0) < o.n and o is not e:
                    e.h.wait_ge(self.sems[o.key], o.n)
                    e.seen[o.key] = o.n
                for k, v in zip(o.dkeys, o.dvals):
                    if v > 0 and e.seen.get(k, 0) < v:
                        e.h.wait_ge(self.sems[k], v)
                        e.seen[k] = v

    def finish(self):
        q = self.sp
        for e in self.engs:
            for k, v in zip(e.dkeys, e.dvals):
                if v > 0 and q.seen.get(k, 0) < v:
                    q.h.wait_ge(self.sems[k], v)
                    q.seen[k] = v
        for e in self.engs:
            if e is not q and e.n > 0:
                q.h.wait_ge(self.sems[e.key], e.n)


def build(dbg=False, stop_after=99):
    nc = bass.Bass("TRN2", target_bir_lowering=False)
    okind = "ExternalOutput" if dbg else "Internal"

    def din(name, shape, dt=F32):
        return nc.dram_tensor(name, list(shape), dt, kind="ExternalInput").ap()

    x_d = din("x", [SEQ, D])
    ctx_d = din("ctx", [CTX, D])
    cvec_d = din("cvec", [128, 16])
    wada_d = din("w_ada", [D, 6 * D])
    bada_d = din("b_ada", [1, 6 * D])
    normg_d = din("normg", [1, 2 * D])
    qkg_d = din("qkg", [1, 1024])
    lamv_d = din("lamv", [1, 256])
    subg_d = din("subg", [1, 512])
    win_d = din("w_in", [D, 2560])
    convw_d = din("convw", [128, 16])
    convb_d = din("convb", [128, 4])
    lruw_d = din("lruw", [128, 2048])
    lrub_d = din("lrub", [128, 16])
    lrul_d = din("lrul", [128, 8])
    wout_d = din("w_out", [D, D])
    wr_d = din("w_router", [D, NE])
    big = stop_after >= 5
    wg_d = din("w_gate", [NE, D, DEXP] if big else [1, 8, 8])
    wu_d = din("w_up", [NE, D, DEXP] if big else [1, 8, 8])
    wd_d = din("w_down", [NE, DEXP, D] if big else [1, 8, 8])
    rope_d = din("rope", [128, NT * 128])
    consts_d = din("consts", [128, NCONST])
    out_d = nc.dram_tensor("out", [SEQ, D], F32, kind="ExternalOutput").ap()

    xl_d = nc.dram_tensor("xl_s", [4, 128, CTX + SEQ], F32, kind=okind).ap()
    gel_d = nc.dram_tensor("gel_s", [4, 128, SEQ], BF16, kind=okind).ap()
    qT_d = nc.dram_tensor("qT_s", [4, 128, SEQ], BF16, kind=okind).ap()
    kT_d = nc.dram_tensor("kT_s", [4, 128, CTX + SEQ], BF16, kind=okind).ap()
    v_d = nc.dram_tensor("v_s", [NT, 128, 520], BF16, kind=okind).ap()
    h2_d = nc.dram_tensor("h2_s", [SEQ, D], BF16, kind=okind).ap()
    lru_d = nc.dram_tensor("lru_s", [4, 128, SEQ], BF16, kind=okind).ap()
    if dbg:
        dbg_mod = nc.dram_tensor("dbg_mod", [1, 8192], F32, kind="ExternalOutput").ap()
        dbg_aff = nc.dram_tensor("dbg_aff", [128, 32 * NE], F32, kind="ExternalOutput").ap()
        dbg_idx = nc.dram_tensor("dbg_idx", [128, NE * 4], I32, kind="ExternalOutput").ap()
        dbg_g = nc.dram_tensor("dbg_g", [128, NE * 4], F32, kind="ExternalOutput").ap()

    R_xl = [Reg() for _ in range(4)]
    R_gel, R_qT, R_kT, R_v, R_h2, R_out, R_lru = Reg(), Reg(), Reg(), Reg(), Reg(), Reg(), Reg()

    with ExitStack() as top:
        S = Sched(nc, top)
        pe, act, dve, pool, sp = S.pe, S.act, S.dve, S.pool, S.sp

        def sb(stack, name, shape, dt=F32):
            return Tile(stack.enter_context(nc.sbuf_tensor("t_" + name, list(shape), dt)))

        PS = top.enter_context(nc.psum_tensor("PS", [128, 8, 512], F32))
        bankR = [Reg() for _ in range(8)]

        def bank(b):
            return PS[:, b, :]

        def bank_bf(b):
            return PS[:, b, :].bitcast(BF16)

        cst = sb(top, "cst", [128, NCONST])
        cstb = sb(top, "cstb", [128, 384], BF16)
        bcG = sb(top, "bcG", [128, 4, D])
        aff = sb(top, "aff", [128, 32, NE])
        idx_i = sb(top, "idx_i", [128, NE, 4], I32)
        gsl = sb(top, "gsl", [128, NE, 4])
        A1, B1, A1C, B1C, G1, A2, B2, G2 = range(8)
        sc2 = sb(top, "sc2", [128, 2])
        epsT = sb(top, "epsT", [128, 1])
        S.dma(sp, lambda h: h.dma_start(out=cst[:], in_=consts_d), wr=[cst])
        S.op(dve, lambda h: h.tensor_copy(out=cstb[:], in_=cst[:, 0:384]), rd=[cst], wr=[cstb])
        S.op(dve, lambda h: h.memset(epsT[:], EPS), wr=[epsT])
        ident_f = cst[:, 0:128]
        ones_f = cst[:, 256:384]
        iota_f = cst[:, 384:896]
        ident_b = cstb[:, 0:128]
        ustr_b = cstb[:, 128:256]
        ones_b = cstb[:, 256:384]

        def rsqrt_ops(dst, src, scale, n, lo=0):
            S.op(act, lambda h: h.activation(out=dst[:, lo:n], in_=src[:, lo:n], func=AF.Sqrt,
                                             scale=scale, bias=epsT[:, 0:1]),
                 rd=[src, epsT], wr=[dst])
            S.op(dve, lambda h: h.reciprocal(out=dst[:, lo:n], in_=dst[:, lo:n]), rd=[dst], wr=[dst])

        p03 = top.enter_context(ExitStack())
        bcs = sb(p03, "bcs", [128, 512])
        p01 = p03.enter_context(ExitStack())
        bcq = sb(p01, "bcq", [128, 1024])
        bcA = sb(p01, "bcA", [128, 4, D])

        def bcr(i):
            return (bcA, i) if i < 4 else (bcG, i - 4)

        winb = sb(p01, "winb", [128, 8, 2560], BF16)
        ropeT = sb(p01, "ropeT", [128, NT, 2, 64])
        for kc in range(8):
            S.dma(pool, lambda h: h.dma_start(
                out=winb[:, kc, :].rearrange("p (a n) -> p a n", n=640),
                in_=win_d[kc * 128:(kc + 1) * 128, :].rearrange("p (a n) -> p a n", n=640)), wr=[winb])
        S.dma(sp, lambda h: h.dma_start(out=ropeT[:].rearrange("p t c d -> p (t c d)"), in_=rope_d), wr=[ropeT])

        with ExitStack() as p0:
            cv = sb(p0, "cv", [128, 16])
            scv = sb(p0, "scv", [128, 16])
            modrow = sb(p0, "modrow", [1, 8192])
            bada = sb(p0, "bada", [1, 6 * D])
            normg = sb(p0, "normg", [1, 2 * D])
            rowt = sb(p0, "rowt", [1, 3, D])
            qkg = sb(p0, "qkg", [1, 1024])
            lamv = sb(p0, "lamv", [1, 256])
            subg = sb(p0, "subg", [1, 512])
            lt = sb(p0, "lt", [1, 16])
            wb = [sb(p0, f"wadab{i}", [128, 8, 256]) for i in range(2)]
            S.dma(sp, lambda h: h.dma_start(out=cv[:], in_=cvec_d), wr=[cv])
            S.dma(sp, lambda h: h.dma_start(out=bada[:], in_=bada_d), wr=[bada])
            S.dma(sp, lambda h: h.dma_start(out=normg[:], in_=normg_d), wr=[normg])
            S.dma(sp, lambda h: h.dma_start(out=qkg[:], in_=qkg_d), wr=[qkg])
            S.dma(sp, lambda h: h.dma_start(out=lamv[:], in_=lamv_d), wr=[lamv])
            S.dma(sp, lambda h: h.dma_start(out=subg[:], in_=subg_d), wr=[subg])
            S.op(act, lambda h: h.activation(out=scv[:], in_=cv[:], func=AF.Silu), rd=[cv], wr=[scv])
            wada_v = wada_d.rearrange("(kc p) n -> p kc n", p=128)
            CW = 256
            for nb in range(6 * D // CW):
                w = wb[nb % 2]
                S.dma(sp, lambda h: h.dma_start(out=w[:], in_=wada_v[:, :, nb * CW:(nb + 1) * CW]), wr=[w])
                for kc in range(8):
                    S.op(pe, lambda h: h.matmul(bank(0)[0:1, 0:CW], lhsT=scv[:, kc:kc + 1], rhs=w[:, kc, :],
                                                start=(kc == 0), stop=(kc == 7)),
                         rd=[scv, w], wr=[bankR[0]], self_sync=False)
                S.op(dve, lambda h: h.tensor_tensor(out=modrow[0:1, nb * CW:(nb + 1) * CW], in0=bank(0)[0:1, 0:CW],
                                                    in1=bada[0:1, nb * CW:(nb + 1) * CW], op=ALU.add),
                     rd=[bankR[0], bada], wr=[modrow])
                if nb < 2 * D // CW:
                    for kc in range(8):
                        S.op(pe, lambda h: h.matmul(bank(1)[0:1, 0:CW], lhsT=scv[:, 8 + kc:9 + kc], rhs=w[:, kc, :],
                                                    start=(kc == 0), stop=(kc == 7)),
                             rd=[scv, w], wr=[bankR[1]], self_sync=False)
                    S.op(dve, lambda h: h.tensor_tensor(out=modrow[0:1, 6144 + nb * CW:6144 + (nb + 1) * CW],
                                                        in0=bank(1)[0:1, 0:CW], in1=bada[0:1, nb * CW:(nb + 1) * CW],
                                                        op=ALU.add),
                         rd=[bankR[1], bada], wr=[modrow])
            if dbg:
                S.dma(sp, lambda h: h.dma_start(out=dbg_mod, in_=modrow[:]), rd=[modrow])

            def mrow(i):
                return modrow[0:1, i * D:(i + 1) * D]

            for ti, (si, go) in enumerate([(1, 0), (7, 0), (4, D)]):
                S.op(dve, lambda h: h.scalar_tensor_tensor(out=rowt[0:1, ti, :], in0=mrow(si), scalar=1.0,
                                                           in1=normg[0:1, go:go + D], op0=ALU.add, op1=ALU.mult),
                     rd=[modrow, normg], wr=[rowt])
            rows = {A1: rowt[0:1, 0, :], B1: mrow(0), A1C: rowt[0:1, 1, :], B1C: mrow(6), G1: mrow(2),
                    A2: rowt[0:1, 2, :], B2: mrow(3), G2: mrow(5)}
            cnt = 0
            for bi, row in rows.items():
                for hf in range(2):
                    b = 2 + cnt % 2
                    cnt += 1
                    S.op(pe, lambda h: h.matmul(bank(b), lhsT=ones_f[0:1, :], rhs=row[0:1, hf * 512:(hf + 1) * 512],
                                                start=True, stop=True),
                         rd=[cst, modrow, rowt], wr=[bankR[b]], self_sync=False)
                    bt_, bi_ = bcr(bi)
                    S.op(act, lambda h: h.copy(out=bt_[:, bi_, hf * 512:(hf + 1) * 512], in_=bank(b)),
                         rd=[bankR[b]], wr=[bt_])
            for hf in range(2):
                b = 2 + hf
                S.op(pe, lambda h: h.matmul(bank(b), lhsT=ones_f[0:1, :], rhs=qkg[0:1, hf * 512:(hf + 1) * 512],
                                            start=True, stop=True), rd=[cst, qkg], wr=[bankR[b]], self_sync=False)
                S.op(act, lambda h: h.mul(out=bcq[:, hf * 512:(hf + 1) * 512], in_=bank(b),
                                          mul=(0.125 if hf == 0 else 1.0)), rd=[bankR[b]], wr=[bcq])
            S.op(pe, lambda h: h.matmul(bank(2), lhsT=ones_f[0:1, :], rhs=subg[0:1, :], start=True, stop=True),
                 rd=[cst, subg], wr=[bankR[2]], self_sync=False)
            S.op(act, lambda h: h.mul(out=bcs[:], in_=bank(2), mul=1.0 - LAM_INIT), rd=[bankR[2]], wr=[bcs])
            S.op(dve, lambda h: h.tensor_tensor(out=lamv[0:1, 0:64], in0=lamv[0:1, 0:64], in1=lamv[0:1, 64:128],
                                                op=ALU.mult), rd=[lamv], wr=[lamv])
            S.op(dve, lambda h: h.tensor_tensor(out=lamv[0:1, 128:192], in0=lamv[0:1, 128:192],
                                                in1=lamv[0:1, 192:256], op=ALU.mult), rd=[lamv], wr=[lamv])
            S.op(dve, lambda h: h.reduce_sum(out=lt[0:1, 0:1], in_=lamv[0:1, 0:64], axis=AX.X), rd=[lamv], wr=[lt])
            S.op(dve, lambda h: h.reduce_sum(out=lt[0:1, 1:2], in_=lamv[0:1, 128:192], axis=AX.X), rd=[lamv], wr=[lt])
            S.op(act, lambda h: h.activation(out=lt[0:1, 2:4], in_=lt[0:1, 0:2], func=AF.Exp), rd=[lt], wr=[lt])
            S.op(dve, lambda h: h.tensor_tensor(out=lt[0:1, 4:5], in0=lt[0:1, 3:4], in1=lt[0:1, 2:3],
                                                op=ALU.subtract), rd=[lt], wr=[lt])
            S.op(dve, lambda h: h.tensor_scalar_add(out=lt[0:1, 4:5], in0=lt[0:1, 4:5], scalar1=-LAM_INIT),
                 rd=[lt], wr=[lt])
            S.op(dve, lambda h: h.reduce_max(out=lt[0:1, 6:7], in_=qkg[0:1, 0:64], axis=AX.X,
                                             apply_absolute_value=True), rd=[qkg], wr=[lt])
            S.op(dve, lambda h: h.reduce_max(out=lt[0:1, 7:8], in_=qkg[0:1, 512:576], axis=AX.X,
                                             apply_absolute_value=True), rd=[qkg], wr=[lt])
            S.op(dve, lambda h: h.tensor_tensor(out=lt[0:1, 5:6], in0=lt[0:1, 6:7], in1=lt[0:1, 7:8], op=ALU.mult),
                 rd=[lt], wr=[lt])
            S.op(dve, lambda h: h.tensor_scalar_mul(out=lt[0:1, 5:6], in0=lt[0:1, 5:6], scalar1=-8.0),
                 rd=[lt], wr=[lt])
            S.op(pe, lambda h: h.matmul(bank(3)[:, 0:2], lhsT=ones_f[0:1, :], rhs=lt[0:1, 4:6], start=True, stop=True),
                 rd=[cst, lt], wr=[bankR[3]], self_sync=False)
            S.op(act, lambda h: h.copy(out=sc2[:], in_=bank(3)[:, 0:2]), rd=[bankR[3]], wr=[sc2])
        S.barrier()
        if stop_after < 1:
            S.finish()
            return nc

        with ExitStack() as p1:
            hlT = [sb(p1, f"hlT{i}", [128, 8, 512], BF16) for i in range(2)]
            xb = [sb(p1, f"xb{i}", [128, D]) for i in range(4)]
            junk = sb(p1, "junk", [128, D], BF16)
            ss4 = [sb(p1, f"ss4{i}", [128, 4]) for i in range(2)]
            rs4 = [sb(p1, f"rs4{i}", [128, 4]) for i in range(2)]
            t1 = sb(p1, "t1_0", [128, D])
            hl = [sb(p1, f"hl{i}", [128, D], BF16) for i in range(2)]
            xlst = [sb(p1, f"xlst{i}", [128, 512]) for i in range(2)]
            gst = sb(p1, "gst0", [128, 4, 512], BF16)
            vst = [sb(p1, f"vst{i}", [128, 4, 4, 130], BF16) for i in range(2)]
            sq = [sb(p1, f"sq{i}", [128, D]) for i in range(2)]
            ss16 = [sb(p1, f"ss16{i}", [128, 16]) for i in range(2)]
            rs16 = [sb(p1, f"rs16{i}", [128, 16]) for i in range(2)]
            tq = [sb(p1, f"tq{i}", [128, D]) for i in range(2)]
            r1 = [sb(p1, f"r1{i}", [128, D]) for i in range(2)]
            qkr = [sb(p1, f"qkr{i}", [128, D], BF16) for i in range(2)]
            qkst = [sb(p1, f"qkst{i}", [128, 8, 512], BF16) for i in range(2)]
            for v in vst:
                S.op(pool, lambda h: h.memset(v[:], 1.0), wr=[v])

            def blk_tiles(blk):
                return [0, 1] if blk == 0 else [2 + 4 * (blk - 1) + i for i in range(4)]

            cnts = {"x": 0, "f": 0, "t": 0}

            hl_state = {}

            def hl_chain(blk, i):
                xts, r4 = hl_state[blk]
                ai, bi = (A1C, B1C) if blk == 0 else (A1, B1)
                xt, hh = xts[i], hl[i % 2]
                S.op(dve, lambda h: h.scalar_tensor_tensor(out=t1[:], in0=xt[:], scalar=r4[:, i:i + 1],
                                                           in1=bcA[:, ai, :], op0=ALU.mult, op1=ALU.mult),
                     rd=[xt, r4, bcA], wr=[t1])
                S.op(dve, lambda h: h.tensor_tensor(out=hh[:], in0=t1[:], in1=bcA[:, bi, :], op=ALU.add),
                     rd=[t1, bcA], wr=[hh])

            def hl_T(blk, i):
                hT, hh = hlT[blk % 2], hl[i % 2]
                for kc in range(8):
                    S.op(pe, lambda h: h.transpose(out=bank_bf(0)[:, kc * 128:(kc + 1) * 128],
                                                   in_=hh[:, kc * 128:(kc + 1) * 128], identity=ident_b),
                         rd=[hh, cstb], wr=[bankR[0]], self_sync=False)
                S.op(act, lambda h: h.copy(out=hT[:, :, i * 128:(i + 1) * 128],
                                           in_=bank_bf(0).rearrange("p (k t) -> p k t", t=128)),
                     rd=[bankR[0]], wr=[hT])

            def hl_front(blk):
                tiles = blk_tiles(blk)
                nt = len(tiles)
                s4, r4 = ss4[blk % 2], rs4[blk % 2]
                xts = []
                for i, T in enumerate(tiles):
                    xt = xb[cnts["x"] % 4]
                    cnts["x"] += 1
                    src = ctx_d[T * 128:(T + 1) * 128, :] if T < 2 else x_d[(T - 2) * 128:(T - 1) * 128, :]
                    S.dma(sp, lambda h: h.dma_start(out=xt[:], in_=src), wr=[xt])
                    S.op(act, lambda h: h.activation(out=junk[:], in_=xt[:], func=AF.Square,
                                                     accum_out=s4[:, i:i + 1]), rd=[xt], wr=[junk, s4])
                    xts.append(xt)
                rsqrt_ops(r4, s4, 1.0 / D, nt)
                hl_state[blk] = (xts, r4)
                for i in range(min(2, nt)):
                    hl_chain(blk, i)

            def hl_back(blk):
                nt = len(blk_tiles(blk))
                for i in range(nt):
                    if i >= 2:
                        hl_chain(blk, i)
                    hl_T(blk, i)

            def hlstage(blk):
                hl_front(blk)
                hl_back(blk)

            def fmstage(blk):
                tiles = blk_tiles(blk)
                W = 128 * len(tiles)
                toff = 0 if blk == 0 else CTX + (blk - 1) * 512
                loff = (blk - 1) * 512
                hT = hlT[blk % 2]
                for oc in range(4 if blk == 0 else 8):
                    b = 2 + cnts["f"] % 2
                    cnts["f"] += 1
                    for kc in range(8):
                        S.op(pe, lambda h: h.matmul(bank(b)[:, 0:W], lhsT=winb[:, kc, oc * 128:(oc + 1) * 128],
                                                    rhs=hT[:, kc, 0:W], start=(kc == 0), stop=(kc == 7)),
                             rd=[winb, hT], wr=[bankR[b]], self_sync=False)
                    if oc < 4:
                        st = xlst[oc % 2]
                        S.op(dve, lambda h: h.tensor_copy(out=st[:, 0:W], in_=bank(b)[:, 0:W]),
                             rd=[bankR[b]], wr=[st])
                        S.dma(sp, lambda h: h.dma_start(out=xl_d[oc, :, toff:toff + W], in_=st[:, 0:W]),
                              rd=[st], wr=[R_xl[oc]])
                    else:
                        S.op(act, lambda h: h.activation(out=gst[:, oc - 4, :], in_=bank(b), func=AF.Gelu_apprx_tanh),
                             rd=[bankR[b]], wr=[gst])
                if blk > 0:
                    S.dma(sp, lambda h: h.dma_start(out=gel_d.rearrange("c p t -> p c t")[:, :, loff:loff + 512],
                                                    in_=gst[:]), rd=[gst], wr=[R_gel])

            def stage_a(blk, i):
                T = blk_tiles(blk)[i]
                hT = hlT[blk % 2]
                vs = vst[blk % 2]
                g0 = 8 if blk == 0 else 0
                c0 = g0 * 64
                u = cnts["t"] % 2
                qb_ = 4 + 2 * u
                for (b, col) in ([(qb_, 1024)] if blk > 0 else []) + [(qb_ + 1, 1536), (1, 2048)]:
                    for kc in range(8):
                        S.op(pe, lambda h: h.matmul(bank(b), lhsT=hT[:, kc, i * 128:(i + 1) * 128],
                                                    rhs=winb[:, kc, col:col + 512], start=(kc == 0), stop=(kc == 7)),
                             rd=[winb, hT], wr=[bankR[b]], self_sync=False)
                S.op(act, lambda h: h.copy(out=vs[:, i, :, 0:128],
                                           in_=bank(1).rearrange("p (a e) -> p a e", e=128)),
                     rd=[bankR[1]], wr=[vs])
                pqk = PS[:, qb_:qb_ + 2, :].rearrange("p a n -> p (a n)")
                S.op(act, lambda h: h.activation(out=sq[u][:, c0:], in_=pqk[:, c0:], func=AF.Square),
                     rd=[bankR[qb_], bankR[qb_ + 1]], wr=[sq[u]])
                cnts["t"] += 1
                return u

            def stage_a2(blk, i, u):
                g0 = 8 if blk == 0 else 0
                c0 = g0 * 64
                qb_ = 4 + 2 * u
                pqk = PS[:, qb_:qb_ + 2, :].rearrange("p a n -> p (a n)")
                S.op(dve, lambda h: h.tensor_reduce(out=ss16[u][:, g0:], in_=sq[u][:, c0:].rearrange("p (g d) -> p g d", d=64),
                                                    axis=AX.X, op=ALU.add), rd=[sq[u]], wr=[ss16[u]])
                rsqrt_ops(rs16[u], ss16[u], 1.0 / 64, 16, g0)
                S.op(dve, lambda h: h.tensor_tensor(
                    out=tq[u][:, c0:].rearrange("p (g d) -> p g d", d=64),
                    in0=pqk[:, c0:].rearrange("p (g d) -> p g d", d=64),
                    in1=rs16[u][:, g0:].unsqueeze(2).broadcast_to([128, 16 - g0, 64]), op=ALU.mult),
                    rd=[bankR[qb_], bankR[qb_ + 1], rs16[u]], wr=[tq[u]])
                S.op(dve, lambda h: h.tensor_tensor(out=tq[u][:, c0:], in0=tq[u][:, c0:], in1=bcq[:, c0:], op=ALU.mult),
                     rd=[tq[u], bcq], wr=[tq[u]])

            def stage_b(blk, i, u):
                T = blk_tiles(blk)[i]
                qs = qkst[blk % 2]
                g0 = 8 if blk == 0 else 0
                c0 = g0 * 64
                ng = 16 - g0
                r2 = sq[u]
                S.op(pool, lambda h: h.tensor_tensor(
                    out=r1[u][:, c0:].rearrange("p (g d) -> p g d", d=64),
                    in0=tq[u][:, c0:].rearrange("p (g d) -> p g d", d=64),
                    in1=ropeT[:, T, 0, :].unsqueeze(1).broadcast_to([128, ng, 64]), op=ALU.mult),
                    rd=[tq[u], ropeT], wr=[r1[u]])
                tq5 = tq[u][:, c0:].rearrange("p (g t h w) -> p g t h w", t=2, h=2, w=16)
                r25 = r2[:, c0:].rearrange("p (g t h w) -> p g t h w", t=2, h=2, w=16)
                sn4 = ropeT[:, T, 1, :].rearrange("p (t h w) -> p t h w", t=2, h=2)
                for hv in range(2):
                    S.op(pool, lambda h: h.tensor_tensor(
                        out=r25[:, :, :, hv, :], in0=tq5[:, :, :, 1 - hv, :],
                        in1=sn4[:, :, hv, :].unsqueeze(1).broadcast_to([128, ng, 2, 16]), op=ALU.mult),
                        rd=[tq[u], ropeT], wr=[r2])

            def stage_b2(blk, i, u):
                qs = qkst[blk % 2]
                g0 = 8 if blk == 0 else 0
                c0 = g0 * 64
                r2 = sq[u]
                qq = qkr[u]
                S.op(pool, lambda h: h.tensor_tensor(out=qq[:, c0:], in0=r1[u][:, c0:], in1=r2[:, c0:], op=ALU.add),
                     rd=[r1[u], r2], wr=[qq])
                k0 = g0 // 2
                for kc in range(k0, 8):
                    S.op(pe, lambda h: h.transpose(out=bank_bf(0)[:, kc * 128:(kc + 1) * 128],
                                                   in_=qq[:, kc * 128:(kc + 1) * 128], identity=ident_b),
                         rd=[qq, cstb], wr=[bankR[0]], self_sync=False)
                S.op(act, lambda h: h.copy(out=qs[:, k0:8, i * 128:(i + 1) * 128],
                                           in_=bank_bf(0).rearrange("p (k t) -> p k t", t=128)[:, k0:8, :]),
                     rd=[bankR[0]], wr=[qs])

            def outstage(blk):
                tiles = blk_tiles(blk)
                nt = len(tiles)
                W = 128 * nt
                toff = 0 if blk == 0 else CTX + (blk - 1) * 512
                loff = (blk - 1) * 512
                qs = qkst[blk % 2]
                vs = vst[blk % 2]
                if blk > 0:
                    S.dma(sp, lambda h: h.dma_start(out=qT_d.rearrange("c p t -> p c t")[:, :, loff:loff + 512],
                                                    in_=qs[:, 0:4, :]), rd=[qs], wr=[R_qT])
                S.dma(sp, lambda h: h.dma_start(out=kT_d.rearrange("c p t -> p c t")[:, :, toff:toff + W],
                                                in_=qs[:, 4:8, 0:W]), rd=[qs], wr=[R_kT])
                S.dma(sp, lambda h: h.dma_start(
                    out=v_d[tiles[0]:tiles[0] + nt, :, :].rearrange("t p f -> p t f"),
                    in_=vs[:, 0:nt, :, :].rearrange("p t a e -> p t (a e)")), rd=[vs], wr=[R_v])

            flat = [(blk, i) for blk in range(9) for i in range(len(blk_tiles(blk)))]
            hlstage(0)
            fmstage(0)
            hlstage(1)
            prev = None
            for (blk, i) in flat:
                u = stage_a(blk, i)
                last = (i == len(blk_tiles(blk)) - 1)
                if i == len(blk_tiles(blk)) - 2 and blk + 2 < 9:
                    hl_front(blk + 2)
                if last and blk + 1 < 9:
                    fmstage(blk + 1)
                    if blk + 2 < 9:
                        hl_back(blk + 2)
                if prev is not None:
                    stage_b(*prev)
                stage_a2(blk, i, u)
                if prev is not None:
                    stage_b2(*prev)
                    if prev[1] == len(blk_tiles(prev[0])) - 1:
                        outstage(prev[0])
                prev = (blk, i, u)
            stage_b(*prev)
            stage_b2(*prev)
            outstage(prev[0])
        p01.close()
        S.barrier()
        if stop_after < 2:
            S.finish()
            return nc


        with ExitStack() as p2:
            TT = CTX + SEQ
            HL = SEQ // 2
            convw = sb(p2, "convw", [128, 16])
            convb = sb(p2, "convb", [128, 4])
            lrub = sb(p2, "lrub", [128, 16])
            lrul = sb(p2, "lrul", [128, 8])
            cL = sb(p2, "cL", [128, 8])
            cL2 = sb(p2, "cL2", [128, 8])
            onesT = sb(p2, "onesT", [128, 1])
            lruwb = sb(p2, "lruwb", [128, 16, 128], BF16)
            XP = sb(p2, "XP", [128, TT + 8])
            xcs = [sb(p2, f"xc{i}", [128, TT]) for i in range(2)]
            xcbs = [sb(p2, f"xcb{i}", [128, TT], BF16) for i in range(2)]
            Rs = [sb(p2, f"Rr{i}", [128, HL]) for i in range(2)]
            As = [sb(p2, f"A2_{i}", [128, HL]) for i in range(2)]
            Is = [sb(p2, f"Ii{i}", [128, HL]) for i in range(2)]
            Hf = sb(p2, "Hf", [128, TT])
            Hb = sb(p2, "Hb", [128, TT])
            gl = sb(p2, "gl", [128, SEQ], BF16)
            lst = sb(p2, "lst", [128, SEQ], BF16)
            S.dma(sp, lambda h: h.dma_start(out=convw[:], in_=convw_d), wr=[convw])
            S.dma(sp, lambda h: h.dma_start(out=convb[:], in_=convb_d), wr=[convb])
            S.dma(sp, lambda h: h.dma_start(out=lrub[:], in_=lrub_d), wr=[lrub])
            S.dma(sp, lambda h: h.dma_start(out=lrul[:], in_=lrul_d), wr=[lrul])
            S.dma(pool, lambda h: h.dma_start(out=lruwb[:], in_=lruw_d.rearrange("p (a n) -> p a n", n=128)),
                  wr=[lruwb])
            S.op(act, lambda h: h.activation(out=cL[:], in_=lrul[:], func=AF.Exp, scale=-1.0), rd=[lrul], wr=[cL])
            S.op(act, lambda h: h.activation(out=cL[:], in_=cL[:], func=AF.Ln, bias=1.0), rd=[cL], wr=[cL])
            S.op(dve, lambda h: h.tensor_scalar_mul(out=cL2[:], in0=cL[:], scalar1=-16.0), rd=[cL], wr=[cL2])
            S.op(dve, lambda h: h.tensor_scalar_mul(out=cL[:], in0=cL[:], scalar1=-8.0), rd=[cL, cL2], wr=[cL])
            S.op(dve, lambda h: h.memset(onesT[:], 1.0), wr=[onesT])
            S.op(dve, lambda h: h.memset(XP[:], 0.0), wr=[XP])
            segs = [(1, 0, CTX), (260, CTX, SEQ)]

            def conv(j):
                xc, xcb = xcs[j % 2], xcbs[j % 2]
                S.dma(sp, lambda h: h.dma_start(out=XP[:, 1:1 + CTX], in_=xl_d[j, :, 0:CTX]), rd=[R_xl[j]], wr=[XP])
                S.dma(sp, lambda h: h.dma_start(out=XP[:, 260:260 + SEQ], in_=xl_d[j, :, CTX:TT]),
                      rd=[R_xl[j]], wr=[XP])
                for (xo, to, ln) in segs:
                    S.op(dve, lambda h: h.tensor_scalar(out=xc[:, to:to + ln], in0=XP[:, xo - 1:xo - 1 + ln],
                                                         scalar1=convw[:, j * 4:j * 4 + 1], scalar2=convb[:, j:j + 1],
                                                         op0=ALU.mult, op1=ALU.add),
                         rd=[XP, convw, convb], wr=[xc])
                    for k in range(1, 4):
                        S.op(dve, lambda h: h.scalar_tensor_tensor(
                            out=xc[:, to:to + ln], in0=XP[:, xo - 1 + k:xo - 1 + k + ln],
                            scalar=convw[:, j * 4 + k:j * 4 + k + 1], in1=xc[:, to:to + ln],
                            op0=ALU.mult, op1=ALU.add), rd=[XP, convw, xc], wr=[xc])
                S.op(dve, lambda h: h.tensor_copy(out=xcb[:], in_=xc[:]), rd=[xc], wr=[xcb])

            pc = {"n": 0, "b": 0}

            def piece(j, d, t0, ln, first_col, init, rev):
                xc, xcb = xcs[j % 2], xcbs[j % 2]
                s_ = pc["n"] % 2
                pc["n"] += 1
                Rr, A2_, Ii = Rs[s_], As[s_], Is[s_]
                H = Hf if d == 0 else Hb
                for o in range(0, ln, 512):
                    w_ = min(512, ln - o)
                    for gi, dst in enumerate([Rr, Ii]):
                        b = 2 + pc["b"] % 4
                        pc["b"] += 1
                        S.op(pe, lambda h: h.matmul(bank(b)[:, 0:w_], lhsT=lruwb[:, (d * 2 + gi) * 4 + j, :],
                                                    rhs=xcb[:, t0 + o:t0 + o + w_], start=True, stop=True),
                             rd=[lruwb, xcb], wr=[bankR[b]], self_sync=False)
                        bi_ = (d * 2 + gi) * 4 + j
                        S.op(act, lambda h: h.activation(out=dst[:, o:o + w_], in_=bank(b)[:, 0:w_],
                                                         func=AF.Sigmoid, bias=lrub[:, bi_:bi_ + 1]),
                             rd=[bankR[b], lrub], wr=[dst])
                ci = d * 4 + j
                S.op(act, lambda h: h.activation(out=Rr[:, 0:ln], in_=Rr[:, 0:ln], func=AF.Exp, scale=cL[:, ci:ci + 1]),
                     rd=[Rr, cL], wr=[Rr])
                S.op(dve, lambda h: h.scalar_tensor_tensor(out=A2_[:, 0:ln], in0=Rr[:, 0:ln], scalar=1.0, in1=Rr[:, 0:ln],
                                                           op0=ALU.min, op1=ALU.mult), rd=[Rr], wr=[A2_])
                S.op(act, lambda h: h.activation(out=A2_[:, 0:ln], in_=A2_[:, 0:ln], func=AF.Sqrt, scale=-1.0,
                                                 bias=onesT[:, 0:1]), rd=[A2_, onesT], wr=[A2_])
                if first_col is not None:
                    S.op(dve, lambda h: h.memset(A2_[:, first_col:first_col + 1], 1.0), wr=[A2_])
                S.op(dve, lambda h: h.tensor_tensor(out=Ii[:, 0:ln], in0=Ii[:, 0:ln], in1=A2_[:, 0:ln], op=ALU.mult),
                     rd=[Ii, A2_], wr=[Ii])
                S.op(dve, lambda h: h.tensor_tensor(out=Ii[:, 0:ln], in0=Ii[:, 0:ln], in1=xc[:, t0:t0 + ln], op=ALU.mult),
                     rd=[Ii, xc], wr=[Ii])
                hv, av, uv = H[:, t0:t0 + ln], Rr[:, 0:ln], Ii[:, 0:ln]
                if rev:
                    hv, av, uv = hv[:, ::-1], av[:, ::-1], uv[:, ::-1]
                S.op(dve, lambda h: h.tensor_tensor_scan(out=hv, data0=av, data1=uv, initial=init,
                                                         op0=ALU.mult, op1=ALU.add), rd=[Rr, Ii, H], wr=[H])

            conv(0)
            for j in range(4):
                S.dma(sp, lambda h: h.dma_start(out=gl[:], in_=gel_d[j, :, :]), rd=[R_gel], wr=[gl])
                if j + 1 < 4:
                    conv(j + 1)
                piece(j, 0, 0, CTX, 0, 0.0, False)
                piece(j, 0, CTX, HL, None, Hf[:, CTX - 1:CTX], False)
                piece(j, 0, CTX + HL, HL, None, Hf[:, CTX + HL - 1:CTX + HL], False)
                piece(j, 1, 0, CTX, CTX - 1, 0.0, True)
                piece(j, 1, CTX + HL, HL, None, Hb[:, 0:1], True)
                piece(j, 1, CTX, HL, None, Hb[:, CTX + HL:CTX + HL + 1], True)
                S.op(dve, lambda h: h.tensor_tensor(out=Hf[:, CTX:TT], in0=Hf[:, CTX:TT], in1=Hb[:, CTX:TT], op=ALU.add),
                     rd=[Hf, Hb], wr=[Hf])
                S.op(dve, lambda h: h.tensor_tensor(out=lst[:], in0=Hf[:, CTX:TT], in1=gl[:], op=ALU.mult),
                     rd=[Hf, gl], wr=[lst])
                S.dma(sp, lambda h: h.dma_start(out=lru_d[j, :, :], in_=lst[:]), rd=[lst], wr=[R_lru])
        S.barrier()
        if stop_after < 3:
            S.finish()
            return nc


        with ExitStack() as p3:
            kT = sb(p3, "kT", [128, 4, CTX + SEQ], BF16)
            v1 = sb(p3, "v1", [128, NT, 520], BF16)
            woutb = sb(p3, "woutb", [128, 8, D], BF16)
            wrt = sb(p3, "wrt", [128, 8, NE])
            qz = [[sb(p3, f"qz{i}_{c}", [128, 4, 512], BF16) for c in range(2)] for i in range(2)]
            for i in range(2):
                for c in range(2):
                    S.op(pool, lambda h: h.memset(qz[i][c][:], 0.0), wr=[qz[i][c]])
            lruB = [sb(p3, f"lruB{i}", [128, 4, 512], BF16) for i in range(2)]
            Eb = [sb(p3, f"Eb{i}", [128, 1024], BF16) for i in range(3)]
            osb = [sb(p3, f"osb{i}", [128, 4, 128]) for i in range(2)]
            rl = sb(p3, "rl", [128, 8])
            ssn = sb(p3, "ssn", [128, 8])
            rsn = sb(p3, "rsn", [128, 8])
            junk3 = sb(p3, "junk3", [128, D], BF16)
            att = sb(p3, "att", [128, 4, 512], BF16)
            attT = sb(p3, "attT", [128, 4, 512], BF16)
            xres = [sb(p3, f"xres{i}", [128, D]) for i in range(2)]
            x1t = [sb(p3, f"x1t{i}", [128, D]) for i in range(2)]
            tmp3s = [sb(p3, f"tmp3{i}", [128, D]) for i in range(2)]
            h2fs = [sb(p3, f"h2f{i}", [128, D]) for i in range(2)]
            h2b = [sb(p3, f"h2b{i}", [128, D], BF16) for i in range(2)]
            h2Ts = [sb(p3, f"h2T{i}", [128, 8, 128]) for i in range(2)]
            lg = sb(p3, "lg", [128, NE])
            mx = sb(p3, "mx", [128, 4])
            S.dma(sp, lambda h: h.dma_start(out=kT[:], in_=kT_d.rearrange("c p t -> p c t")), rd=[R_kT], wr=[kT])
            S.dma(sp, lambda h: h.dma_start(out=v1[:], in_=v_d.rearrange("t p f -> p t f")), rd=[R_v], wr=[v1])
            S.dma(sp, lambda h: h.dma_start(out=wrt[:], in_=wr_d.rearrange("(kc p) n -> p kc n", p=128)), wr=[wrt])
            for kc in range(8):
                S.dma(pool, lambda h: h.dma_start(out=woutb[:, kc, :], in_=wout_d[kc * 128:(kc + 1) * 128, :]),
                      wr=[woutb])
            ssn2 = sb(p3, "ssn2", [128, 2])
            rsn2 = sb(p3, "rsn2", [128, 2])
            junk4 = sb(p3, "junk4", [128, D], BF16)

            def tail_steps(qb, lb, B0, B1):
                steps = []
                for s_ in range(4):
                    def st_t(s_=s_):
                        for hd in range(4):
                            S.op(pe, lambda h: h.transpose(out=bank_bf(B0)[:, hd * 128:(hd + 1) * 128],
                                                           in_=att[:, s_, hd * 128:(hd + 1) * 128], identity=ident_b),
                                 rd=[att, cstb], wr=[bankR[B0]], self_sync=False)
                        S.op(act, lambda h: h.copy(out=attT[:, :, s_ * 128:(s_ + 1) * 128],
                                                   in_=bank_bf(B0)[:, 0:512].rearrange("p (k t) -> p k t", t=128)),
                             rd=[bankR[B0]], wr=[attT])
                    steps.append(st_t)
                per_tile = []
                for s_ in range(4):
                    tok0 = qb * 512 + s_ * 128
                    tl = qb * 4 + s_
                    xr, x1 = xres[s_ % 2], x1t[s_ % 2]
                    hb_ = h2b[s_ % 2]
                    tmp3, h2f, h2T = tmp3s[s_ % 2], h2fs[s_ % 2], h2Ts[s_ % 2]

                    def st_w(fh, s_=s_, tok0=tok0, xr=xr, tmp3=tmp3):
                        bk = B0 if fh == 0 else B1
                        if fh == 0:
                            S.dma(sp, lambda h: h.dma_start(out=xr[:], in_=x_d[tok0:tok0 + 128, :]), wr=[xr])
                        for kc in range(8):
                            lhs = (lb[:, kc, s_ * 128:(s_ + 1) * 128] if kc < 4 else attT[:, kc - 4, s_ * 128:(s_ + 1) * 128])
                            S.op(pe, lambda h: h.matmul(bank(bk), lhsT=lhs, rhs=woutb[:, kc, fh * 512:(fh + 1) * 512],
                                                        start=(kc == 0), stop=(kc == 7)),
                                 rd=[lb, attT, woutb], wr=[bankR[bk]], self_sync=False)
                        S.op(dve, lambda h: h.tensor_tensor(out=tmp3[:, fh * 512:(fh + 1) * 512], in0=bank(bk),
                                                            in1=bcG[:, 0, fh * 512:(fh + 1) * 512], op=ALU.mult),
                             rd=[bankR[bk], bcG], wr=[tmp3])
                    tile_steps = {}
                    tile_steps["w0"] = (lambda st_w=st_w: st_w(0))

                    def st_n(st_w=st_w, tok0=tok0, xr=xr, x1=x1, hb_=hb_, tmp3=tmp3, h2f=h2f):
                        st_w(1)
                        S.op(dve, lambda h: h.tensor_tensor(out=x1[:], in0=tmp3[:], in1=xr[:], op=ALU.add),
                             rd=[tmp3, xr], wr=[x1])
                        S.dma(sp, lambda h: h.dma_start(out=out_d[tok0:tok0 + 128, :], in_=x1[:]), rd=[x1], wr=[R_out])
                        S.op(act, lambda h: h.activation(out=junk4[:], in_=x1[:], func=AF.Square, accum_out=ssn2[:, 0:1]),
                             rd=[x1], wr=[junk4, ssn2])
                        S.op(act, lambda h: h.activation(out=rsn2[:, 0:1], in_=ssn2[:, 0:1], func=AF.Ln, scale=1.0 / D,
                                                         bias=epsT[:, 0:1]), rd=[ssn2, epsT], wr=[rsn2])
                        S.op(act, lambda h: h.activation(out=rsn2[:, 0:1], in_=rsn2[:, 0:1], func=AF.Exp, scale=-0.5),
                             rd=[rsn2], wr=[rsn2])
                        S.op(dve, lambda h: h.scalar_tensor_tensor(out=tmp3[:], in0=x1[:], scalar=rsn2[:, 0:1], in1=bcG[:, 1, :],
                                                                   op0=ALU.mult, op1=ALU.mult), rd=[x1, rsn2, bcG], wr=[tmp3])
                        S.op(dve, lambda h: h.tensor_tensor(out=h2f[:], in0=tmp3[:], in1=bcG[:, 2, :], op=ALU.add),
                             rd=[tmp3, bcG], wr=[h2f])
                        S.op(act, lambda h: h.copy(out=hb_[:], in_=h2f[:]), rd=[h2f], wr=[hb_])
                        S.dma(sp, lambda h: h.dma_start(out=h2_d[tok0:tok0 + 128, :], in_=hb_[:]), rd=[hb_], wr=[R_h2])
                    tile_steps["n"] = st_n

                    def st_r(half, h2f=h2f, h2T=h2T):
                        for k4 in range(4):
                            kc = half * 4 + k4
                            S.op(pe, lambda h: h.transpose(out=bank(B0)[:, k4 * 128:(k4 + 1) * 128],
                                                           in_=h2f[:, kc * 128:(kc + 1) * 128], identity=ident_f),
                                 rd=[h2f, cst], wr=[bankR[B0]], self_sync=False)
                        S.op(dve, lambda h: h.tensor_copy(out=h2T[:, half * 4:half * 4 + 4, :],
                                                          in_=bank(B0).rearrange("p (k t) -> p k t", t=128)),
                             rd=[bankR[B0]], wr=[h2T])
                    tile_steps["r0"] = (lambda st_r=st_r: st_r(0))
                    tile_steps["r1"] = (lambda st_r=st_r: st_r(1))

                    def st_s(tl=tl, h2T=h2T):
                        for kc in range(8):
                            S.op(pe, lambda h: h.matmul(bank(B0)[:, 0:NE], lhsT=h2T[:, kc, :], rhs=wrt[:, kc, :],
                                                        start=(kc == 0), stop=(kc == 7)),
                                 rd=[h2T, wrt], wr=[bankR[B0]], self_sync=False)
                        S.op(dve, lambda h: h.reduce_max(out=mx[:, 0:1], in_=bank(B0)[:, 0:NE], axis=AX.X),
                             rd=[bankR[B0]], wr=[mx])
                        S.op(dve, lambda h: h.tensor_scalar_mul(out=mx[:, 1:2], in0=mx[:, 0:1], scalar1=-1.0), rd=[mx], wr=[mx])
                        S.op(act, lambda h: h.activation(out=lg[:], in_=bank(B0)[:, 0:NE], func=AF.Exp, bias=mx[:, 1:2],
                                                         accum_out=mx[:, 2:3]), rd=[bankR[B0], mx], wr=[lg, mx])
                        S.op(dve, lambda h: h.reciprocal(out=mx[:, 3:4], in_=mx[:, 2:3]), rd=[mx], wr=[mx])
                        S.op(dve, lambda h: h.tensor_scalar_mul(out=aff[:, tl, :], in0=lg[:], scalar1=mx[:, 3:4]),
                             rd=[lg, mx], wr=[aff])
                    tile_steps["sm"] = st_s
                    per_tile.append(tile_steps)
                order = [("w0", 0), ("n", 0), ("w0", 1), ("n", 1), ("r0", 0), ("r1", 0), ("w0", 2), ("n", 2), ("sm", 0),
                         ("r0", 1), ("r1", 1), ("w0", 3), ("n", 3), ("sm", 1), ("r0", 2), ("r1", 2), ("sm", 2),
                         ("r0", 3), ("r1", 3), ("sm", 3)]
                for (k_, t_) in order:
                    steps.append(per_tile[t_][k_])
                return steps

            pending = []
            deferred = []
            gcnt = {"g": 0}
            for qb in range(8):
                qt = qz[qb % 2]
                for c in range(2):
                    S.dma(sp, lambda h: h.dma_start(
                        out=qt[c][c * 64:(c + 1) * 64, :, :],
                        in_=qT_d.rearrange("c p t -> p c t")[c * 64:(c + 1) * 64, :, qb * 512:(qb + 1) * 512]),
                        rd=[R_qT], wr=[qt[c]])
                lb = lruB[qb % 2]
                S.dma(sp, lambda h: h.dma_start(out=lb[:], in_=lru_d.rearrange("c p t -> p c t")[:, :, qb * 512:(qb + 1) * 512]),
                      rd=[R_lru], wr=[lb])
                NP = NT // 2
                items = [(hd, c, kp) for hd in range(4) for c in range(2) for kp in range(NP)]

                def emit_S(i):
                    hd, c, kp = items[i]
                    pb = (i % 2) * 2
                    for u in range(2):
                        kt = kp * 2 + u
                        S.op(pe, lambda h: h.matmul(bank(pb + u), lhsT=kT[:, hd, kt * 128:(kt + 1) * 128],
                                                    rhs=qt[c][:, hd, :], start=True, stop=True),
                             rd=[kT, qt[c]], wr=[bankR[pb + u]], self_sync=False)

                emit_S(0)
                emit_S(1)
                for i, (hd, c, kp) in enumerate(items):
                    os_ = osb[hd % 2]
                    pb = (i % 2) * 2
                    ab = 4 + 2 * ((hd * 2 + c) % 2)
                    E = Eb[i % 3]
                    S.op(act, lambda h: h.activation(out=E[:], in_=PS[:, pb:pb + 2, :].rearrange("p a n -> p (a n)"),
                                                     func=AF.Exp, bias=sc2[:, 1:2]),
                         rd=[bankR[pb], bankR[pb + 1], sc2], wr=[E])
                    if i + 2 < len(items):
                        emit_S(i + 2)
                    for u in range(2):
                        kt = kp * 2 + u
                        for s_ in range(4):
                            bb = ab + s_ // 2
                            co = (s_ % 2) * 256
                            S.op(pe, lambda h: h.matmul(bank(bb)[:, co:co + 129],
                                                        lhsT=E[:, u * 512 + s_ * 128:u * 512 + (s_ + 1) * 128],
                                                        rhs=v1[:, kt, hd * 130:hd * 130 + 129],
                                                        start=(kt == 0 and s_ % 2 == 0), stop=(kt == NT - 1),
                                                        skip_group_check=True),
                                 rd=[E, v1], wr=[bankR[bb]], self_sync=False)
                    gcnt["g"] += 1
                    while deferred and deferred[0][0] <= gcnt["g"]:
                        deferred.pop(0)[1]()
                    if not deferred and i < NP - 1:
                        for _ in range(2):
                            if pending:
                                pending.pop(0)()
                    elif i == NP - 1:
                        while deferred:
                            deferred.pop(0)[1]()
                        while pending:
                            pending.pop(0)()
                    if kp < NP - 1:
                        continue
                    for s_ in range(4):
                        bb = ab + s_ // 2
                        co = (s_ % 2) * 256
                        S.op(dve, lambda h: h.reciprocal(out=rl[:, s_:s_ + 1], in_=bank(bb)[:, co + 128:co + 129]),
                             rd=[bankR[bb]], wr=[rl])
                        if c == 0:
                            S.op(dve, lambda h: h.tensor_scalar_mul(out=os_[:, s_, :], in0=bank(bb)[:, co:co + 128],
                                                                    scalar1=rl[:, s_:s_ + 1]),
                                 rd=[bankR[bb], rl], wr=[os_])
                        else:
                            S.op(dve, lambda h: h.tensor_tensor(out=rl[:, 4 + s_:5 + s_], in0=rl[:, s_:s_ + 1],
                                                                in1=sc2[:, 0:1], op=ALU.mult), rd=[rl, sc2], wr=[rl])
                            S.op(dve, lambda h: h.scalar_tensor_tensor(out=os_[:, s_, :], in0=bank(bb)[:, co:co + 128],
                                                                       scalar=rl[:, 4 + s_:5 + s_], in1=os_[:, s_, :],
                                                                       op0=ALU.mult, op1=ALU.add),
                                 rd=[bankR[bb], rl, os_], wr=[os_])
                    if c == 0:
                        continue
                    def subln(hd=hd, os_=os_):
                        for s_ in range(4):
                            S.op(act, lambda h: h.activation(out=junk3[:, 0:128], in_=os_[:, s_, :], func=AF.Square,
                                                             accum_out=ssn[:, s_:s_ + 1]), rd=[os_], wr=[junk3, ssn])
                        S.op(act, lambda h: h.activation(out=rsn[:, 0:4], in_=ssn[:, 0:4], func=AF.Ln, scale=1.0 / 128,
                                                         bias=epsT[:, 0:1]), rd=[ssn, epsT], wr=[rsn])
                        S.op(act, lambda h: h.activation(out=rsn[:, 0:4], in_=rsn[:, 0:4], func=AF.Exp, scale=-0.5),
                             rd=[rsn], wr=[rsn])
                        for s_ in range(4):
                            S.op(dve, lambda h: h.scalar_tensor_tensor(out=att[:, s_, hd * 128:(hd + 1) * 128], in0=os_[:, s_, :],
                                                                       scalar=rsn[:, s_:s_ + 1], in1=bcs[:, hd * 128:(hd + 1) * 128],
                                                                       op0=ALU.mult, op1=ALU.mult),
                                 rd=[os_, rsn, bcs], wr=[att])
                    deferred.append((gcnt["g"] + 3, subln))
                pending = tail_steps(qb, lb, 6, 7)
            while deferred:
                deferred.pop(0)[1]()
            for st in pending:
                st()
            if dbg:
                S.dma(sp, lambda h: h.dma_start(out=dbg_aff, in_=aff[:].rearrange("p t e -> p (t e)")), rd=[aff])
        p03.close()
        S.barrier()
        if stop_after < 4:
            S.finish()
            return nc

        p45 = top.enter_context(ExitStack())
        NGU = 4
        NB = 11
        wgb = [sb(p45, f"wgb{i}", [128, 8, 256], BF16) for i in range(NGU)]
        wub = [sb(p45, f"wub{i}", [128, 8, 256], BF16) for i in range(NGU)]
        wdb = [sb(p45, f"wdb{i}", [128, NJ, 512], BF16) for i in range(2)]

        def load_gu(e, b):
            i = (e * NB + b) % NGU
            for (dst, srcw) in ((wgb[i], wg_d), (wub[i], wu_d)):
                S.dma(pool, lambda h: h.dma_start(
                    out=dst[:], in_=srcw[e].rearrange("(kc p) n -> p kc n", p=128)[:, :, b * 256:(b + 1) * 256]),
                    wr=[dst])

        def load_d(e, fh):
            dst = wdb[fh]
            S.dma(pool, lambda h: h.dma_start(
                out=dst[:], in_=wd_d[e].rearrange("(j p) f -> p j f", p=128)[:, :, fh * 512:(fh + 1) * 512]),
                wr=[dst])

        if stop_after >= 5:
            for b_ in range(NGU):
                load_gu(0, b_)
            load_d(0, 0)
            load_d(0, 1)

        with ExitStack() as p4:
            lo = sb(p4, "lo", [128, NE])
            hi = sb(p4, "hi", [128, NE])
            mid = sb(p4, "mid", [128, NE])
            ta = sb(p4, "ta", [128, NE])
            cmpb = sb(p4, "cmpb", [128, 32, NE], BF16)
            maskb = sb(p4, "maskb", [128, 32, NE], BF16)
            cum = sb(p4, "cum", [128, 32, NE], BF16)
            posm = sb(p4, "posm", [128, 32, NE])
            TI = sb(p4, "TI", [128, NE, 32, 8], BF16)
            rres = sb(p4, "rres", [128, 32, NE])
            Sb = [sb(p4, f"Sb{i}", [128, 512], BF16) for i in range(4)]
            idxf = sb(p4, "idxf", [128, 4])
            pvs = sb(p4, "pvs", [128, 32])
            S.op(dve, lambda h: h.memset(lo[:], 0.0), wr=[lo])
            S.op(dve, lambda h: h.memset(hi[:], 1.0), wr=[hi])
            S.op(dve, lambda h: h.memset(mid[:], 0.5), wr=[mid])
            for it in range(32):
                S.op(dve, lambda h: h.tensor_tensor(out=cmpb[:], in0=aff[:],
                                                    in1=mid[:].unsqueeze(1).broadcast_to([128, 32, NE]), op=ALU.is_ge),
                     rd=[aff, mid], wr=[cmpb])
                for t in range(32):
                    S.op(pe, lambda h: h.matmul(bank(0)[:, 0:NE], lhsT=ones_b, rhs=cmpb[:, t, :],
                                                start=(t == 0), stop=(t == 31)),
                         rd=[cmpb, cstb], wr=[bankR[0]], self_sync=False)
                S.op(dve, lambda h: h.scalar_tensor_tensor(out=ta[:], in0=bank(0)[:, 0:NE], scalar=float(CAP) - 0.5,
                                                           in1=mid[:], op0=ALU.is_ge, op1=ALU.mult),
                     rd=[bankR[0], mid], wr=[ta])
                S.op(dve, lambda h: h.tensor_tensor(out=lo[:], in0=lo[:], in1=ta[:], op=ALU.max), rd=[lo, ta], wr=[lo])
                S.op(dve, lambda h: h.scalar_tensor_tensor(out=ta[:], in0=bank(0)[:, 0:NE], scalar=float(CAP) - 0.5,
                                                           in1=mid[:], op0=ALU.is_ge, op1=ALU.add),
                     rd=[bankR[0], mid], wr=[ta])
                S.op(dve, lambda h: h.tensor_tensor(out=hi[:], in0=hi[:], in1=ta[:], op=ALU.min), rd=[hi, ta], wr=[hi])
                S.op(dve, lambda h: h.tensor_tensor(out=mid[:], in0=lo[:], in1=hi[:], op=ALU.add), rd=[lo, hi], wr=[mid])
                S.op(dve, lambda h: h.tensor_scalar_mul(out=mid[:], in0=mid[:], scalar1=0.5), rd=[mid], wr=[mid])
            S.op(dve, lambda h: h.tensor_tensor(out=maskb[:], in0=aff[:],
                                                in1=lo[:].unsqueeze(1).broadcast_to([128, 32, NE]), op=ALU.is_ge),
                 rd=[aff, lo], wr=[maskb])
            S.op(dve, lambda h: h.memset(cum[:, 0, :], 0.0), wr=[cum])
            for t in range(1, 32):
                S.op(dve, lambda h: h.tensor_tensor(out=cum[:, t, :], in0=cum[:, t - 1, :], in1=maskb[:, t - 1, :],
                                                    op=ALU.add), rd=[cum, maskb], wr=[cum])
            S.op(pe, lambda h: h.matmul(bank(1), lhsT=ustr_b, rhs=maskb[:].rearrange("p t e -> p (t e)"),
                                        start=True, stop=False), rd=[maskb, cstb], wr=[bankR[1]], self_sync=False)
            S.op(pe, lambda h: h.matmul(bank(1), lhsT=ones_b, rhs=cum[:].rearrange("p t e -> p (t e)"),
                                        start=False, stop=True), rd=[cum, cstb], wr=[bankR[1]], self_sync=False)
            S.op(dve, lambda h: h.scalar_tensor_tensor(out=posm[:].rearrange("p t e -> p (t e)"), in0=bank(1), scalar=1.0,
                                                       in1=maskb[:].rearrange("p t e -> p (t e)"),
                                                       op0=ALU.add, op1=ALU.mult), rd=[bankR[1], maskb], wr=[posm])
            S.op(dve, lambda h: h.tensor_scalar_add(out=posm[:], in0=posm[:], scalar1=-1.0), rd=[posm], wr=[posm])
            S.op(pool, lambda h: h.memset(TI[:], 0.0), wr=[TI])
            S.op(dve, lambda h: h.tensor_copy(out=TI[:, :, :, 0],
                                              in_=cst[:, 897:929].unsqueeze(1).broadcast_to([128, NE, 32])),
                 rd=[cst, TI], wr=[TI])
            S.op(dve, lambda h: h.tensor_copy(out=TI[:, :, :, 1],
                                              in_=cst[:, 896:897].unsqueeze(1).broadcast_to([128, NE, 32])),
                 rd=[cst, TI], wr=[TI])
            affv = aff[:].rearrange("p t e -> p e t")
            rresv = rres[:].rearrange("p t e -> p e t")
            S.op(dve, lambda h: h.tensor_copy(out=TI[:, :, :, 2], in_=affv), rd=[aff, TI], wr=[TI])
            S.op(dve, lambda h: h.tensor_tensor(out=rresv, in0=affv, in1=TI[:, :, :, 2], op=ALU.subtract),
                 rd=[aff, TI], wr=[rres])
            S.op(dve, lambda h: h.tensor_copy(out=TI[:, :, :, 3], in_=rresv), rd=[rres, TI], wr=[TI])
            S.op(dve, lambda h: h.tensor_tensor(out=rresv, in0=rresv, in1=TI[:, :, :, 3], op=ALU.subtract),
                 rd=[rres, TI], wr=[rres])
            S.op(dve, lambda h: h.tensor_copy(out=TI[:, :, :, 4], in_=rresv), rd=[rres, TI], wr=[TI])
            scn = 0
            for e in range(NE):
                b = 2 + e % 2
                for t in range(32):
                    Sx = Sb[scn % 4]
                    scn += 1
                    S.op(dve, lambda h: h.tensor_scalar(out=Sx[:], in0=iota_f, scalar1=posm[:, t, e:e + 1], scalar2=None,
                                                        op0=ALU.is_equal), rd=[cst, posm], wr=[Sx])
                    for sc in range(4):
                        S.op(pe, lambda h: h.matmul(bank(b)[:, sc * 8:sc * 8 + 8], lhsT=Sx[:, sc * 128:(sc + 1) * 128],
                                                    rhs=TI[:, e, t, :], start=(t == 0 and sc == 0), stop=(t == 31),
                                                    skip_group_check=True),
                             rd=[Sx, TI], wr=[bankR[b]], self_sync=False)
                S.op(dve, lambda h: h.tensor_copy(out=pvs[:], in_=bank(b)[:, 0:32]), rd=[bankR[b]], wr=[pvs])
                pv = pvs[:].rearrange("p (s c) -> p s c", c=8)
                S.op(dve, lambda h: h.scalar_tensor_tensor(out=idxf[:], in0=pv[:, :, 0], scalar=128.0, in1=pv[:, :, 1],
                                                           op0=ALU.mult, op1=ALU.add), rd=[pvs], wr=[idxf])
                S.op(dve, lambda h: h.tensor_copy(out=idx_i[:, e, :], in_=idxf[:]), rd=[idxf], wr=[idx_i])
                S.op(dve, lambda h: h.tensor_tensor(out=gsl[:, e, :], in0=pv[:, :, 2], in1=pv[:, :, 3], op=ALU.add),
                     rd=[pvs], wr=[gsl])
                S.op(dve, lambda h: h.tensor_tensor(out=gsl[:, e, :], in0=gsl[:, e, :], in1=pv[:, :, 4], op=ALU.add),
                     rd=[pvs, gsl], wr=[gsl])
            if dbg:
                S.dma(sp, lambda h: h.dma_start(out=dbg_idx, in_=idx_i[:].rearrange("p e s -> p (e s)")), rd=[idx_i])
                S.dma(sp, lambda h: h.dma_start(out=dbg_g, in_=gsl[:].rearrange("p e s -> p (e s)")), rd=[gsl])
        S.barrier()
        if stop_after < 5:
            S.finish()
            return nc

        with ExitStack() as p5:
            xe = [sb(p5, f"xe{i}", [128, 4, D], BF16) for i in range(2)]
            xeT = [sb(p5, f"xeT{i}", [128, 8, 512], BF16) for i in range(2)]
            hT = [sb(p5, f"hTe{i}", [128, NJ, 512], BF16) for i in range(2)]
            old = [sb(p5, f"old{i}", [128, D]) for i in range(4)]
            sg = [sb(p5, f"sg{i}", [128, 512]) for i in range(2)]
            tmp5 = [sb(p5, f"tmp5{i}", [128, 512]) for i in range(2)]
            xe_r = [[Reg() for _ in range(4)] for _ in range(2)]
            R_osc = [Reg() for _ in range(4)]

            def gather_xe(e, scs=range(4)):
                xg = xe[e % 2]
                for sc in scs:
                    S.dma(pool, lambda h: h.indirect_dma_start(
                        out=xg[:, sc, :], out_offset=None, in_=h2_d[:, :],
                        in_offset=bass.IndirectOffsetOnAxis(ap=idx_i[:, e, sc:sc + 1], axis=0)),
                        rd=[R_h2, idx_i], wr=[xg])

            def gather_old(e, scs=range(4)):
                for sc in scs:
                    S.dma(pool, lambda h: h.indirect_dma_start(
                        out=old[sc][:], out_offset=None, in_=out_d[:, :],
                        in_offset=bass.IndirectOffsetOnAxis(ap=idx_i[:, e, sc:sc + 1], axis=0)),
                        rd=[R_out, idx_i], wr=[old[sc]])

            def make_xeT(e):
                xg, xt_ = xe[e % 2], xeT[e % 2]
                for kc in range(8):
                    for sc in range(4):
                        S.op(pe, lambda h: h.transpose(out=bank_bf(0)[:, sc * 128:(sc + 1) * 128],
                                                       in_=xg[:, sc, kc * 128:(kc + 1) * 128], identity=ident_b),
                             rd=[xg, cstb], wr=[bankR[0]], self_sync=False)
                    S.op(act, lambda h: h.copy(out=xt_[:, kc, :], in_=bank_bf(0)[:, 0:512]), rd=[bankR[0]], wr=[xt_])

            def scatter_new(e, scs=range(4)):
                for sc in scs:
                    S.dma(pool, lambda h: h.indirect_dma_start(
                        out=out_d[:, :], out_offset=bass.IndirectOffsetOnAxis(ap=idx_i[:, e, sc:sc + 1], axis=0),
                        in_=old[sc][:], in_offset=None), rd=[old[sc], idx_i], wr=[R_out])

            gather_xe(0)
            gather_old(0)
            make_xeT(0)
            pcnt = 0
            dcnt = 0
            for e in range(NE):
                xt_ = xeT[e % 2]
                hT_ = hT[e % 2]
                for b in range(NB):
                    i = (e * NB + b) % NGU
                    if b < 4 and e + 1 < NE:
                        gather_xe(e + 1, [b])
                    if 4 <= b < 8 and e > 0:
                        scatter_new(e - 1, [b - 4])
                    if b >= 8 and e > 0:
                        gather_old(e, [b - 8])
                    for jj in range(2):
                        j = b * 2 + jj
                        bg = 1 + (pcnt % 2) * 2
                        pcnt += 1
                        for (bk, wt) in ((bg, wgb[i]), (bg + 1, wub[i])):
                            for kc in range(8):
                                S.op(pe, lambda h: h.matmul(bank(bk), lhsT=wt[:, kc, jj * 128:(jj + 1) * 128], rhs=xt_[:, kc, :],
                                                            start=(kc == 0), stop=(kc == 7)),
                                     rd=[wt, xt_], wr=[bankR[bk]], self_sync=False)
                        sg_ = sg[j % 2]
                        S.op(act, lambda h: h.activation(out=sg_[:], in_=bank(bg), func=AF.Silu), rd=[bankR[bg]], wr=[sg_])
                        S.op(dve, lambda h: h.tensor_tensor(out=hT_[:, j, :], in0=sg_[:], in1=bank(bg + 1), op=ALU.mult),
                             rd=[sg_, bankR[bg + 1]], wr=[hT_])
                    nb_ = b + NGU
                    if nb_ < NB:
                        load_gu(e, nb_)
                    elif e + 1 < NE:
                        load_gu(e + 1, nb_ - NB)
                if e > 0:
                    gather_old(e, [3])
                if e + 1 < NE:
                    make_xeT(e + 1)
                for fh in range(2):
                    wd_ = wdb[fh]
                    for sc in range(4):
                        bk = 5 + dcnt % 3
                        dcnt += 1
                        for j in range(NJ):
                            S.op(pe, lambda h: h.matmul(bank(bk), lhsT=hT_[:, j, sc * 128:(sc + 1) * 128], rhs=wd_[:, j, :],
                                                        start=(j == 0), stop=(j == NJ - 1)),
                                 rd=[hT_, wd_], wr=[bankR[bk]], self_sync=False)
                        t5 = tmp5[sc % 2]
                        S.op(dve, lambda h: h.scalar_tensor_tensor(out=t5[:], in0=bank(bk), scalar=gsl[:, e, sc:sc + 1],
                                                                   in1=bcG[:, 3, fh * 512:(fh + 1) * 512],
                                                                   op0=ALU.mult, op1=ALU.mult),
                             rd=[bankR[bk], gsl, bcG], wr=[t5])
                        S.op(dve, lambda h: h.tensor_tensor(out=old[sc][:, fh * 512:(fh + 1) * 512],
                                                             in0=old[sc][:, fh * 512:(fh + 1) * 512], in1=t5[:], op=ALU.add),
                             rd=[old[sc], t5], wr=[old[sc]])
                    if e + 1 < NE:
                        load_d(e + 1, fh)
            scatter_new(NE - 1)
        S.finish()
    return nc


def _consts():
    c = np.zeros((128, NCONST), np.float32)
    c[:, 0:128] = np.eye(128, dtype=np.float32)
    p = np.arange(128)
    c[:, 128:256] = (p[:, None] < p[None, :]).astype(np.float32)
    c[:, 256:384] = 1.0
    c[:, 384:896] = np.arange(512, dtype=np.float32)[None, :]
    c[:, 896] = p.astype(np.float32)
    c[:, 897:929] = np.arange(32, dtype=np.float32)[None, :]
    return c


def _rope_table16():
    tab = np.zeros((128, NT, 2, 64), np.float32)
    tab[:, :, 0, :] = 1.0
    inv = (np.float32(10000.0) ** (-np.arange(16, dtype=np.float32) / np.float32(16))).astype(np.float32)
    for T in range(2, NT):
        tok = (T - 2) * 128 + np.arange(128)
        row = (tok // 64).astype(np.float32)
        col = (tok % 64).astype(np.float32)
        ar = (row[:, None] * inv[None, :]).astype(np.float32)
        ac = (col[:, None] * inv[None, :]).astype(np.float32)
        cr, sr, cc, sc_ = np.cos(ar), np.sin(ar), np.cos(ac), np.sin(ac)
        tab[:, T, 0, 0:16] = cr
        tab[:, T, 0, 16:32] = cr
        tab[:, T, 0, 32:48] = cc
        tab[:, T, 0, 48:64] = cc
        tab[:, T, 1, 0:16] = -sr
        tab[:, T, 1, 16:32] = sr
        tab[:, T, 1, 32:48] = -sc_
        tab[:, T, 1, 48:64] = sc_
    return tab.reshape(128, NT * 128)


def make_in_maps(inp, cores):
    f = lambda a: np.ascontiguousarray(np.asarray(a, dtype=np.float32))
    L = 0
    c_ctx = f(inp["c_ctx"])
    conv_w = f(inp["conv_w"][L])
    convw = np.zeros((128, 16), np.float32)
    for j in range(4):
        for k in range(4):
            convw[:, j * 4 + k] = conv_w[k, j * 128:(j + 1) * 128]
    convb = f(inp["conv_b"][L]).reshape(4, 128).T
    lruw = np.zeros((128, 2, 2, 4, 128), np.float32)
    lrub = np.zeros((128, 2, 2, 4), np.float32)
    for d in range(2):
        for gi, (wn, bn) in enumerate((("lru_wa", "lru_ba"), ("lru_wi", "lru_bi"))):
            w = f(inp[wn][L][d])
            bb = f(inp[bn][L][d])
            for j in range(4):
                for hh in range(2):
                    lruw[hh * 64:(hh + 1) * 64, d, gi, j, hh * 64:(hh + 1) * 64] = w[2 * j + hh]
                    lrub[hh * 64:(hh + 1) * 64, d, gi, j] = bb[2 * j + hh]
    lrul = np.zeros((128, 2, 4), np.float32)
    lam = f(inp["lru_lambda"][L])
    for d in range(2):
        lrul[:, d, :] = lam[d].reshape(4, 128).T
    shared = {
        "w_ada": f(inp["w_ada"][L]),
        "b_ada": f(inp["b_ada"][L]).reshape(1, -1),
        "normg": np.concatenate([f(inp["norm1_g"][L]), f(inp["norm2_g"][L])]).reshape(1, -1),
        "qkg": np.concatenate([np.tile(f(inp["q_norm_g"][L]), 8), np.tile(f(inp["k_norm_g"][L]), 8)]).reshape(1, -1),
        "lamv": np.concatenate([f(inp["lambda_q1"][L]), f(inp["lambda_k1"][L]),
                                f(inp["lambda_q2"][L]), f(inp["lambda_k2"][L])]).reshape(1, -1),
        "subg": np.tile(f(inp["subln_g"][L]), 4).reshape(1, -1),
        "w_in": f(inp["w_in"][L]),
        "convw": convw,
        "convb": np.ascontiguousarray(convb),
        "lruw": np.ascontiguousarray(lruw.reshape(128, 2048)),
        "lrub": np.ascontiguousarray(lrub.reshape(128, 16)),
        "lrul": np.ascontiguousarray(lrul.reshape(128, 8)),
        "w_out": f(inp["w_out"][L]),
        "w_router": f(inp["w_router"][L]),
        "w_gate": f(inp["w_gate"][L]),
        "w_up": f(inp["w_up"][L]),
        "w_down": f(inp["w_down"][L]),
        "rope": _rope_table16(),
        "consts": _consts(),
    }
    maps = []
    for b in cores:
        cvec = np.zeros((128, 16), np.float32)
        cvec[:, 0:8] = f(inp["c"][b]).reshape(8, 128).T
        cvec[:, 8:16] = c_ctx.reshape(8, 128).T
        m = dict(shared)
        m["x"] = f(inp["x"][b])
        m["ctx"] = f(inp["ctx"][b])
        m["cvec"] = cvec
        maps.append(m)
    return maps


def kernel(**inputs):
    nc = build()
    maps = make_in_maps(inputs, list(range(8)))
    res = run_bass_kernel_spmd(nc, maps, core_ids=list(range(8)))
    return np.stack([np.asarray(r["out"], dtype=np.float32) for r in res.results], axis=0)
```

```python
import math
from contextlib import ExitStack

import numpy as np
import concourse.bass as bass
import concourse.mybir as mybir
from concourse.bass_utils import run_bass_kernel_spmd

F32 = mybir.dt.float32
BF16 = mybir.dt.bfloat16
I32 = mybir.dt.int32
AF = mybir.ActivationFunctionType
ALU = mybir.AluOpType
AX = mybir.AxisListType

D = 1024
SEQ = 4096
CTX = 256
NT = 34
NE = 16
CAP = 512
DEXP = 2816
NJ = 22
EPS = 1e-6
LAM_INIT = 0.2
NCONST = 936


class Reg:
    __slots__ = ("w", "rs")

    def __init__(self):
        self.w = {}
        self.rs = {}


class Tile:
    def __init__(self, t):
        self.t = t
        self.r = Reg()

    def __getitem__(self, k):
        return self.t[k]


class Eng:
    def __init__(self, h, key, dkeys):
        self.h = h
        self.key = key
        self.n = 0
        self.seen = {}
        self.dkeys = dkeys
        self.dvals = [0] * len(dkeys)
        self.di = 0


def _reg(x):
    return x.r if isinstance(x, Tile) else x


class Sched:
    def __init__(self, nc, stack):
        self.nc = nc
        self.sems = []

        def mk(name):
            s = stack.enter_context(nc.semaphore(name))
            self.sems.append(s)
            return len(self.sems) - 1

        self.pe = Eng(nc.tensor, mk("s_pe"), [])
        self.act = Eng(nc.scalar, mk("s_act"), [])
        self.dve = Eng(nc.vector, mk("s_dve"), [])
        self.pool = Eng(nc.gpsimd, mk("s_pool"), [mk(f"d_pool{i}") for i in range(12)])
        self.sp = Eng(nc.sync, mk("s_sp"), [mk(f"d_sp{i}") for i in range(12)])
        self.engs = [self.pe, self.act, self.dve, self.pool, self.sp]

    def _deps(self, rd, wr):
        deps = {}

        def add(k, v):
            if deps.get(k, 0) < v:
                deps[k] = v

        for r in rd:
            r = _reg(r)
            for k, v in r.w.items():
                add(k, v)
        for r in wr:
            r = _reg(r)
            for k, v in r.w.items():
                add(k, v)
            for k, v in r.rs.items():
                add(k, v)
        return deps

    def _wait(self, e, deps, self_sync):
        for k, v in deps.items():
            if k == e.key and not self_sync:
                continue
            if e.seen.get(k, 0) >= v:
                continue
            e.h.wait_ge(self.sems[k], v)
            e.seen[k] = v

    def _mark(self, ev, rd, wr):
        k, v = ev
        for r in rd:
            r = _reg(r)
            if r.rs.get(k, 0) < v:
                r.rs[k] = v
        for r in wr:
            r = _reg(r)
            r.w[k] = v
            r.rs = {}

    def op(self, e, fn, rd=(), wr=(), self_sync=True):
        self._wait(e, self._deps(rd, wr), self_sync)
        ins = fn(e.h)
        e.n += 1
        ins.then_inc(self.sems[e.key], 1)
        self._mark((e.key, e.n), rd, wr)
        return ins

    def dma(self, q, fn, rd=(), wr=()):
        self._wait(q, self._deps(rd, wr), True)
        slot = q.di % len(q.dkeys)
        q.di += 1
        k = q.dkeys[slot]
        prev = q.dvals[slot]
        if q.seen.get(k, 0) < prev:
            q.h.wait_ge(self.sems[k], prev)
            q.seen[k] = prev
        ins = fn(q.h)
        q.dvals[slot] = prev + 16
        ins.then_inc(self.sems[k], 16)
        self._mark((k, prev + 16), rd, wr)
        return ins

    def barrier(self):
        for e in self.engs:
            for o in self.engs:
                if o.n > 0 and e.seen.get(o.key, 0) < o.n and o is not e:
                    e.h.wait_ge(self.sems[o.key], o.n)
                    e.seen[o.key] = o.n
                for k, v in zip(o.dkeys, o.dvals):
                    if v > 0 and e.seen.get(k, 0) < v:
                        e.h.wait_ge(self.sems[k], v)
                        e.seen[k] = v

    def finish(self):
        q = self.sp
        for e in self.engs:
            for k, v in zip(e.dkeys, e.dvals):
                if v > 0 and q.seen.get(k, 0) < v:
                    q.h.wait_ge(self.sems[k], v)
                    q.seen[k] = v
        for e in self.engs:
            if e is not q and e.n > 0:
                q.h.wait_ge(self.sems[e.key], e.n)


def build(dbg=False, stop_after=99):
    nc = bass.Bass("TRN2", target_bir_lowering=False)
    okind = "ExternalOutput" if dbg else "Internal"

    def din(name, shape, dt=F32):
        return nc.dram_tensor(name, list(shape), dt, kind="ExternalInput").ap()

    x_d = din("x", [SEQ, D])
    ctx_d = din("ctx", [CTX, D])
    cvec_d = din("cvec", [128, 16])
    wada_d = din("w_ada", [D, 6 * D])
    bada_d = din("b_ada", [1, 6 * D])
    normg_d = din("normg", [1, 2 * D])
    qkg_d = din("qkg", [1, 1024])
    lamv_d = din("lamv", [1, 256])
    subg_d = din("subg", [1, 512])
    win_d = din("w_in", [D, 2560])
    convw_d = din("convw", [128, 16])
    convb_d = din("convb", [128, 4])
    lruw_d = din("lruw", [128, 2048])
    lrub_d = din("lrub", [128, 16])
    lrul_d = din("lrul", [128, 8])
    wout_d = din("w_out", [D, D])
    wr_d = din("w_router", [D, NE])
    big = stop_after >= 5
    wg_d = din("w_gate", [NE, D, DEXP] if big else [1, 8, 8])
    wu_d = din("w_up", [NE, D, DEXP] if big else [1, 8, 8])
    wd_d = din("w_down", [NE, DEXP, D] if big else [1, 8, 8])
    rope_d = din("rope", [128, NT * 128])
    consts_d = din("consts", [128, NCONST])
    out_d = nc.dram_tensor("out", [SEQ, D], F32, kind="ExternalOutput").ap()

    xl_d = nc.dram_tensor("xl_s", [4, 128, CTX + SEQ], F32, kind=okind).ap()
    gel_d = nc.dram_tensor("gel_s", [4, 128, SEQ], BF16, kind=okind).ap()
    qT_d = nc.dram_tensor("qT_s", [4, 128, SEQ], BF16, kind=okind).ap()
    kT_d = nc.dram_tensor("kT_s", [4, 128, CTX + SEQ], BF16, kind=okind).ap()
    v_d = nc.dram_tensor("v_s", [NT, 128, 520], BF16, kind=okind).ap()
    h2_d = nc.dram_tensor("h2_s", [SEQ, D], BF16, kind=okind).ap()
    lru_d = nc.dram_tensor("lru_s", [4, 128, SEQ], BF16, kind=okind).ap()
    if dbg:
        dbg_mod = nc.dram_tensor("dbg_mod", [1, 8192], F32, kind="ExternalOutput").ap()
        dbg_aff = nc.dram_tensor("dbg_aff", [128, 32 * NE], F32, kind="ExternalOutput").ap()
        dbg_idx = nc.dram_tensor("dbg_idx", [128, NE * 4], I32, kind="ExternalOutput").ap()
        dbg_g = nc.dram_tensor("dbg_g", [128, NE * 4], F32, kind="ExternalOutput").ap()

    R_xl = [Reg() for _ in range(4)]
    R_gel, R_qT, R_kT, R_v, R_h2, R_out, R_lru = Reg(), Reg(), Reg(), Reg(), Reg(), Reg(), Reg()

    with ExitStack() as top:
        S = Sched(nc, top)
        pe, act, dve, pool, sp = S.pe, S.act, S.dve, S.pool, S.sp

        def sb(stack, name, shape, dt=F32):
            return Tile(stack.enter_context(nc.sbuf_tensor("t_" + name, list(shape), dt)))

        PS = top.enter_context(nc.psum_tensor("PS", [128, 8, 512], F32))
        bankR = [Reg() for _ in range(8)]

        def bank(b):
            return PS[:, b, :]

        def bank_bf(b):
            return PS[:, b, :].bitcast(BF16)

        cst = sb(top, "cst", [128, NCONST])
        cstb = sb(top, "cstb", [128, 384], BF16)
        bcG = sb(top, "bcG", [128, 4, D])
        aff = sb(top, "aff", [128, 32, NE])
        idx_i = sb(top, "idx_i", [128, NE, 4], I32)
        gsl = sb(top, "gsl", [128, NE, 4])
        A1, B1, A1C, B1C, G1, A2, B2, G2 = range(8)
        sc2 = sb(top, "sc2", [128, 2])
        epsT = sb(top, "epsT", [128, 1])
        S.dma(sp, lambda h: h.dma_start(out=cst[:], in_=consts_d), wr=[cst])
        S.op(dve, lambda h: h.tensor_copy(out=cstb[:], in_=cst[:, 0:384]), rd=[cst], wr=[cstb])
        S.op(dve, lambda h: h.memset(epsT[:], EPS), wr=[epsT])
        ident_f = cst[:, 0:128]
        ones_f = cst[:, 256:384]
        iota_f = cst[:, 384:896]
        ident_b = cstb[:, 0:128]
        ustr_b = cstb[:, 128:256]
        ones_b = cstb[:, 256:384]

        def rsqrt_ops(dst, src, scale, n, lo=0):
            S.op(act, lambda h: h.activation(out=dst[:, lo:n], in_=src[:, lo:n], func=AF.Sqrt,
                                             scale=scale, bias=epsT[:, 0:1]),
                 rd=[src, epsT], wr=[dst])
            S.op(dve, lambda h: h.reciprocal(out=dst[:, lo:n], in_=dst[:, lo:n]), rd=[dst], wr=[dst])

        p03 = top.enter_context(ExitStack())
        bcs = sb(p03, "bcs", [128, 512])
        p01 = p03.enter_context(ExitStack())
        bcq = sb(p01, "bcq", [128, 1024])
        bcA = sb(p01, "bcA", [128, 4, D])

        def bcr(i):
            return (bcA, i) if i < 4 else (bcG, i - 4)

        winb = sb(p01, "winb", [128, 8, 2560], BF16)
        ropeT = sb(p01, "ropeT", [128, NT, 2, 64])
        for kc in range(8):
            S.dma(pool, lambda h: h.dma_start(
                out=winb[:, kc, :].rearrange("p (a n) -> p a n", n=640),
                in_=win_d[kc * 128:(kc + 1) * 128, :].rearrange("p (a n) -> p a n", n=640)), wr=[winb])
        S.dma(sp, lambda h: h.dma_start(out=ropeT[:].rearrange("p t c d -> p (t c d)"), in_=rope_d), wr=[ropeT])

        with ExitStack() as p0:
            cv = sb(p0, "cv", [128, 16])
            scv = sb(p0, "scv", [128, 16])
            modrow = sb(p0, "modrow", [1, 8192])
            bada = sb(p0, "bada", [1, 6 * D])
            normg = sb(p0, "normg", [1, 2 * D])
            rowt = sb(p0, "rowt", [1, 3, D])
            qkg = sb(p0, "qkg", [1, 1024])
            lamv = sb(p0, "lamv", [1, 256])
            subg = sb(p0, "subg", [1, 512])
            lt = sb(p0, "lt", [1, 16])
            wb = [sb(p0, f"wadab{i}", [128, 8, 256]) for i in range(2)]
            S.dma(sp, lambda h: h.dma_start(out=cv[:], in_=cvec_d), wr=[cv])
            S.dma(sp, lambda h: h.dma_start(out=bada[:], in_=bada_d), wr=[bada])
            S.dma(sp, lambda h: h.dma_start(out=normg[:], in_=normg_d), wr=[normg])
            S.dma(sp, lambda h: h.dma_start(out=qkg[:], in_=qkg_d), wr=[qkg])
            S.dma(sp, lambda h: h.dma_start(out=lamv[:], in_=lamv_d), wr=[lamv])
            S.dma(sp, lambda h: h.dma_start(out=subg[:], in_=subg_d), wr=[subg])
            S.op(act, lambda h: h.activation(out=scv[:], in_=cv[:], func=AF.Silu), rd=[cv], wr=[scv])
            wada_v = wada_d.rearrange("(kc p) n -> p kc n", p=128)
            CW = 256
            for nb in range(6 * D // CW):
                w = wb[nb % 2]
                S.dma(sp, lambda h: h.dma_start(out=w[:], in_=wada_v[:, :, nb * CW:(nb + 1) * CW]), wr=[w])
                for kc in range(8):
                    S.op(pe, lambda h: h.matmul(bank(0)[0:1, 0:CW], lhsT=scv[:, kc:kc + 1], rhs=w[:, kc, :],
                                                start=(kc == 0), stop=(kc == 7)),
                         rd=[scv, w], wr=[bankR[0]], self_sync=False)
                S.op(dve, lambda h: h.tensor_tensor(out=modrow[0:1, nb * CW:(nb + 1) * CW], in0=bank(0)[0:1, 0:CW],
                                                    in1=bada[0:1, nb * CW:(nb + 1) * CW], op=ALU.add),
                     rd=[bankR[0], bada], wr=[modrow])
                if nb < 2 * D // CW:
                    for kc in range(8):
                        S.op(pe, lambda h: h.matmul(bank(1)[0:1, 0:CW], lhsT=scv[:, 8 + kc:9 + kc], rhs=w[:, kc, :],
                                                    start=(kc == 0), stop=(kc == 7)),
                             rd=[scv, w], wr=[bankR[1]], self_sync=False)
                    S.op(dve, lambda h: h.tensor_tensor(out=modrow[0:1, 6144 + nb * CW:6144 + (nb + 1) * CW],
                                                        in0=bank(1)[0:1, 0:CW], in1=bada[0:1, nb * CW:(nb + 1) * CW],
                                                        op=ALU.add),
                         rd=[bankR[1], bada], wr=[modrow])
            if dbg:
                S.dma(sp, lambda h: h.dma_start(out=dbg_mod, in_=modrow[:]), rd=[modrow])

            def mrow(i):
                return modrow[0:1, i * D:(i + 1) * D]

            for ti, (si, go) in enumerate([(1, 0), (7, 0), (4, D)]):
                S.op(dve, lambda h: h.scalar_tensor_tensor(out=rowt[0:1, ti, :], in0=mrow(si), scalar=1.0,
                                                           in1=normg[0:1, go:go + D], op0=ALU.add, op1=ALU.mult),
                     rd=[modrow, normg], wr=[rowt])
            rows = {A1: rowt[0:1, 0, :], B1: mrow(0), A1C: rowt[0:1, 1, :], B1C: mrow(6), G1: mrow(2),
                    A2: rowt[0:1, 2, :], B2: mrow(3), G2: mrow(5)}
            cnt = 0
            for bi, row in rows.items():
                for hf in range(2):
                    b = 2 + cnt % 2
                    cnt += 1
                    S.op(pe, lambda h: h.matmul(bank(b), lhsT=ones_f[0:1, :], rhs=row[0:1, hf * 512:(hf + 1) * 512],
                                                start=True, stop=True),
                         rd=[cst, modrow, rowt], wr=[bankR[b]], self_sync=False)
                    bt_, bi_ = bcr(bi)
                    S.op(act, lambda h: h.copy(out=bt_[:, bi_, hf * 512:(hf + 1) * 512], in_=bank(b)),
                         rd=[bankR[b]], wr=[bt_])
            for hf in range(2):
                b = 2 + hf
                S.op(pe, lambda h: h.matmul(bank(b), lhsT=ones_f[0:1, :], rhs=qkg[0:1, hf * 512:(hf + 1) * 512],
                                            start=True, stop=True), rd=[cst, qkg], wr=[bankR[b]], self_sync=False)
                S.op(act, lambda h: h.mul(out=bcq[:, hf * 512:(hf + 1) * 512], in_=bank(b),
                                          mul=(0.125 if hf == 0 else 1.0)), rd=[bankR[b]], wr=[bcq])
            S.op(pe, lambda h: h.matmul(bank(2), lhsT=ones_f[0:1, :], rhs=subg[0:1, :], start=True, stop=True),
                 rd=[cst, subg], wr=[bankR[2]], self_sync=False)
            S.op(act, lambda h: h.mul(out=bcs[:], in_=bank(2), mul=1.0 - LAM_INIT), rd=[bankR[2]], wr=[bcs])
            S.op(dve, lambda h: h.tensor_tensor(out=lamv[0:1, 0:64], in0=lamv[0:1, 0:64], in1=lamv[0:1, 64:128],
                                                op=ALU.mult), rd=[lamv], wr=[lamv])
            S.op(dve, lambda h: h.tensor_tensor(out=lamv[0:1, 128:192], in0=lamv[0:1, 128:192],
                                                in1=lamv[0:1, 192:256], op=ALU.mult), rd=[lamv], wr=[lamv])
            S.op(dve, lambda h: h.reduce_sum(out=lt[0:1, 0:1], in_=lamv[0:1, 0:64], axis=AX.X), rd=[lamv], wr=[lt])
            S.op(dve, lambda h: h.reduce_sum(out=lt[0:1, 1:2], in_=lamv[0:1, 128:192], axis=AX.X), rd=[lamv], wr=[lt])
            S.op(act, lambda h: h.activation(out=lt[0:1, 2:4], in_=lt[0:1, 0:2], func=AF.Exp), rd=[lt], wr=[lt])
            S.op(dve, lambda h: h.tensor_tensor(out=lt[0:1, 4:5], in0=lt[0:1, 3:4], in1=lt[0:1, 2:3],
                                                op=ALU.subtract), rd=[lt], wr=[lt])
            S.op(dve, lambda h: h.tensor_scalar_add(out=lt[0:1, 4:5], in0=lt[0:1, 4:5], scalar1=-LAM_INIT),
                 rd=[lt], wr=[lt])
            S.op(dve, lambda h: h.reduce_max(out=lt[0:1, 6:7], in_=qkg[0:1, 0:64], axis=AX.X,
                                             apply_absolute_value=True), rd=[qkg], wr=[lt])
            S.op(dve, lambda h: h.reduce_max(out=lt[0:1, 7:8], in_=qkg[0:1, 512:576], axis=AX.X,
                                             apply_absolute_value=True), rd=[qkg], wr=[lt])
            S.op(dve, lambda h: h.tensor_tensor(out=lt[0:1, 5:6], in0=lt[0:1, 6:7], in1=lt[0:1, 7:8], op=ALU.mult),
                 rd=[lt], wr=[lt])
            S.op(dve, lambda h: h.tensor_scalar_mul(out=lt[0:1, 5:6], in0=lt[0:1, 5:6], scalar1=-8.0),
                 rd=[lt], wr=[lt])
            S.op(pe, lambda h: h.matmul(bank(3)[:, 0:2], lhsT=ones_f[0:1, :], rhs=lt[0:1, 4:6], start=True, stop=True),
                 rd=[cst, lt], wr=[bankR[3]], self_sync=False)
            S.op(act, lambda h: h.copy(out=sc2[:], in_=bank(3)[:, 0:2]), rd=[bankR[3]], wr=[sc2])
        S.barrier()
        if stop_after < 1:
            S.finish()
            return nc

        with ExitStack() as p1:
            hlT = [sb(p1, f"hlT{i}", [128, 8, 512], BF16) for i in range(2)]
            xb = [sb(p1, f"xb{i}", [128, D]) for i in range(4)]
            junk = sb(p1, "junk", [128, D], BF16)
            ss4 = [sb(p1, f"ss4{i}", [128, 4]) for i in range(2)]
            rs4 = [sb(p1, f"rs4{i}", [128, 4]) for i in range(2)]
            t1 = sb(p1, "t1_0", [128, D])
            hl = [sb(p1, f"hl{i}", [128, D], BF16) for i in range(2)]
            xlst = [sb(p1, f"xlst{i}", [128, 512]) for i in range(2)]
            gst = sb(p1, "gst0", [128, 4, 512], BF16)
            vst = [sb(p1, f"vst{i}", [128, 4, 4, 130], BF16) for i in range(2)]
            sq = [sb(p1, f"sq{i}", [128, D]) for i in range(2)]
            ss16 = [sb(p1, f"ss16{i}", [128, 16]) for i in range(2)]
            rs16 = [sb(p1, f"rs16{i}", [128, 16]) for i in range(2)]
            tq = [sb(p1, f"tq{i}", [128, D]) for i in range(2)]
            r1 = [sb(p1, f"r1{i}", [128, D]) for i in range(2)]
            qkr = [sb(p1, f"qkr{i}", [128, D], BF16) for i in range(2)]
            qkst = [sb(p1, f"qkst{i}", [128, 8, 512], BF16) for i in range(2)]
            for v in vst:
                S.op(pool, lambda h: h.memset(v[:], 1.0), wr=[v])

            def blk_tiles(blk):
                return [0, 1] if blk == 0 else [2 + 4 * (blk - 1) + i for i in range(4)]

            cnts = {"x": 0, "f": 0, "t": 0}

            hl_state = {}

            def hl_chain(blk, i):
                xts, r4 = hl_state[blk]
                ai, bi = (A1C, B1C) if blk == 0 else (A1, B1)
                xt, hh = xts[i], hl[i % 2]
                S.op(dve, lambda h: h.scalar_tensor_tensor(out=t1[:], in0=xt[:], scalar=r4[:, i:i + 1],
                                                           in1=bcA[:, ai, :], op0=ALU.mult, op1=ALU.mult),
                     rd=[xt, r4, bcA], wr=[t1])
                S.op(dve, lambda h: h.tensor_tensor(out=hh[:], in0=t1[:], in1=bcA[:, bi, :], op=ALU.add),
                     rd=[t1, bcA], wr=[hh])

            def hl_T(blk, i):
                hT, hh = hlT[blk % 2], hl[i % 2]
                for kc in range(8):
                    S.op(pe, lambda h: h.transpose(out=bank_bf(0)[:, kc * 128:(kc + 1) * 128],
                                                   in_=hh[:, kc * 128:(kc + 1) * 128], identity=ident_b),
                         rd=[hh, cstb], wr=[bankR[0]], self_sync=False)
                S.op(act, lambda h: h.copy(out=hT[:, :, i * 128:(i + 1) * 128],
                                           in_=bank_bf(0).rearrange("p (k t) -> p k t", t=128)),
                     rd=[bankR[0]], wr=[hT])

            def hl_front(blk):
                tiles = blk_tiles(blk)
                nt = len(tiles)
                s4, r4 = ss4[blk % 2], rs4[blk % 2]
                xts = []
                for i, T in enumerate(tiles):
                    xt = xb[cnts["x"] % 4]
                    cnts["x"] += 1
                    src = ctx_d[T * 128:(T + 1) * 128, :] if T < 2 else x_d[(T - 2) * 128:(T - 1) * 128, :]
                    S.dma(sp, lambda h: h.dma_start(out=xt[:], in_=src), wr=[xt])
                    S.op(act, lambda h: h.activation(out=junk[:], in_=xt[:], func=AF.Square,
                                                     accum_out=s4[:, i:i + 1]), rd=[xt], wr=[junk, s4])
                    xts.append(xt)
                rsqrt_ops(r4, s4, 1.0 / D, nt)
                hl_state[blk] = (xts, r4)
                for i in range(min(2, nt)):
                    hl_chain(blk, i)

            def hl_back(blk):
                nt = len(blk_tiles(blk))
                for i in range(nt):
                    if i >= 2:
                        hl_chain(blk, i)
                    hl_T(blk, i)

            def hlstage(blk):
                hl_front(blk)
                hl_back(blk)

            def fmstage(blk):
                tiles = blk_tiles(blk)
                W = 128 * len(tiles)
                toff = 0 if blk == 0 else CTX + (blk - 1) * 512
                loff = (blk - 1) * 512
                hT = hlT[blk % 2]
                for oc in range(4 if blk == 0 else 8):
                    b = 2 + cnts["f"] % 2
                    cnts["f"] += 1
                    for kc in range(8):
                        S.op(pe, lambda h: h.matmul(bank(b)[:, 0:W], lhsT=winb[:, kc, oc * 128:(oc + 1) * 128],
                                                    rhs=hT[:, kc, 0:W], start=(kc == 0), stop=(kc == 7)),
                             rd=[winb, hT], wr=[bankR[b]], self_sync=False)
                    if oc < 4:
                        st = xlst[oc % 2]
                        S.op(dve, lambda h: h.tensor_copy(out=st[:, 0:W], in_=bank(b)[:, 0:W]),
                             rd=[bankR[b]], wr=[st])
                        S.dma(sp, lambda h: h.dma_start(out=xl_d[oc, :, toff:toff + W], in_=st[:, 0:W]),
                              rd=[st], wr=[R_xl[oc]])
                    else:
                        S.op(act, lambda h: h.activation(out=gst[:, oc - 4, :], in_=bank(b), func=AF.Gelu_apprx_tanh),
                             rd=[bankR[b]], wr=[gst])
                if blk > 0:
                    S.dma(sp, lambda h: h.dma_start(out=gel_d.rearrange("c p t -> p c t")[:, :, loff:loff + 512],
                                                    in_=gst[:]), rd=[gst], wr=[R_gel])

            def stage_a(blk, i):
                T = blk_tiles(blk)[i]
                hT = hlT[blk % 2]
                vs = vst[blk % 2]
                g0 = 8 if blk == 0 else 0
                c0 = g0 * 64
                u = cnts["t"] % 2
                qb_ = 4 + 2 * u
                for (b, col) in ([(qb_, 1024)] if blk > 0 else []) + [(qb_ + 1, 1536), (1, 2048)]:
                    for kc in range(8):
                        S.op(pe, lambda h: h.matmul(bank(b), lhsT=hT[:, kc, i * 128:(i + 1) * 128],
                                                    rhs=winb[:, kc, col:col + 512], start=(kc == 0), stop=(kc == 7)),
                             rd=[winb, hT], wr=[bankR[b]], self_sync=False)
                S.op(act, lambda h: h.copy(out=vs[:, i, :, 0:128],
                                           in_=bank(1).rearrange("p (a e) -> p a e", e=128)),
                     rd=[bankR[1]], wr=[vs])
                pqk = PS[:, qb_:qb_ + 2, :].rearrange("p a n -> p (a n)")
                S.op(act, lambda h: h.activation(out=sq[u][:, c0:], in_=pqk[:, c0:], func=AF.Square),
                     rd=[bankR[qb_], bankR[qb_ + 1]], wr=[sq[u]])
                cnts["t"] += 1
                return u

            def stage_a2(blk, i, u):
                g0 = 8 if blk == 0 else 0
                c0 = g0 * 64
                qb_ = 4 + 2 * u
                pqk = PS[:, qb_:qb_ + 2, :].rearrange("p a n -> p (a n)")
                S.op(dve, lambda h: h.tensor_reduce(out=ss16[u][:, g0:], in_=sq[u][:, c0:].rearrange("p (g d) -> p g d", d=64),
                                                    axis=AX.X, op=ALU.add), rd=[sq[u]], wr=[ss16[u]])
                rsqrt_ops(rs16[u], ss16[u], 1.0 / 64, 16, g0)
                S.op(dve, lambda h: h.tensor_tensor(
                    out=tq[u][:, c0:].rearrange("p (g d) -> p g d", d=64),
                    in0=pqk[:, c0:].rearrange("p (g d) -> p g d", d=64),
                    in1=rs16[u][:, g0:].unsqueeze(2).broadcast_to([128, 16 - g0, 64]), op=ALU.mult),
                    rd=[bankR[qb_], bankR[qb_ + 1], rs16[u]], wr=[tq[u]])
                S.op(dve, lambda h: h.tensor_tensor(out=tq[u][:, c0:], in0=tq[u][:, c0:], in1=bcq[:, c0:], op=ALU.mult),
                     rd=[tq[u], bcq], wr=[tq[u]])

            def stage_b(blk, i, u):
                T = blk_tiles(blk)[i]
                qs = qkst[blk % 2]
                g0 = 8 if blk == 0 else 0
                c0 = g0 * 64
                ng = 16 - g0
                r2 = sq[u]
                S.op(pool, lambda h: h.tensor_tensor(
                    out=r1[u][:, c0:].rearrange("p (g d) -> p g d", d=64),
                    in0=tq[u][:, c0:].rearrange("p (g d) -> p g d", d=64),
                    in1=ropeT[:, T, 0, :].unsqueeze(1).broadcast_to([128, ng, 64]), op=ALU.mult),
                    rd=[tq[u], ropeT], wr=[r1[u]])
                tq5 = tq[u][:, c0:].rearrange("p (g t h w) -> p g t h w", t=2, h=2, w=16)
                r25 = r2[:, c0:].rearrange("p (g t h w) -> p g t h w", t=2, h=2, w=16)
                sn4 = ropeT[:, T, 1, :].rearrange("p (t h w) -> p t h w", t=2, h=2)
                for hv in range(2):
                    S.op(pool, lambda h: h.tensor_tensor(
                        out=r25[:, :, :, hv, :], in0=tq5[:, :, :, 1 - hv, :],
                        in1=sn4[:, :, hv, :].unsqueeze(1).broadcast_to([128, ng, 2, 16]), op=ALU.mult),
                        rd=[tq[u], ropeT], wr=[r2])

            def stage_b2(blk, i, u):
                qs = qkst[blk % 2]
                g0 = 8 if blk == 0 else 0
                c0 = g0 * 64
                r2 = sq[u]
                qq = qkr[u]
                S.op(pool, lambda h: h.tensor_tensor(out=qq[:, c0:], in0=r1[u][:, c0:], in1=r2[:, c0:], op=ALU.add),
                     rd=[r1[u], r2], wr=[qq])
                k0 = g0 // 2
                for kc in range(k0, 8):
                    S.op(pe, lambda h: h.transpose(out=bank_bf(0)[:, kc * 128:(kc + 1) * 128],
                                                   in_=qq[:, kc * 128:(kc + 1) * 128], identity=ident_b),
                         rd=[qq, cstb], wr=[bankR[0]], self_sync=False)
                S.op(act, lambda h: h.copy(out=qs[:, k0:8, i * 128:(i + 1) * 128],
                                           in_=bank_bf(0).rearrange("p (k t) -> p k t", t=128)[:, k0:8, :]),
                     rd=[bankR[0]], wr=[qs])

            def outstage(blk):
                tiles = blk_tiles(blk)
                nt = len(tiles)
                W = 128 * nt
                toff = 0 if blk == 0 else CTX + (blk - 1) * 512
                loff = (blk - 1) * 512
                qs = qkst[blk % 2]
                vs = vst[blk % 2]
                if blk > 0:
                    S.dma(sp, lambda h: h.dma_start(out=qT_d.rearrange("c p t -> p c t")[:, :, loff:loff + 512],
                                                    in_=qs[:, 0:4, :]), rd=[qs], wr=[R_qT])
                S.dma(sp, lambda h: h.dma_start(out=kT_d.rearrange("c p t -> p c t")[:, :, toff:toff + W],
                                                in_=qs[:, 4:8, 0:W]), rd=[qs], wr=[R_kT])
                S.dma(sp, lambda h: h.dma_start(
                    out=v_d[tiles[0]:tiles[0] + nt, :, :].rearrange("t p f -> p t f"),
                    in_=vs[:, 0:nt, :, :].rearrange("p t a e -> p t (a e)")), rd=[vs], wr=[R_v])

            flat = [(blk, i) for blk in range(9) for i in range(len(blk_tiles(blk)))]
            hlstage(0)
            fmstage(0)
            hlstage(1)
            prev = None
            for (blk, i) in flat:
                u = stage_a(blk, i)
                last = (i == len(blk_tiles(blk)) - 1)
                if i == len(blk_tiles(blk)) - 2 and blk + 2 < 9:
                    hl_front(blk + 2)
                if last and blk + 1 < 9:
                    fmstage(blk + 1)
                    if blk + 2 < 9:
                        hl_back(blk + 2)
                if prev is not None:
                    stage_b(*prev)
                stage_a2(blk, i, u)
                if prev is not None:
                    stage_b2(*prev)
                    if prev[1] == len(blk_tiles(prev[0])) - 1:
                        outstage(prev[0])
                prev = (blk, i, u)
            stage_b(*prev)
            stage_b2(*prev)
            outstage(prev[0])
        p01.close()
        S.barrier()
        if stop_after < 2:
            S.finish()
            return nc


        with ExitStack() as p2:
            TT = CTX + SEQ
            HL = SEQ // 2
            convw = sb(p2, "convw", [128, 16])
            convb = sb(p2, "convb", [128, 4])
            lrub = sb(p2, "lrub", [128, 16])
            lrul = sb(p2, "lrul", [128, 8])
            cL = sb(p2, "cL", [128, 8])
            cL2 = sb(p2, "cL2", [128, 8])
            onesT = sb(p2, "onesT", [128, 1])
            lruwb = sb(p2, "lruwb", [128, 16, 128], BF16)
            XP = sb(p2, "XP", [128, TT + 8])
            xcs = [sb(p2, f"xc{i}", [128, TT]) for i in range(2)]
            xcbs = [sb(p2, f"xcb{i}", [128, TT], BF16) for i in range(2)]
            Rs = [sb(p2, f"Rr{i}", [128, HL]) for i in range(2)]
            As = [sb(p2, f"A2_{i}", [128, HL]) for i in range(2)]
            Is = [sb(p2, f"Ii{i}", [128, HL]) for i in range(2)]
            Hf = sb(p2, "Hf", [128, TT])
            Hb = sb(p2, "Hb", [128, TT])
            gl = sb(p2, "gl", [128, SEQ], BF16)
            lst = sb(p2, "lst", [128, SEQ], BF16)
            S.dma(sp, lambda h: h.dma_start(out=convw[:], in_=convw_d), wr=[convw])
            S.dma(sp, lambda h: h.dma_start(out=convb[:], in_=convb_d), wr=[convb])
            S.dma(sp, lambda h: h.dma_start(out=lrub[:], in_=lrub_d), wr=[lrub])
            S.dma(sp, lambda h: h.dma_start(out=lrul[:], in_=lrul_d), wr=[lrul])
            S.dma(pool, lambda h: h.dma_start(out=lruwb[:], in_=lruw_d.rearrange("p (a n) -> p a n", n=128)),
                  wr=[lruwb])
            S.op(act, lambda h: h.activation(out=cL[:], in_=lrul[:], func=AF.Exp, scale=-1.0), rd=[lrul], wr=[cL])
            S.op(act, lambda h: h.activation(out=cL[:], in_=cL[:], func=AF.Ln, bias=1.0), rd=[cL], wr=[cL])
            S.op(dve, lambda h: h.tensor_scalar_mul(out=cL2[:], in0=cL[:], scalar1=-16.0), rd=[cL], wr=[cL2])
            S.op(dve, lambda h: h.tensor_scalar_mul(out=cL[:], in0=cL[:], scalar1=-8.0), rd=[cL, cL2], wr=[cL])
            S.op(dve, lambda h: h.memset(onesT[:], 1.0), wr=[onesT])
            S.op(dve, lambda h: h.memset(XP[:], 0.0), wr=[XP])
            segs = [(1, 0, CTX), (260, CTX, SEQ)]

            def conv(j):
                xc, xcb = xcs[j % 2], xcbs[j % 2]
                S.dma(sp, lambda h: h.dma_start(out=XP[:, 1:1 + CTX], in_=xl_d[j, :, 0:CTX]), rd=[R_xl[j]], wr=[XP])
                S.dma(sp, lambda h: h.dma_start(out=XP[:, 260:260 + SEQ], in_=xl_d[j, :, CTX:TT]),
                      rd=[R_xl[j]], wr=[XP])
                for (xo, to, ln) in segs:
                    S.op(dve, lambda h: h.tensor_scalar(out=xc[:, to:to + ln], in0=XP[:, xo - 1:xo - 1 + ln],
                                                         scalar1=convw[:, j * 4:j * 4 + 1], scalar2=convb[:, j:j + 1],
                                                         op0=ALU.mult, op1=ALU.add),
                         rd=[XP, convw, convb], wr=[xc])
                    for k in range(1, 4):
                        S.op(dve, lambda h: h.scalar_tensor_tensor(
                            out=xc[:, to:to + ln], in0=XP[:, xo - 1 + k:xo - 1 + k + ln],
                            scalar=convw[:, j * 4 + k:j * 4 + k + 1], in1=xc[:, to:to + ln],
                            op0=ALU.mult, op1=ALU.add), rd=[XP, convw, xc], wr=[xc])
                S.op(dve, lambda h: h.tensor_copy(out=xcb[:], in_=xc[:]), rd=[xc], wr=[xcb])

            pc = {"n": 0, "b": 0}

            def piece(j, d, t0, ln, first_col, init, rev):
                xc, xcb = xcs[j % 2], xcbs[j % 2]
                s_ = pc["n"] % 2
                pc["n"] += 1
                Rr, A2_, Ii = Rs[s_], As[s_], Is[s_]
                H = Hf if d == 0 else Hb
                for o in range(0, ln, 512):
                    w_ = min(512, ln - o)
                    for gi, dst in enumerate([Rr, Ii]):
                        b = 2 + pc["b"] % 4
                        pc["b"] += 1
                        S.op(pe, lambda h: h.matmul(bank(b)[:, 0:w_], lhsT=lruwb[:, (d * 2 + gi) * 4 + j, :],
                                                    rhs=xcb[:, t0 + o:t0 + o + w_], start=True, stop=True),
                             rd=[lruwb, xcb], wr=[bankR[b]], self_sync=False)
                        bi_ = (d * 2 + gi) * 4 + j
                        S.op(act, lambda h: h.activation(out=dst[:, o:o + w_], in_=bank(b)[:, 0:w_],
                                                         func=AF.Sigmoid, bias=lrub[:, bi_:bi_ + 1]),
                             rd=[bankR[b], lrub], wr=[dst])
                ci = d * 4 + j
                S.op(act, lambda h: h.activation(out=Rr[:, 0:ln], in_=Rr[:, 0:ln], func=AF.Exp, scale=cL[:, ci:ci + 1]),
                     rd=[Rr, cL], wr=[Rr])
                S.op(dve, lambda h: h.scalar_tensor_tensor(out=A2_[:, 0:ln], in0=Rr[:, 0:ln], scalar=1.0, in1=Rr[:, 0:ln],
                                                           op0=ALU.min, op1=ALU.mult), rd=[Rr], wr=[A2_])
                S.op(act, lambda h: h.activation(out=A2_[:, 0:ln], in_=A2_[:, 0:ln], func=AF.Sqrt, scale=-1.0,
                                                 bias=onesT[:, 0:1]), rd=[A2_, onesT], wr=[A2_])
                if first_col is not None:
                    S.op(dve, lambda h: h.memset(A2_[:, first_col:first_col + 1], 1.0), wr=[A2_])
                S.op(dve, lambda h: h.tensor_tensor(out=Ii[:, 0:ln], in0=Ii[:, 0:ln], in1=A2_[:, 0:ln], op=ALU.mult),
                     rd=[Ii, A2_], wr=[Ii])
                S.op(dve, lambda h: h.tensor_tensor(out=Ii[:, 0:ln], in0=Ii[:, 0:ln], in1=xc[:, t0:t0 + ln], op=ALU.mult),
                     rd=[Ii, xc], wr=[Ii])
                hv, av, uv = H[:, t0:t0 + ln], Rr[:, 0:ln], Ii[:, 0:ln]
                if rev:
                    hv, av, uv = hv[:, ::-1], av[:, ::-1], uv[:, ::-1]
                S.op(dve, lambda h: h.tensor_tensor_scan(out=hv, data0=av, data1=uv, initial=init,
                                                         op0=ALU.mult, op1=ALU.add), rd=[Rr, Ii, H], wr=[H])

            conv(0)
            for j in range(4):
                S.dma(sp, lambda h: h.dma_start(out=gl[:], in_=gel_d[j, :, :]), rd=[R_gel], wr=[gl])
                if j + 1 < 4:
                    conv(j + 1)
                piece(j, 0, 0, CTX, 0, 0.0, False)
                piece(j, 0, CTX, HL, None, Hf[:, CTX - 1:CTX], False)
                piece(j, 0, CTX + HL, HL, None, Hf[:, CTX + HL - 1:CTX + HL], False)
                piece(j, 1, 0, CTX, CTX - 1, 0.0, True)
                piece(j, 1, CTX + HL, HL, None, Hb[:, 0:1], True)
                piece(j, 1, CTX, HL, None, Hb[:, CTX + HL:CTX + HL + 1], True)
                S.op(dve, lambda h: h.tensor_tensor(out=Hf[:, CTX:TT], in0=Hf[:, CTX:TT], in1=Hb[:, CTX:TT], op=ALU.add),
                     rd=[Hf, Hb], wr=[Hf])
                S.op(dve, lambda h: h.tensor_tensor(out=lst[:], in0=Hf[:, CTX:TT], in1=gl[:], op=ALU.mult),
                     rd=[Hf, gl], wr=[lst])
                S.dma(sp, lambda h: h.dma_start(out=lru_d[j, :, :], in_=lst[:]), rd=[lst], wr=[R_lru])
        S.barrier()
        if stop_after < 3:
            S.finish()
            return nc


        with ExitStack() as p3:
            kT = sb(p3, "kT", [128, 4, CTX + SEQ], BF16)
            v1 = sb(p3, "v1", [128, NT, 520], BF16)
            woutb = sb(p3, "woutb", [128, 8, D], BF16)
            wrt = sb(p3, "wrt", [128, 8, NE])
            qz = [[sb(p3, f"qz{i}_{c}", [128, 4, 512], BF16) for c in range(2)] for i in range(2)]
            for i in range(2):
                for c in range(2):
                    S.op(pool, lambda h: h.memset(qz[i][c][:], 0.0), wr=[qz[i][c]])
            lruB = [sb(p3, f"lruB{i}", [128, 4, 512], BF16) for i in range(2)]
            Eb = [sb(p3, f"Eb{i}", [128, 1024], BF16) for i in range(3)]
            osb = [sb(p3, f"osb{i}", [128, 4, 128]) for i in range(2)]
            rl = sb(p3, "rl", [128, 8])
            ssn = sb(p3, "ssn", [128, 8])
            rsn = sb(p3, "rsn", [128, 8])
            junk3 = sb(p3, "junk3", [128, D], BF16)
            att = sb(p3, "att", [128, 4, 512], BF16)
            attT = sb(p3, "attT", [128, 4, 512], BF16)
            xres = [sb(p3, f"xres{i}", [128, D]) for i in range(2)]
            x1t = [sb(p3, f"x1t{i}", [128, D]) for i in range(2)]
            tmp3s = [sb(p3, f"tmp3{i}", [128, D]) for i in range(2)]
            h2fs = [sb(p3, f"h2f{i}", [128, D]) for i in range(2)]
            h2b = [sb(p3, f"h2b{i}", [128, D], BF16) for i in range(2)]
            h2Ts = [sb(p3, f"h2T{i}", [128, 8, 128]) for i in range(2)]
            lg = sb(p3, "lg", [128, NE])
            mx = sb(p3, "mx", [128, 4])
            S.dma(sp, lambda h: h.dma_start(out=kT[:], in_=kT_d.rearrange("c p t -> p c t")), rd=[R_kT], wr=[kT])
            S.dma(sp, lambda h: h.dma_start(out=v1[:], in_=v_d.rearrange("t p f -> p t f")), rd=[R_v], wr=[v1])
            S.dma(sp, lambda h: h.dma_start(out=wrt[:], in_=wr_d.rearrange("(kc p) n -> p kc n", p=128)), wr=[wrt])
            for kc in range(8):
                S.dma(pool, lambda h: h.dma_start(out=woutb[:, kc, :], in_=wout_d[kc * 128:(kc + 1) * 128, :]),
                      wr=[woutb])
            ssn2 = sb(p3, "ssn2", [128, 2])
            rsn2 = sb(p3, "rsn2", [128, 2])
            junk4 = sb(p3, "junk4", [128, D], BF16)

            def tail_steps(qb, lb, B0, B1):
                steps = []
                for s_ in range(4):
                    def st_t(s_=s_):
                        for hd in range(4):
                            S.op(pe, lambda h: h.transpose(out=bank_bf(B0)[:, hd * 128:(hd + 1) * 128],
                                                           in_=att[:, s_, hd * 128:(hd + 1) * 128], identity=ident_b),
                                 rd=[att, cstb], wr=[bankR[B0]], self_sync=False)
                        S.op(act, lambda h: h.copy(out=attT[:, :, s_ * 128:(s_ + 1) * 128],
                                                   in_=bank_bf(B0)[:, 0:512].rearrange("p (k t) -> p k t", t=128)),
                             rd=[bankR[B0]], wr=[attT])
                    steps.append(st_t)
                per_tile = []
                for s_ in range(4):
                    tok0 = qb * 512 + s_ * 128
                    tl = qb * 4 + s_
                    xr, x1 = xres[s_ % 2], x1t[s_ % 2]
                    hb_ = h2b[s_ % 2]
                    tmp3, h2f, h2T = tmp3s[s_ % 2], h2fs[s_ % 2], h2Ts[s_ % 2]

                    def st_w(fh, s_=s_, tok0=tok0, xr=xr, tmp3=tmp3):
                        bk = B0 if fh == 0 else B1
                        if fh == 0:
                            S.dma(sp, lambda h: h.dma_start(out=xr[:], in_=x_d[tok0:tok0 + 128, :]), wr=[xr])
                        for kc in range(8):
                            lhs = (lb[:, kc, s_ * 128:(s_ + 1) * 128] if kc < 4 else attT[:, kc - 4, s_ * 128:(s_ + 1) * 128])
                            S.op(pe, lambda h: h.matmul(bank(bk), lhsT=lhs, rhs=woutb[:, kc, fh * 512:(fh + 1) * 512],
                                                        start=(kc == 0), stop=(kc == 7)),
                                 rd=[lb, attT, woutb], wr=[bankR[bk]], self_sync=False)
                        S.op(dve, lambda h: h.tensor_tensor(out=tmp3[:, fh * 512:(fh + 1) * 512], in0=bank(bk),
                                                            in1=bcG[:, 0, fh * 512:(fh + 1) * 512], op=ALU.mult),
                             rd=[bankR[bk], bcG], wr=[tmp3])
                    tile_steps = {}
                    tile_steps["w0"] = (lambda st_w=st_w: st_w(0))

                    cc = s_ % 2

                    def st_c1(st_w=st_w, tok0=tok0, xr=xr, x1=x1, tmp3=tmp3, cc=cc):
                        st_w(1)
                        S.op(dve, lambda h: h.tensor_tensor(out=x1[:], in0=tmp3[:], in1=xr[:], op=ALU.add),
                             rd=[tmp3, xr], wr=[x1])
                        S.dma(sp, lambda h: h.dma_start(out=out_d[tok0:tok0 + 128, :], in_=x1[:]), rd=[x1], wr=[R_out])
                        S.op(act, lambda h: h.activation(out=junk4[:], in_=x1[:], func=AF.Square, accum_out=ssn2[:, cc:cc + 1]),
                             rd=[x1], wr=[junk4, ssn2])
                        S.op(act, lambda h: h.activation(out=rsn2[:, cc:cc + 1], in_=ssn2[:, cc:cc + 1], func=AF.Ln, scale=1.0 / D,
                                                         bias=epsT[:, 0:1]), rd=[ssn2, epsT], wr=[rsn2])
                        S.op(act, lambda h: h.activation(out=rsn2[:, cc:cc + 1], in_=rsn2[:, cc:cc + 1], func=AF.Exp, scale=-0.5),
                             rd=[rsn2], wr=[rsn2])
                    tile_steps["c1"] = st_c1

                    def st_c2(tok0=tok0, x1=x1, hb_=hb_, tmp3=tmp3, h2f=h2f, cc=cc):
                        S.op(dve, lambda h: h.scalar_tensor_tensor(out=tmp3[:], in0=x1[:], scalar=rsn2[:, cc:cc + 1], in1=bcG[:, 1, :],
                                                                   op0=ALU.mult, op1=ALU.mult), rd=[x1, rsn2, bcG], wr=[tmp3])
                        S.op(dve, lambda h: h.tensor_tensor(out=h2f[:], in0=tmp3[:], in1=bcG[:, 2, :], op=ALU.add),
                             rd=[tmp3, bcG], wr=[h2f])
                        S.op(act, lambda h: h.copy(out=hb_[:], in_=h2f[:]), rd=[h2f], wr=[hb_])
                        S.dma(sp, lambda h: h.dma_start(out=h2_d[tok0:tok0 + 128, :], in_=hb_[:]), rd=[hb_], wr=[R_h2])
                    tile_steps["c2"] = st_c2

                    def st_r(half, h2f=h2f, h2T=h2T):
                        for k4 in range(4):
                            kc = half * 4 + k4
                            S.op(pe, lambda h: h.transpose(out=bank(B0)[:, k4 * 128:(k4 + 1) * 128],
                                                           in_=h2f[:, kc * 128:(kc + 1) * 128], identity=ident_f),
                                 rd=[h2f, cst], wr=[bankR[B0]], self_sync=False)
                        S.op(dve, lambda h: h.tensor_copy(out=h2T[:, half * 4:half * 4 + 4, :],
                                                          in_=bank(B0).rearrange("p (k t) -> p k t", t=128)),
                             rd=[bankR[B0]], wr=[h2T])
                    tile_steps["r0"] = (lambda st_r=st_r: st_r(0))
                    tile_steps["r1"] = (lambda st_r=st_r: st_r(1))

                    def st_s(tl=tl, h2T=h2T):
                        for kc in range(8):
                            S.op(pe, lambda h: h.matmul(bank(B0)[:, 0:NE], lhsT=h2T[:, kc, :], rhs=wrt[:, kc, :],
                                                        start=(kc == 0), stop=(kc == 7)),
                                 rd=[h2T, wrt], wr=[bankR[B0]], self_sync=False)
                        S.op(dve, lambda h: h.reduce_max(out=mx[:, 0:1], in_=bank(B0)[:, 0:NE], axis=AX.X),
                             rd=[bankR[B0]], wr=[mx])
                        S.op(dve, lambda h: h.tensor_scalar_mul(out=mx[:, 1:2], in0=mx[:, 0:1], scalar1=-1.0), rd=[mx], wr=[mx])
                        S.op(act, lambda h: h.activation(out=lg[:], in_=bank(B0)[:, 0:NE], func=AF.Exp, bias=mx[:, 1:2],
                                                         accum_out=mx[:, 2:3]), rd=[bankR[B0], mx], wr=[lg, mx])
                        S.op(dve, lambda h: h.reciprocal(out=mx[:, 3:4], in_=mx[:, 2:3]), rd=[mx], wr=[mx])
                        S.op(dve, lambda h: h.tensor_scalar_mul(out=aff[:, tl, :], in0=lg[:], scalar1=mx[:, 3:4]),
                             rd=[lg, mx], wr=[aff])
                    tile_steps["sm"] = st_s
                    per_tile.append(tile_steps)
                order = [("w0", 0), ("c1", 0), ("w0", 1), ("c1", 1), ("c2", 0), ("r0", 0), ("c2", 1), ("r1", 0),
                         ("w0", 2), ("c1", 2), ("sm", 0), ("r0", 1), ("r1", 1), ("c2", 2), ("w0", 3), ("c1", 3), ("sm", 1),
                         ("r0", 2), ("r1", 2), ("c2", 3), ("sm", 2), ("r0", 3), ("r1", 3), ("sm", 3)]
                for (k_, t_) in order:
                    steps.append(per_tile[t_][k_])
                return steps

            pending = []
            deferred = []
            gcnt = {"g": 0}
            for qb in range(8):
                qt = qz[qb % 2]
                for c in range(2):
                    S.dma(sp, lambda h: h.dma_start(
                        out=qt[c][c * 64:(c + 1) * 64, :, :],
                        in_=qT_d.rearrange("c p t -> p c t")[c * 64:(c + 1) * 64, :, qb * 512:(qb + 1) * 512]),
                        rd=[R_qT], wr=[qt[c]])
                lb = lruB[qb % 2]
                S.dma(sp, lambda h: h.dma_start(out=lb[:], in_=lru_d.rearrange("c p t -> p c t")[:, :, qb * 512:(qb + 1) * 512]),
                      rd=[R_lru], wr=[lb])
                NP = NT // 2
                items = [(hd, c, kp) for hd in range(4) for c in range(2) for kp in range(NP)]

                def emit_S(i):
                    hd, c, kp = items[i]
                    pb = (i % 2) * 2
                    for u in range(2):
                        kt = kp * 2 + u
                        S.op(pe, lambda h: h.matmul(bank(pb + u), lhsT=kT[:, hd, kt * 128:(kt + 1) * 128],
                                                    rhs=qt[c][:, hd, :], start=True, stop=True),
                             rd=[kT, qt[c]], wr=[bankR[pb + u]], self_sync=False)

                emit_S(0)
                emit_S(1)
                for i, (hd, c, kp) in enumerate(items):
                    os_ = osb[hd % 2]
                    pb = (i % 2) * 2
                    ab = 4 + 2 * ((hd * 2 + c) % 2)
                    E = Eb[i % 3]
                    S.op(act, lambda h: h.activation(out=E[:], in_=PS[:, pb:pb + 2, :].rearrange("p a n -> p (a n)"),
                                                     func=AF.Exp, bias=sc2[:, 1:2]),
                         rd=[bankR[pb], bankR[pb + 1], sc2], wr=[E])
                    if i + 2 < len(items):
                        emit_S(i + 2)
                    for u in range(2):
                        kt = kp * 2 + u
                        for s_ in range(4):
                            bb = ab + s_ // 2
                            co = (s_ % 2) * 256
                            S.op(pe, lambda h: h.matmul(bank(bb)[:, co:co + 129],
                                                        lhsT=E[:, u * 512 + s_ * 128:u * 512 + (s_ + 1) * 128],
                                                        rhs=v1[:, kt, hd * 130:hd * 130 + 129],
                                                        start=(kt == 0 and s_ % 2 == 0), stop=(kt == NT - 1),
                                                        skip_group_check=True),
                                 rd=[E, v1], wr=[bankR[bb]], self_sync=False)
                    gcnt["g"] += 1
                    while deferred and deferred[0][0] <= gcnt["g"]:
                        deferred.pop(0)[1]()
                    if not deferred and i < NP - 1:
                        for _ in range(2):
                            if pending:
                                pending.pop(0)()
                    elif i == NP - 1:
                        while deferred:
                            deferred.pop(0)[1]()
                        while pending:
                            pending.pop(0)()
                    if kp < NP - 1:
                        continue
                    for s_ in range(4):
                        bb = ab + s_ // 2
                        co = (s_ % 2) * 256
                        S.op(dve, lambda h: h.reciprocal(out=rl[:, s_:s_ + 1], in_=bank(bb)[:, co + 128:co + 129]),
                             rd=[bankR[bb]], wr=[rl])
                        if c == 0:
                            S.op(dve, lambda h: h.tensor_scalar_mul(out=os_[:, s_, :], in0=bank(bb)[:, co:co + 128],
                                                                    scalar1=rl[:, s_:s_ + 1]),
                                 rd=[bankR[bb], rl], wr=[os_])
                        else:
                            S.op(dve, lambda h: h.tensor_tensor(out=rl[:, 4 + s_:5 + s_], in0=rl[:, s_:s_ + 1],
                                                                in1=sc2[:, 0:1], op=ALU.mult), rd=[rl, sc2], wr=[rl])
                            S.op(dve, lambda h: h.scalar_tensor_tensor(out=os_[:, s_, :], in0=bank(bb)[:, co:co + 128],
                                                                       scalar=rl[:, 4 + s_:5 + s_], in1=os_[:, s_, :],
                                                                       op0=ALU.mult, op1=ALU.add),
                                 rd=[bankR[bb], rl, os_], wr=[os_])
                    if c == 0:
                        continue
                    def sub_sq(s_, os_=os_):
                        S.op(act, lambda h: h.activation(out=junk3[:, 0:128], in_=os_[:, s_, :], func=AF.Square,
                                                         accum_out=ssn[:, s_:s_ + 1]), rd=[os_], wr=[junk3, ssn])

                    def sub_fin(hd=hd, os_=os_):
                        S.op(act, lambda h: h.activation(out=rsn[:, 0:4], in_=ssn[:, 0:4], func=AF.Ln, scale=1.0 / 128,
                                                         bias=epsT[:, 0:1]), rd=[ssn, epsT], wr=[rsn])
                        S.op(act, lambda h: h.activation(out=rsn[:, 0:4], in_=rsn[:, 0:4], func=AF.Exp, scale=-0.5),
                             rd=[rsn], wr=[rsn])
                        for s_ in range(4):
                            S.op(dve, lambda h: h.scalar_tensor_tensor(out=att[:, s_, hd * 128:(hd + 1) * 128], in0=os_[:, s_, :],
                                                                       scalar=rsn[:, s_:s_ + 1], in1=bcs[:, hd * 128:(hd + 1) * 128],
                                                                       op0=ALU.mult, op1=ALU.mult),
                                 rd=[os_, rsn, bcs], wr=[att])
                    for s_ in range(4):
                        deferred.append((gcnt["g"] + 2 + s_ // 2, (lambda sub_sq=sub_sq, s_=s_: sub_sq(s_))))
                    deferred.append((gcnt["g"] + 4, sub_fin))
                pending = tail_steps(qb, lb, 6, 7)
            while deferred:
                deferred.pop(0)[1]()
            for st in pending:
                st()
            if dbg:
                S.dma(sp, lambda h: h.dma_start(out=dbg_aff, in_=aff[:].rearrange("p t e -> p (t e)")), rd=[aff])
        p03.close()
        S.barrier()
        if stop_after < 4:
            S.finish()
            return nc

        p45 = top.enter_context(ExitStack())
        NGU = 4
        NB = 11
        wgb = [sb(p45, f"wgb{i}", [128, 8, 256], BF16) for i in range(NGU)]
        wub = [sb(p45, f"wub{i}", [128, 8, 256], BF16) for i in range(NGU)]
        wdb = [sb(p45, f"wdb{i}", [128, NJ, 512], BF16) for i in range(2)]

        def load_gu(e, b):
            i = (e * NB + b) % NGU
            for (dst, srcw) in ((wgb[i], wg_d), (wub[i], wu_d)):
                S.dma(pool, lambda h: h.dma_start(
                    out=dst[:], in_=srcw[e].rearrange("(kc p) n -> p kc n", p=128)[:, :, b * 256:(b + 1) * 256]),
                    wr=[dst])

        def load_d(e, fh):
            dst = wdb[fh]
            S.dma(pool, lambda h: h.dma_start(
                out=dst[:], in_=wd_d[e].rearrange("(j p) f -> p j f", p=128)[:, :, fh * 512:(fh + 1) * 512]),
                wr=[dst])

        if stop_after >= 5:
            for b_ in range(NGU):
                load_gu(0, b_)
            load_d(0, 0)
            load_d(0, 1)

        with ExitStack() as p4:
            lo = sb(p4, "lo", [128, NE])
            hi = sb(p4, "hi", [128, NE])
            mid = sb(p4, "mid", [128, NE])
            ta = sb(p4, "ta", [128, NE])
            cmpb = sb(p4, "cmpb", [128, 32, NE], BF16)
            maskb = sb(p4, "maskb", [128, 32, NE], BF16)
            cum = sb(p4, "cum", [128, 32, NE], BF16)
            posm = sb(p4, "posm", [128, 32, NE])
            TI = sb(p4, "TI", [128, NE, 32, 8], BF16)
            rres = sb(p4, "rres", [128, 32, NE])
            Sb = [sb(p4, f"Sb{i}", [128, 512], BF16) for i in range(4)]
            idxf = sb(p4, "idxf", [128, 4])
            pvs = sb(p4, "pvs", [128, 32])
            S.op(dve, lambda h: h.memset(lo[:], 0.0), wr=[lo])
            S.op(dve, lambda h: h.memset(hi[:], 1.0), wr=[hi])
            S.op(dve, lambda h: h.memset(mid[:], 0.5), wr=[mid])
            for it in range(32):
                S.op(dve, lambda h: h.tensor_tensor(out=cmpb[:], in0=aff[:],
                                                    in1=mid[:].unsqueeze(1).broadcast_to([128, 32, NE]), op=ALU.is_ge),
                     rd=[aff, mid], wr=[cmpb])
                for t in range(32):
                    S.op(pe, lambda h: h.matmul(bank(0)[:, 0:NE], lhsT=ones_b, rhs=cmpb[:, t, :],
                                                start=(t == 0), stop=(t == 31)),
                         rd=[cmpb, cstb], wr=[bankR[0]], self_sync=False)
                S.op(dve, lambda h: h.scalar_tensor_tensor(out=ta[:], in0=bank(0)[:, 0:NE], scalar=float(CAP) - 0.5,
                                                           in1=mid[:], op0=ALU.is_ge, op1=ALU.mult),
                     rd=[bankR[0], mid], wr=[ta])
                S.op(dve, lambda h: h.tensor_tensor(out=lo[:], in0=lo[:], in1=ta[:], op=ALU.max), rd=[lo, ta], wr=[lo])
                S.op(dve, lambda h: h.scalar_tensor_tensor(out=ta[:], in0=bank(0)[:, 0:NE], scalar=float(CAP) - 0.5,
                                                           in1=mid[:], op0=ALU.is_ge, op1=ALU.add),
                     rd=[bankR[0], mid], wr=[ta])
                S.op(dve, lambda h: h.tensor_tensor(out=hi[:], in0=hi[:], in1=ta[:], op=ALU.min), rd=[hi, ta], wr=[hi])
                S.op(dve, lambda h: h.tensor_tensor(out=mid[:], in0=lo[:], in1=hi[:], op=ALU.add), rd=[lo, hi], wr=[mid])
                S.op(dve, lambda h: h.tensor_scalar_mul(out=mid[:], in0=mid[:], scalar1=0.5), rd=[mid], wr=[mid])
            S.op(dve, lambda h: h.tensor_tensor(out=maskb[:], in0=aff[:],
                                                in1=lo[:].unsqueeze(1).broadcast_to([128, 32, NE]), op=ALU.is_ge),
                 rd=[aff, lo], wr=[maskb])
            S.op(dve, lambda h: h.memset(cum[:, 0, :], 0.0), wr=[cum])
            for t in range(1, 32):
                S.op(dve, lambda h: h.tensor_tensor(out=cum[:, t, :], in0=cum[:, t - 1, :], in1=maskb[:, t - 1, :],
                                                    op=ALU.add), rd=[cum, maskb], wr=[cum])
            S.op(pe, lambda h: h.matmul(bank(1), lhsT=ustr_b, rhs=maskb[:].rearrange("p t e -> p (t e)"),
                                        start=True, stop=False), rd=[maskb, cstb], wr=[bankR[1]], self_sync=False)
            S.op(pe, lambda h: h.matmul(bank(1), lhsT=ones_b, rhs=cum[:].rearrange("p t e -> p (t e)"),
                                        start=False, stop=True), rd=[cum, cstb], wr=[bankR[1]], self_sync=False)
            S.op(dve, lambda h: h.scalar_tensor_tensor(out=posm[:].rearrange("p t e -> p (t e)"), in0=bank(1), scalar=1.0,
                                                       in1=maskb[:].rearrange("p t e -> p (t e)"),
                                                       op0=ALU.add, op1=ALU.mult), rd=[bankR[1], maskb], wr=[posm])
            S.op(dve, lambda h: h.tensor_scalar_add(out=posm[:], in0=posm[:], scalar1=-1.0), rd=[posm], wr=[posm])
            S.op(pool, lambda h: h.memset(TI[:], 0.0), wr=[TI])
            S.op(dve, lambda h: h.tensor_copy(out=TI[:, :, :, 0],
                                              in_=cst[:, 897:929].unsqueeze(1).broadcast_to([128, NE, 32])),
                 rd=[cst, TI], wr=[TI])
            S.op(dve, lambda h: h.tensor_copy(out=TI[:, :, :, 1],
                                              in_=cst[:, 896:897].unsqueeze(1).broadcast_to([128, NE, 32])),
                 rd=[cst, TI], wr=[TI])
            affv = aff[:].rearrange("p t e -> p e t")
            rresv = rres[:].rearrange("p t e -> p e t")
            S.op(dve, lambda h: h.tensor_copy(out=TI[:, :, :, 2], in_=affv), rd=[aff, TI], wr=[TI])
            S.op(dve, lambda h: h.tensor_tensor(out=rresv, in0=affv, in1=TI[:, :, :, 2], op=ALU.subtract),
                 rd=[aff, TI], wr=[rres])
            S.op(dve, lambda h: h.tensor_copy(out=TI[:, :, :, 3], in_=rresv), rd=[rres, TI], wr=[TI])
            S.op(dve, lambda h: h.tensor_tensor(out=rresv, in0=rresv, in1=TI[:, :, :, 3], op=ALU.subtract),
                 rd=[rres, TI], wr=[rres])
            S.op(dve, lambda h: h.tensor_copy(out=TI[:, :, :, 4], in_=rresv), rd=[rres, TI], wr=[TI])
            scn = 0
            for e in range(NE):
                b = 2 + e % 2
                for t in range(32):
                    Sx = Sb[scn % 4]
                    scn += 1
                    S.op(dve, lambda h: h.tensor_scalar(out=Sx[:], in0=iota_f, scalar1=posm[:, t, e:e + 1], scalar2=None,
                                                        op0=ALU.is_equal), rd=[cst, posm], wr=[Sx])
                    for sc in range(4):
                        S.op(pe, lambda h: h.matmul(bank(b)[:, sc * 8:sc * 8 + 8], lhsT=Sx[:, sc * 128:(sc + 1) * 128],
                                                    rhs=TI[:, e, t, :], start=(t == 0 and sc == 0), stop=(t == 31),
                                                    skip_group_check=True),
                             rd=[Sx, TI], wr=[bankR[b]], self_sync=False)
                S.op(dve, lambda h: h.tensor_copy(out=pvs[:], in_=bank(b)[:, 0:32]), rd=[bankR[b]], wr=[pvs])
                pv = pvs[:].rearrange("p (s c) -> p s c", c=8)
                S.op(dve, lambda h: h.scalar_tensor_tensor(out=idxf[:], in0=pv[:, :, 0], scalar=128.0, in1=pv[:, :, 1],
                                                           op0=ALU.mult, op1=ALU.add), rd=[pvs], wr=[idxf])
                S.op(dve, lambda h: h.tensor_copy(out=idx_i[:, e, :], in_=idxf[:]), rd=[idxf], wr=[idx_i])
                S.op(dve, lambda h: h.tensor_tensor(out=gsl[:, e, :], in0=pv[:, :, 2], in1=pv[:, :, 3], op=ALU.add),
                     rd=[pvs], wr=[gsl])
                S.op(dve, lambda h: h.tensor_tensor(out=gsl[:, e, :], in0=gsl[:, e, :], in1=pv[:, :, 4], op=ALU.add),
                     rd=[pvs, gsl], wr=[gsl])
            if dbg:
                S.dma(sp, lambda h: h.dma_start(out=dbg_idx, in_=idx_i[:].rearrange("p e s -> p (e s)")), rd=[idx_i])
                S.dma(sp, lambda h: h.dma_start(out=dbg_g, in_=gsl[:].rearrange("p e s -> p (e s)")), rd=[gsl])
        S.barrier()
        if stop_after < 5:
            S.finish()
            return nc

        with ExitStack() as p5:
            xe = [sb(p5, f"xe{i}", [128, 4, D], BF16) for i in range(2)]
            xeT = [sb(p5, f"xeT{i}", [128, 8, 512], BF16) for i in range(2)]
            hT = [sb(p5, f"hTe{i}", [128, NJ, 512], BF16) for i in range(2)]
            old = [sb(p5, f"old{i}", [128, D]) for i in range(4)]
            sg = [sb(p5, f"sg{i}", [128, 512]) for i in range(2)]
            tmp5 = [sb(p5, f"tmp5{i}", [128, 512]) for i in range(2)]
            xe_r = [[Reg() for _ in range(4)] for _ in range(2)]
            R_osc = [Reg() for _ in range(4)]

            def gather_xe(e, scs=range(4)):
                xg = xe[e % 2]
                for sc in scs:
                    S.dma(pool, lambda h: h.indirect_dma_start(
                        out=xg[:, sc, :], out_offset=None, in_=h2_d[:, :],
                        in_offset=bass.IndirectOffsetOnAxis(ap=idx_i[:, e, sc:sc + 1], axis=0)),
                        rd=[R_h2, idx_i], wr=[xg])

            def gather_old(e, scs=range(4)):
                for sc in scs:
                    S.dma(pool, lambda h: h.indirect_dma_start(
                        out=old[sc][:], out_offset=None, in_=out_d[:, :],
                        in_offset=bass.IndirectOffsetOnAxis(ap=idx_i[:, e, sc:sc + 1], axis=0)),
                        rd=[R_out, idx_i], wr=[old[sc]])

            def make_xeT(e):
                xg, xt_ = xe[e % 2], xeT[e % 2]
                for kc in range(8):
                    for sc in range(4):
                        S.op(pe, lambda h: h.transpose(out=bank_bf(0)[:, sc * 128:(sc + 1) * 128],
                                                       in_=xg[:, sc, kc * 128:(kc + 1) * 128], identity=ident_b),
                             rd=[xg, cstb], wr=[bankR[0]], self_sync=False)
                    S.op(act, lambda h: h.copy(out=xt_[:, kc, :], in_=bank_bf(0)[:, 0:512]), rd=[bankR[0]], wr=[xt_])

            def scatter_new(e, scs=range(4)):
                for sc in scs:
                    S.dma(pool, lambda h: h.indirect_dma_start(
                        out=out_d[:, :], out_offset=bass.IndirectOffsetOnAxis(ap=idx_i[:, e, sc:sc + 1], axis=0),
                        in_=old[sc][:], in_offset=None), rd=[old[sc], idx_i], wr=[R_out])

            gather_xe(0)
            gather_old(0)
            make_xeT(0)
            pcnt = 0
            dcnt = 0
            for e in range(NE):
                xt_ = xeT[e % 2]
                hT_ = hT[e % 2]
                for b in range(NB):
                    i = (e * NB + b) % NGU
                    if b < 4 and e + 1 < NE:
                        gather_xe(e + 1, [b])
                    if 4 <= b < 8 and e > 0:
                        scatter_new(e - 1, [b - 4])
                    if b >= 8 and e > 0:
                        gather_old(e, [b - 8])
                    for jj in range(2):
                        j = b * 2 + jj
                        bg = 1 + (pcnt % 2) * 2
                        pcnt += 1
                        for (bk, wt) in ((bg, wgb[i]), (bg + 1, wub[i])):
                            for kc in range(8):
                                S.op(pe, lambda h: h.matmul(bank(bk), lhsT=wt[:, kc, jj * 128:(jj + 1) * 128], rhs=xt_[:, kc, :],
                                                            start=(kc == 0), stop=(kc == 7)),
                                     rd=[wt, xt_], wr=[bankR[bk]], self_sync=False)
                        sg_ = sg[j % 2]
                        S.op(act, lambda h: h.activation(out=sg_[:], in_=bank(bg), func=AF.Silu), rd=[bankR[bg]], wr=[sg_])
                        S.op(dve, lambda h: h.tensor_tensor(out=hT_[:, j, :], in0=sg_[:], in1=bank(bg + 1), op=ALU.mult),
                             rd=[sg_, bankR[bg + 1]], wr=[hT_])
                    nb_ = b + NGU
                    if nb_ < NB:
                        load_gu(e, nb_)
                    elif e + 1 < NE:
                        load_gu(e + 1, nb_ - NB)
                if e > 0:
                    gather_old(e, [3])
                if e + 1 < NE:
                    make_xeT(e + 1)
                for fh in range(2):
                    wd_ = wdb[fh]
                    for sc in range(4):
                        bk = 5 + dcnt % 3
                        dcnt += 1
                        for j in range(NJ):
                            S.op(pe, lambda h: h.matmul(bank(bk), lhsT=hT_[:, j, sc * 128:(sc + 1) * 128], rhs=wd_[:, j, :],
                                                        start=(j == 0), stop=(j == NJ - 1)),
                                 rd=[hT_, wd_], wr=[bankR[bk]], self_sync=False)
                        t5 = tmp5[sc % 2]
                        S.op(dve, lambda h: h.scalar_tensor_tensor(out=t5[:], in0=bank(bk), scalar=gsl[:, e, sc:sc + 1],
                                                                   in1=bcG[:, 3, fh * 512:(fh + 1) * 512],
                                                                   op0=ALU.mult, op1=ALU.mult),
                             rd=[bankR[bk], gsl, bcG], wr=[t5])
                        S.op(dve, lambda h: h.tensor_tensor(out=old[sc][:, fh * 512:(fh + 1) * 512],
                                                             in0=old[sc][:, fh * 512:(fh + 1) * 512], in1=t5[:], op=ALU.add),
                             rd=[old[sc], t5], wr=[old[sc]])
                    if e + 1 < NE:
                        load_d(e + 1, fh)
            scatter_new(NE - 1)
        S.finish()
    return nc


def _consts():
    c = np.zeros((128, NCONST), np.float32)
    c[:, 0:128] = np.eye(128, dtype=np.float32)
    p = np.arange(128)
    c[:, 128:256] = (p[:, None] < p[None, :]).astype(np.float32)
    c[:, 256:384] = 1.0
    c[:, 384:896] = np.arange(512, dtype=np.float32)[None, :]
    c[:, 896] = p.astype(np.float32)
    c[:, 897:929] = np.arange(32, dtype=np.float32)[None, :]
    return c


def _rope_table16():
    tab = np.zeros((128, NT, 2, 64), np.float32)
    tab[:, :, 0, :] = 1.0
    inv = (np.float32(10000.0) ** (-np.arange(16, dtype=np.float32) / np.float32(16))).astype(np.float32)
    for T in range(2, NT):
        tok = (T - 2) * 128 + np.arange(128)
        row = (tok // 64).astype(np.float32)
        col = (tok % 64).astype(np.float32)
        ar = (row[:, None] * inv[None, :]).astype(np.float32)
        ac = (col[:, None] * inv[None, :]).astype(np.float32)
        cr, sr, cc, sc_ = np.cos(ar), np.sin(ar), np.cos(ac), np.sin(ac)
        tab[:, T, 0, 0:16] = cr
        tab[:, T, 0, 16:32] = cr
        tab[:, T, 0, 32:48] = cc
        tab[:, T, 0, 48:64] = cc
        tab[:, T, 1, 0:16] = -sr
        tab[:, T, 1, 16:32] = sr
        tab[:, T, 1, 32:48] = -sc_
        tab[:, T, 1, 48:64] = sc_
    return tab.reshape(128, NT * 128)


def make_in_maps(inp, cores):
    f = lambda a: np.ascontiguousarray(np.asarray(a, dtype=np.float32))
    L = 0
    c_ctx = f(inp["c_ctx"])
    conv_w = f(inp["conv_w"][L])
    convw = np.zeros((128, 16), np.float32)
    for j in range(4):
        for k in range(4):
            convw[:, j * 4 + k] = conv_w[k, j * 128:(j + 1) * 128]
    convb = f(inp["conv_b"][L]).reshape(4, 128).T
    lruw = np.zeros((128, 2, 2, 4, 128), np.float32)
    lrub = np.zeros((128, 2, 2, 4), np.float32)
    for d in range(2):
        for gi, (wn, bn) in enumerate((("lru_wa", "lru_ba"), ("lru_wi", "lru_bi"))):
            w = f(inp[wn][L][d])
            bb = f(inp[bn][L][d])
            for j in range(4):
                for hh in range(2):
                    lruw[hh * 64:(hh + 1) * 64, d, gi, j, hh * 64:(hh + 1) * 64] = w[2 * j + hh]
                    lrub[hh * 64:(hh + 1) * 64, d, gi, j] = bb[2 * j + hh]
    lrul = np.zeros((128, 2, 4), np.float32)
    lam = f(inp["lru_lambda"][L])
    for d in range(2):
        lrul[:, d, :] = lam[d].reshape(4, 128).T
    shared = {
        "w_ada": f(inp["w_ada"][L]),
        "b_ada": f(inp["b_ada"][L]).reshape(1, -1),
        "normg": np.concatenate([f(inp["norm1_g"][L]), f(inp["norm2_g"][L])]).reshape(1, -1),
        "qkg": np.concatenate([np.tile(f(inp["q_norm_g"][L]), 8), np.tile(f(inp["k_norm_g"][L]), 8)]).reshape(1, -1),
        "lamv": np.concatenate([f(inp["lambda_q1"][L]), f(inp["lambda_k1"][L]),
                                f(inp["lambda_q2"][L]), f(inp["lambda_k2"][L])]).reshape(1, -1),
        "subg": np.tile(f(inp["subln_g"][L]), 4).reshape(1, -1),
        "w_in": f(inp["w_in"][L]),
        "convw": convw,
        "convb": np.ascontiguousarray(convb),
        "lruw": np.ascontiguousarray(lruw.reshape(128, 2048)),
        "lrub": np.ascontiguousarray(lrub.reshape(128, 16)),
        "lrul": np.ascontiguousarray(lrul.reshape(128, 8)),
        "w_out": f(inp["w_out"][L]),
        "w_router": f(inp["w_router"][L]),
        "w_gate": f(inp["w_gate"][L]),
        "w_up": f(inp["w_up"][L]),
        "w_down": f(inp["w_down"][L]),
        "rope": _rope_table16(),
        "consts": _consts(),
    }
    maps = []
    for b in cores:
        cvec = np.zeros((128, 16), np.float32)
        cvec[:, 0:8] = f(inp["c"][b]).reshape(8, 128).T
        cvec[:, 8:16] = c_ctx.reshape(8, 128).T
        m = dict(shared)
        m["x"] = f(inp["x"][b])
        m["ctx"] = f(inp["ctx"][b])
        m["cvec"] = cvec
        maps.append(m)
    return maps


def kernel(**inputs):
    nc = build()
    maps = make_in_maps(inputs, list(range(8)))
    res = run_bass_kernel_spmd(nc, maps, core_ids=list(range(8)))
    return np.stack([np.asarray(r["out"], dtype=np.float32) for r in res.results], axis=0)
```

```python
import math
from contextlib import ExitStack

import numpy as np
import concourse.bass as bass
import concourse.mybir as mybir
from concourse.bass_utils import run_bass_kernel_spmd

F32 = mybir.dt.float32
BF16 = mybir.dt.bfloat16
I32 = mybir.dt.int32
AF = mybir.ActivationFunctionType
ALU = mybir.AluOpType
AX = mybir.AxisListType

D = 1024
SEQ = 4096
CTX = 256
NT = 34
NE = 16
CAP = 512
DEXP = 2816
NJ = 22
EPS = 1e-6
LAM_INIT = 0.2
NCONST = 936


class Reg:
    __slots__ = ("w", "rs")

    def __init__(self):
        self.w = {}
        self.rs = {}


class Tile:
    def __init__(self, t):
        self.t = t
        self.r = Reg()

    def __getitem__(self, k):
        return self.t[k]


class Eng:
    def __init__(self, h, key, dkeys):
        self.h = h
        self.key = key
        self.n = 0
        self.seen = {}
        self.dkeys = dkeys
        self.dvals = [0] * len(dkeys)
        self.di = 0


def _reg(x):
    return x.r if isinstance(x, Tile) else x


class Sched:
    def __init__(self, nc, stack):
        self.nc = nc
        self.sems = []

        def mk(name):
            s = stack.enter_context(nc.semaphore(name))
            self.sems.append(s)
            return len(self.sems) - 1

        self.pe = Eng(nc.tensor, mk("s_pe"), [])
        self.act = Eng(nc.scalar, mk("s_act"), [])
        self.dve = Eng(nc.vector, mk("s_dve"), [])
        self.pool = Eng(nc.gpsimd, mk("s_pool"), [mk(f"d_pool{i}") for i in range(12)])
        self.sp = Eng(nc.sync, mk("s_sp"), [mk(f"d_sp{i}") for i in range(12)])
        self.engs = [self.pe, self.act, self.dve, self.pool, self.sp]

    def _deps(self, rd, wr):
        deps = {}

        def add(k, v):
            if deps.get(k, 0) < v:
                deps[k] = v

        for r in rd:
            r = _reg(r)
            for k, v in r.w.items():
                add(k, v)
        for r in wr:
            r = _reg(r)
            for k, v in r.w.items():
                add(k, v)
            for k, v in r.rs.items():
                add(k, v)
        return deps

    def _wait(self, e, deps, self_sync):
        for k, v in deps.items():
            if k == e.key and not self_sync:
                continue
            if e.seen.get(k, 0) >= v:
                continue
            e.h.wait_ge(self.sems[k], v)
            e.seen[k] = v

    def _mark(self, ev, rd, wr):
        k, v = ev
        for r in rd:
            r = _reg(r)
            if r.rs.get(k, 0) < v:
                r.rs[k] = v
        for r in wr:
            r = _reg(r)
            r.w[k] = v
            r.rs = {}

    def op(self, e, fn, rd=(), wr=(), self_sync=True):
        self._wait(e, self._deps(rd, wr), self_sync)
        ins = fn(e.h)
        e.n += 1
        ins.then_inc(self.sems[e.key], 1)
        self._mark((e.key, e.n), rd, wr)
        return ins

    def dma(self, q, fn, rd=(), wr=()):
        self._wait(q, self._deps(rd, wr), True)
        slot = q.di % len(q.dkeys)
        q.di += 1
        k = q.dkeys[slot]
        prev = q.dvals[slot]
        if q.seen.get(k, 0) < prev:
            q.h.wait_ge(self.sems[k], prev)
            q.seen[k] = prev
        ins = fn(q.h)
        q.dvals[slot] = prev + 16
        ins.then_inc(self.sems[k], 16)
        self._mark((k, prev + 16), rd, wr)
        return ins

    def barrier(self):
        for e in self.engs:
            for o in self.engs:
                if o.n > 0 and e.seen.get(o.key, 0) < o.n and o is not e:
                    e.h.wait_ge(self.sems[o.key], o.n)
                    e.seen[o.key] = o.n
                for k, v in zip(o.dkeys, o.dvals):
                    if v > 0 and e.seen.get(k, 0) < v:
                        e.h.wait_ge(self.sems[k], v)
                        e.seen[k] = v

    def finish(self):
        q = self.sp
        for e in self.engs:
            for k, v in zip(e.dkeys, e.dvals):
                if v > 0 and q.seen.get(k, 0) < v:
                    q.h.wait_ge(self.sems[k], v)
                    q.seen[k] = v
        for e in self.engs:
            if e is not q and e.n > 0:
                q.h.wait_ge(self.sems[e.key], e.n)


def build(dbg=False, stop_after=99):
    nc = bass.Bass("TRN2", target_bir_lowering=False)
    okind = "ExternalOutput" if dbg else "Internal"

    def din(name, shape, dt=F32):
        return nc.dram_tensor(name, list(shape), dt, kind="ExternalInput").ap()

    x_d = din("x", [SEQ, D])
    ctx_d = din("ctx", [CTX, D])
    cvec_d = din("cvec", [128, 16])
    wada_d = din("w_ada", [D, 6 * D])
    bada_d = din("b_ada", [1, 6 * D])
    normg_d = din("normg", [1, 2 * D])
    qkg_d = din("qkg", [1, 1024])
    lamv_d = din("lamv", [1, 256])
    subg_d = din("subg", [1, 512])
    win_d = din("w_in", [D, 2560])
    convw_d = din("convw", [128, 16])
    convb_d = din("convb", [128, 4])
    lruw_d = din("lruw", [128, 2048])
    lrub_d = din("lrub", [128, 16])
    lrul_d = din("lrul", [128, 8])
    wout_d = din("w_out", [D, D])
    wr_d = din("w_router", [D, NE])
    big = stop_after >= 5
    wg_d = din("w_gate", [NE, D, DEXP] if big else [1, 8, 8])
    wu_d = din("w_up", [NE, D, DEXP] if big else [1, 8, 8])
    wd_d = din("w_down", [NE, DEXP, D] if big else [1, 8, 8])
    rope_d = din("rope", [128, NT * 128])
    consts_d = din("consts", [128, NCONST])
    out_d = nc.dram_tensor("out", [SEQ, D], F32, kind="ExternalOutput").ap()

    xl_d = nc.dram_tensor("xl_s", [4, 128, CTX + SEQ], F32, kind=okind).ap()
    gel_d = nc.dram_tensor("gel_s", [4, 128, SEQ], BF16, kind=okind).ap()
    qT_d = nc.dram_tensor("qT_s", [4, 128, SEQ], BF16, kind=okind).ap()
    kT_d = nc.dram_tensor("kT_s", [4, 128, CTX + SEQ], BF16, kind=okind).ap()
    v_d = nc.dram_tensor("v_s", [NT, 128, 520], BF16, kind=okind).ap()
    h2_d = nc.dram_tensor("h2_s", [SEQ, D], BF16, kind=okind).ap()
    lru_d = nc.dram_tensor("lru_s", [4, 128, SEQ], BF16, kind=okind).ap()
    if dbg:
        dbg_mod = nc.dram_tensor("dbg_mod", [1, 8192], F32, kind="ExternalOutput").ap()
        dbg_aff = nc.dram_tensor("dbg_aff", [128, 32 * NE], F32, kind="ExternalOutput").ap()
        dbg_idx = nc.dram_tensor("dbg_idx", [128, NE * 4], I32, kind="ExternalOutput").ap()
        dbg_g = nc.dram_tensor("dbg_g", [128, NE * 4], F32, kind="ExternalOutput").ap()

    R_xl = [Reg() for _ in range(4)]
    R_gel, R_qT, R_kT, R_v, R_h2, R_out, R_lru = Reg(), Reg(), Reg(), Reg(), Reg(), Reg(), Reg()

    with ExitStack() as top:
        S = Sched(nc, top)
        pe, act, dve, pool, sp = S.pe, S.act, S.dve, S.pool, S.sp

        def sb(stack, name, shape, dt=F32):
            return Tile(stack.enter_context(nc.sbuf_tensor("t_" + name, list(shape), dt)))

        PS = top.enter_context(nc.psum_tensor("PS", [128, 8, 512], F32))
        bankR = [Reg() for _ in range(8)]

        def bank(b):
            return PS[:, b, :]

        def bank_bf(b):
            return PS[:, b, :].bitcast(BF16)

        cst = sb(top, "cst", [128, NCONST])
        cstb = sb(top, "cstb", [128, 384], BF16)
        bcG = sb(top, "bcG", [128, 4, D])
        aff = sb(top, "aff", [128, 32, NE])
        idx_i = sb(top, "idx_i", [128, NE, 4], I32)
        gsl = sb(top, "gsl", [128, NE, 4])
        A1, B1, A1C, B1C, G1, A2, B2, G2 = range(8)
        sc2 = sb(top, "sc2", [128, 2])
        epsT = sb(top, "epsT", [128, 1])
        S.dma(sp, lambda h: h.dma_start(out=cst[:], in_=consts_d), wr=[cst])
        S.op(dve, lambda h: h.tensor_copy(out=cstb[:], in_=cst[:, 0:384]), rd=[cst], wr=[cstb])
        S.op(dve, lambda h: h.memset(epsT[:], EPS), wr=[epsT])
        ident_f = cst[:, 0:128]
        ones_f = cst[:, 256:384]
        iota_f = cst[:, 384:896]
        ident_b = cstb[:, 0:128]
        ustr_b = cstb[:, 128:256]
        ones_b = cstb[:, 256:384]

        def rsqrt_ops(dst, src, scale, n, lo=0):
            S.op(act, lambda h: h.activation(out=dst[:, lo:n], in_=src[:, lo:n], func=AF.Sqrt,
                                             scale=scale, bias=epsT[:, 0:1]),
                 rd=[src, epsT], wr=[dst])
            S.op(dve, lambda h: h.reciprocal(out=dst[:, lo:n], in_=dst[:, lo:n]), rd=[dst], wr=[dst])

        p03 = top.enter_context(ExitStack())
        bcs = sb(p03, "bcs", [128, 512])
        p01 = p03.enter_context(ExitStack())
        bcq = sb(p01, "bcq", [128, 1024])
        bcA = sb(p01, "bcA", [128, 4, D])

        def bcr(i):
            return (bcA, i) if i < 4 else (bcG, i - 4)

        winb = sb(p01, "winb", [128, 8, 2560], BF16)
        ropeT = sb(p01, "ropeT", [128, NT, 2, 64])
        for kc in range(8):
            S.dma(pool, lambda h: h.dma_start(
                out=winb[:, kc, :].rearrange("p (a n) -> p a n", n=640),
                in_=win_d[kc * 128:(kc + 1) * 128, :].rearrange("p (a n) -> p a n", n=640)), wr=[winb])
        S.dma(sp, lambda h: h.dma_start(out=ropeT[:].rearrange("p t c d -> p (t c d)"), in_=rope_d), wr=[ropeT])

        with ExitStack() as p0:
            cv = sb(p0, "cv", [128, 16])
            scv = sb(p0, "scv", [128, 16])
            modrow = sb(p0, "modrow", [1, 8192])
            bada = sb(p0, "bada", [1, 6 * D])
            normg = sb(p0, "normg", [1, 2 * D])
            rowt = sb(p0, "rowt", [1, 3, D])
            qkg = sb(p0, "qkg", [1, 1024])
            lamv = sb(p0, "lamv", [1, 256])
            subg = sb(p0, "subg", [1, 512])
            lt = sb(p0, "lt", [1, 16])
            wb = [sb(p0, f"wadab{i}", [128, 8, 256]) for i in range(2)]
            S.dma(sp, lambda h: h.dma_start(out=cv[:], in_=cvec_d), wr=[cv])
            S.dma(sp, lambda h: h.dma_start(out=bada[:], in_=bada_d), wr=[bada])
            S.dma(sp, lambda h: h.dma_start(out=normg[:], in_=normg_d), wr=[normg])
            S.dma(sp, lambda h: h.dma_start(out=qkg[:], in_=qkg_d), wr=[qkg])
            S.dma(sp, lambda h: h.dma_start(out=lamv[:], in_=lamv_d), wr=[lamv])
            S.dma(sp, lambda h: h.dma_start(out=subg[:], in_=subg_d), wr=[subg])
            S.op(act, lambda h: h.activation(out=scv[:], in_=cv[:], func=AF.Silu), rd=[cv], wr=[scv])
            wada_v = wada_d.rearrange("(kc p) n -> p kc n", p=128)
            CW = 256
            for nb in range(6 * D // CW):
                w = wb[nb % 2]
                S.dma(sp, lambda h: h.dma_start(out=w[:], in_=wada_v[:, :, nb * CW:(nb + 1) * CW]), wr=[w])
                for kc in range(8):
                    S.op(pe, lambda h: h.matmul(bank(0)[0:1, 0:CW], lhsT=scv[:, kc:kc + 1], rhs=w[:, kc, :],
                                                start=(kc == 0), stop=(kc == 7)),
                         rd=[scv, w], wr=[bankR[0]], self_sync=False)
                S.op(dve, lambda h: h.tensor_tensor(out=modrow[0:1, nb * CW:(nb + 1) * CW], in0=bank(0)[0:1, 0:CW],
                                                    in1=bada[0:1, nb * CW:(nb + 1) * CW], op=ALU.add),
                     rd=[bankR[0], bada], wr=[modrow])
                if nb < 2 * D // CW:
                    for kc in range(8):
                        S.op(pe, lambda h: h.matmul(bank(1)[0:1, 0:CW], lhsT=scv[:, 8 + kc:9 + kc], rhs=w[:, kc, :],
                                                    start=(kc == 0), stop=(kc == 7)),
                             rd=[scv, w], wr=[bankR[1]], self_sync=False)
                    S.op(dve, lambda h: h.tensor_tensor(out=modrow[0:1, 6144 + nb * CW:6144 + (nb + 1) * CW],
                                                        in0=bank(1)[0:1, 0:CW], in1=bada[0:1, nb * CW:(nb + 1) * CW],
                                                        op=ALU.add),
                         rd=[bankR[1], bada], wr=[modrow])
            if dbg:
                S.dma(sp, lambda h: h.dma_start(out=dbg_mod, in_=modrow[:]), rd=[modrow])

            def mrow(i):
                return modrow[0:1, i * D:(i + 1) * D]

            for ti, (si, go) in enumerate([(1, 0), (7, 0), (4, D)]):
                S.op(dve, lambda h: h.scalar_tensor_tensor(out=rowt[0:1, ti, :], in0=mrow(si), scalar=1.0,
                                                           in1=normg[0:1, go:go + D], op0=ALU.add, op1=ALU.mult),
                     rd=[modrow, normg], wr=[rowt])
            rows = {A1: rowt[0:1, 0, :], B1: mrow(0), A1C: rowt[0:1, 1, :], B1C: mrow(6), G1: mrow(2),
                    A2: rowt[0:1, 2, :], B2: mrow(3), G2: mrow(5)}
            cnt = 0
            for bi, row in rows.items():
                for hf in range(2):
                    b = 2 + cnt % 2
                    cnt += 1
                    S.op(pe, lambda h: h.matmul(bank(b), lhsT=ones_f[0:1, :], rhs=row[0:1, hf * 512:(hf + 1) * 512],
                                                start=True, stop=True),
                         rd=[cst, modrow, rowt], wr=[bankR[b]], self_sync=False)
                    bt_, bi_ = bcr(bi)
                    S.op(act, lambda h: h.copy(out=bt_[:, bi_, hf * 512:(hf + 1) * 512], in_=bank(b)),
                         rd=[bankR[b]], wr=[bt_])
            for hf in range(2):
                b = 2 + hf
                S.op(pe, lambda h: h.matmul(bank(b), lhsT=ones_f[0:1, :], rhs=qkg[0:1, hf * 512:(hf + 1) * 512],
                                            start=True, stop=True), rd=[cst, qkg], wr=[bankR[b]], self_sync=False)
                S.op(act, lambda h: h.mul(out=bcq[:, hf * 512:(hf + 1) * 512], in_=bank(b),
                                          mul=(0.125 if hf == 0 else 1.0)), rd=[bankR[b]], wr=[bcq])
            S.op(pe, lambda h: h.matmul(bank(2), lhsT=ones_f[0:1, :], rhs=subg[0:1, :], start=True, stop=True),
                 rd=[cst, subg], wr=[bankR[2]], self_sync=False)
            S.op(act, lambda h: h.mul(out=bcs[:], in_=bank(2), mul=1.0 - LAM_INIT), rd=[bankR[2]], wr=[bcs])
            S.op(dve, lambda h: h.tensor_tensor(out=lamv[0:1, 0:64], in0=lamv[0:1, 0:64], in1=lamv[0:1, 64:128],
                                                op=ALU.mult), rd=[lamv], wr=[lamv])
            S.op(dve, lambda h: h.tensor_tensor(out=lamv[0:1, 128:192], in0=lamv[0:1, 128:192],
                                                in1=lamv[0:1, 192:256], op=ALU.mult), rd=[lamv], wr=[lamv])
            S.op(dve, lambda h: h.reduce_sum(out=lt[0:1, 0:1], in_=lamv[0:1, 0:64], axis=AX.X), rd=[lamv], wr=[lt])
            S.op(dve, lambda h: h.reduce_sum(out=lt[0:1, 1:2], in_=lamv[0:1, 128:192], axis=AX.X), rd=[lamv], wr=[lt])
            S.op(act, lambda h: h.activation(out=lt[0:1, 2:4], in_=lt[0:1, 0:2], func=AF.Exp), rd=[lt], wr=[lt])
            S.op(dve, lambda h: h.tensor_tensor(out=lt[0:1, 4:5], in0=lt[0:1, 3:4], in1=lt[0:1, 2:3],
                                                op=ALU.subtract), rd=[lt], wr=[lt])
            S.op(dve, lambda h: h.tensor_scalar_add(out=lt[0:1, 4:5], in0=lt[0:1, 4:5], scalar1=-LAM_INIT),
                 rd=[lt], wr=[lt])
            S.op(dve, lambda h: h.reduce_max(out=lt[0:1, 6:7], in_=qkg[0:1, 0:64], axis=AX.X,
                                             apply_absolute_value=True), rd=[qkg], wr=[lt])
            S.op(dve, lambda h: h.reduce_max(out=lt[0:1, 7:8], in_=qkg[0:1, 512:576], axis=AX.X,
                                             apply_absolute_value=True), rd=[qkg], wr=[lt])
            S.op(dve, lambda h: h.tensor_tensor(out=lt[0:1, 5:6], in0=lt[0:1, 6:7], in1=lt[0:1, 7:8], op=ALU.mult),
                 rd=[lt], wr=[lt])
            S.op(dve, lambda h: h.tensor_scalar_mul(out=lt[0:1, 5:6], in0=lt[0:1, 5:6], scalar1=-8.0),
                 rd=[lt], wr=[lt])
            S.op(pe, lambda h: h.matmul(bank(3)[:, 0:2], lhsT=ones_f[0:1, :], rhs=lt[0:1, 4:6], start=True, stop=True),
                 rd=[cst, lt], wr=[bankR[3]], self_sync=False)
            S.op(act, lambda h: h.copy(out=sc2[:], in_=bank(3)[:, 0:2]), rd=[bankR[3]], wr=[sc2])
        S.barrier()
        if stop_after < 1:
            S.finish()
            return nc

        with ExitStack() as p1:
            hlT = [sb(p1, f"hlT{i}", [128, 8, 512], BF16) for i in range(2)]
            xb = [sb(p1, f"xb{i}", [128, D]) for i in range(4)]
            junk = sb(p1, "junk", [128, D], BF16)
            ss4 = [sb(p1, f"ss4{i}", [128, 4]) for i in range(2)]
            rs4 = [sb(p1, f"rs4{i}", [128, 4]) for i in range(2)]
            t1 = sb(p1, "t1_0", [128, D])
            hl = [sb(p1, f"hl{i}", [128, D], BF16) for i in range(2)]
            xlst = [sb(p1, f"xlst{i}", [128, 512]) for i in range(2)]
            gst = sb(p1, "gst0", [128, 4, 512], BF16)
            vst = [sb(p1, f"vst{i}", [128, 4, 4, 130], BF16) for i in range(2)]
            sq = [sb(p1, f"sq{i}", [128, D]) for i in range(2)]
            ss16 = [sb(p1, f"ss16{i}", [128, 16]) for i in range(2)]
            rs16 = [sb(p1, f"rs16{i}", [128, 16]) for i in range(2)]
            tq = [sb(p1, f"tq{i}", [128, D]) for i in range(2)]
            r1 = [sb(p1, f"r1{i}", [128, D]) for i in range(2)]
            qkr = [sb(p1, f"qkr{i}", [128, D], BF16) for i in range(2)]
            qkst = [sb(p1, f"qkst{i}", [128, 8, 512], BF16) for i in range(2)]
            for v in vst:
                S.op(pool, lambda h: h.memset(v[:], 1.0), wr=[v])

            def blk_tiles(blk):
                return [0, 1] if blk == 0 else [2 + 4 * (blk - 1) + i for i in range(4)]

            cnts = {"x": 0, "f": 0, "t": 0}

            hl_state = {}

            def hl_chain(blk, i):
                xts, r4 = hl_state[blk]
                ai, bi = (A1C, B1C) if blk == 0 else (A1, B1)
                xt, hh = xts[i], hl[i % 2]
                S.op(dve, lambda h: h.scalar_tensor_tensor(out=t1[:], in0=xt[:], scalar=r4[:, i:i + 1],
                                                           in1=bcA[:, ai, :], op0=ALU.mult, op1=ALU.mult),
                     rd=[xt, r4, bcA], wr=[t1])
                S.op(dve, lambda h: h.tensor_tensor(out=hh[:], in0=t1[:], in1=bcA[:, bi, :], op=ALU.add),
                     rd=[t1, bcA], wr=[hh])

            def hl_T(blk, i):
                hT, hh = hlT[blk % 2], hl[i % 2]
                for kc in range(8):
                    S.op(pe, lambda h: h.transpose(out=bank_bf(0)[:, kc * 128:(kc + 1) * 128],
                                                   in_=hh[:, kc * 128:(kc + 1) * 128], identity=ident_b),
                         rd=[hh, cstb], wr=[bankR[0]], self_sync=False)
                S.op(act, lambda h: h.copy(out=hT[:, :, i * 128:(i + 1) * 128],
                                           in_=bank_bf(0).rearrange("p (k t) -> p k t", t=128)),
                     rd=[bankR[0]], wr=[hT])

            def hl_front(blk):
                tiles = blk_tiles(blk)
                nt = len(tiles)
                s4, r4 = ss4[blk % 2], rs4[blk % 2]
                xts = []
                for i, T in enumerate(tiles):
                    xt = xb[cnts["x"] % 4]
                    cnts["x"] += 1
                    src = ctx_d[T * 128:(T + 1) * 128, :] if T < 2 else x_d[(T - 2) * 128:(T - 1) * 128, :]
                    S.dma(sp, lambda h: h.dma_start(out=xt[:], in_=src), wr=[xt])
                    S.op(act, lambda h: h.activation(out=junk[:], in_=xt[:], func=AF.Square,
                                                     accum_out=s4[:, i:i + 1]), rd=[xt], wr=[junk, s4])
                    xts.append(xt)
                rsqrt_ops(r4, s4, 1.0 / D, nt)
                hl_state[blk] = (xts, r4)
                for i in range(min(2, nt)):
                    hl_chain(blk, i)

            def hl_back(blk):
                nt = len(blk_tiles(blk))
                for i in range(nt):
                    if i >= 2:
                        hl_chain(blk, i)
                    hl_T(blk, i)

            def hlstage(blk):
                hl_front(blk)
                hl_back(blk)

            def fmstage(blk):
                tiles = blk_tiles(blk)
                W = 128 * len(tiles)
                toff = 0 if blk == 0 else CTX + (blk - 1) * 512
                loff = (blk - 1) * 512
                hT = hlT[blk % 2]
                for oc in range(4 if blk == 0 else 8):
                    b = 2 + cnts["f"] % 2
                    cnts["f"] += 1
                    for kc in range(8):
                        S.op(pe, lambda h: h.matmul(bank(b)[:, 0:W], lhsT=winb[:, kc, oc * 128:(oc + 1) * 128],
                                                    rhs=hT[:, kc, 0:W], start=(kc == 0), stop=(kc == 7)),
                             rd=[winb, hT], wr=[bankR[b]], self_sync=False)
                    if oc < 4:
                        st = xlst[oc % 2]
                        S.op(dve, lambda h: h.tensor_copy(out=st[:, 0:W], in_=bank(b)[:, 0:W]),
                             rd=[bankR[b]], wr=[st])
                        S.dma(sp, lambda h: h.dma_start(out=xl_d[oc, :, toff:toff + W], in_=st[:, 0:W]),
                              rd=[st], wr=[R_xl[oc]])
                    else:
                        S.op(act, lambda h: h.activation(out=gst[:, oc - 4, :], in_=bank(b), func=AF.Gelu_apprx_tanh),
                             rd=[bankR[b]], wr=[gst])
                if blk > 0:
                    S.dma(sp, lambda h: h.dma_start(out=gel_d.rearrange("c p t -> p c t")[:, :, loff:loff + 512],
                                                    in_=gst[:]), rd=[gst], wr=[R_gel])

            def stage_a(blk, i):
                T = blk_tiles(blk)[i]
                hT = hlT[blk % 2]
                vs = vst[blk % 2]
                g0 = 8 if blk == 0 else 0
                c0 = g0 * 64
                u = cnts["t"] % 2
                qb_ = 4 + 2 * u
                for (b, col) in ([(qb_, 1024)] if blk > 0 else []) + [(qb_ + 1, 1536), (1, 2048)]:
                    for kc in range(8):
                        S.op(pe, lambda h: h.matmul(bank(b), lhsT=hT[:, kc, i * 128:(i + 1) * 128],
                                                    rhs=winb[:, kc, col:col + 512], start=(kc == 0), stop=(kc == 7)),
                             rd=[winb, hT], wr=[bankR[b]], self_sync=False)
                S.op(act, lambda h: h.copy(out=vs[:, i, :, 0:128],
                                           in_=bank(1).rearrange("p (a e) -> p a e", e=128)),
                     rd=[bankR[1]], wr=[vs])
                pqk = PS[:, qb_:qb_ + 2, :].rearrange("p a n -> p (a n)")
                S.op(act, lambda h: h.activation(out=sq[u][:, c0:], in_=pqk[:, c0:], func=AF.Square),
                     rd=[bankR[qb_], bankR[qb_ + 1]], wr=[sq[u]])
                cnts["t"] += 1
                return u

            def stage_a2(blk, i, u):
                g0 = 8 if blk == 0 else 0
                c0 = g0 * 64
                qb_ = 4 + 2 * u
                pqk = PS[:, qb_:qb_ + 2, :].rearrange("p a n -> p (a n)")
                S.op(dve, lambda h: h.tensor_reduce(out=ss16[u][:, g0:], in_=sq[u][:, c0:].rearrange("p (g d) -> p g d", d=64),
                                                    axis=AX.X, op=ALU.add), rd=[sq[u]], wr=[ss16[u]])
                rsqrt_ops(rs16[u], ss16[u], 1.0 / 64, 16, g0)
                S.op(dve, lambda h: h.tensor_tensor(
                    out=tq[u][:, c0:].rearrange("p (g d) -> p g d", d=64),
                    in0=pqk[:, c0:].rearrange("p (g d) -> p g d", d=64),
                    in1=rs16[u][:, g0:].unsqueeze(2).broadcast_to([128, 16 - g0, 64]), op=ALU.mult),
                    rd=[bankR[qb_], bankR[qb_ + 1], rs16[u]], wr=[tq[u]])
                S.op(dve, lambda h: h.tensor_tensor(out=tq[u][:, c0:], in0=tq[u][:, c0:], in1=bcq[:, c0:], op=ALU.mult),
                     rd=[tq[u], bcq], wr=[tq[u]])

            def stage_b(blk, i, u):
                T = blk_tiles(blk)[i]
                qs = qkst[blk % 2]
                g0 = 8 if blk == 0 else 0
                c0 = g0 * 64
                ng = 16 - g0
                r2 = sq[u]
                S.op(pool, lambda h: h.tensor_tensor(
                    out=r1[u][:, c0:].rearrange("p (g d) -> p g d", d=64),
                    in0=tq[u][:, c0:].rearrange("p (g d) -> p g d", d=64),
                    in1=ropeT[:, T, 0, :].unsqueeze(1).broadcast_to([128, ng, 64]), op=ALU.mult),
                    rd=[tq[u], ropeT], wr=[r1[u]])
                tq5 = tq[u][:, c0:].rearrange("p (g t h w) -> p g t h w", t=2, h=2, w=16)
                r25 = r2[:, c0:].rearrange("p (g t h w) -> p g t h w", t=2, h=2, w=16)
                sn4 = ropeT[:, T, 1, :].rearrange("p (t h w) -> p t h w", t=2, h=2)
                for hv in range(2):
                    S.op(pool, lambda h: h.tensor_tensor(
                        out=r25[:, :, :, hv, :], in0=tq5[:, :, :, 1 - hv, :],
                        in1=sn4[:, :, hv, :].unsqueeze(1).broadcast_to([128, ng, 2, 16]), op=ALU.mult),
                        rd=[tq[u], ropeT], wr=[r2])

            def stage_b2(blk, i, u):
                qs = qkst[blk % 2]
                g0 = 8 if blk == 0 else 0
                c0 = g0 * 64
                r2 = sq[u]
                qq = qkr[u]
                S.op(pool, lambda h: h.tensor_tensor(out=qq[:, c0:], in0=r1[u][:, c0:], in1=r2[:, c0:], op=ALU.add),
                     rd=[r1[u], r2], wr=[qq])
                k0 = g0 // 2
                for kc in range(k0, 8):
                    S.op(pe, lambda h: h.transpose(out=bank_bf(0)[:, kc * 128:(kc + 1) * 128],
                                                   in_=qq[:, kc * 128:(kc + 1) * 128], identity=ident_b),
                         rd=[qq, cstb], wr=[bankR[0]], self_sync=False)
                S.op(act, lambda h: h.copy(out=qs[:, k0:8, i * 128:(i + 1) * 128],
                                           in_=bank_bf(0).rearrange("p (k t) -> p k t", t=128)[:, k0:8, :]),
                     rd=[bankR[0]], wr=[qs])

            def outstage(blk):
                tiles = blk_tiles(blk)
                nt = len(tiles)
                W = 128 * nt
                toff = 0 if blk == 0 else CTX + (blk - 1) * 512
                loff = (blk - 1) * 512
                qs = qkst[blk % 2]
                vs = vst[blk % 2]
                if blk > 0:
                    S.dma(sp, lambda h: h.dma_start(out=qT_d.rearrange("c p t -> p c t")[:, :, loff:loff + 512],
                                                    in_=qs[:, 0:4, :]), rd=[qs], wr=[R_qT])
                S.dma(sp, lambda h: h.dma_start(out=kT_d.rearrange("c p t -> p c t")[:, :, toff:toff + W],
                                                in_=qs[:, 4:8, 0:W]), rd=[qs], wr=[R_kT])
                S.dma(sp, lambda h: h.dma_start(
                    out=v_d[tiles[0]:tiles[0] + nt, :, :].rearrange("t p f -> p t f"),
                    in_=vs[:, 0:nt, :, :].rearrange("p t a e -> p t (a e)")), rd=[vs], wr=[R_v])

            flat = [(blk, i) for blk in range(9) for i in range(len(blk_tiles(blk)))]
            hlstage(0)
            fmstage(0)
            hlstage(1)
            prev = None
            for (blk, i) in flat:
                u = stage_a(blk, i)
                last = (i == len(blk_tiles(blk)) - 1)
                if i == len(blk_tiles(blk)) - 2 and blk + 2 < 9:
                    hl_front(blk + 2)
                if last and blk + 1 < 9:
                    fmstage(blk + 1)
                    if blk + 2 < 9:
                        hl_back(blk + 2)
                if prev is not None:
                    stage_b(*prev)
                stage_a2(blk, i, u)
                if prev is not None:
                    stage_b2(*prev)
                    if prev[1] == len(blk_tiles(prev[0])) - 1:
                        outstage(prev[0])
                prev = (blk, i, u)
            stage_b(*prev)
            stage_b2(*prev)
            outstage(prev[0])
        p01.close()
        S.barrier()
        if stop_after < 2:
            S.finish()
            return nc


        with ExitStack() as p2:
            TT = CTX + SEQ
            HL = SEQ // 2
            convw = sb(p2, "convw", [128, 16])
            convb = sb(p2, "convb", [128, 4])
            lrub = sb(p2, "lrub", [128, 16])
            lrul = sb(p2, "lrul", [128, 8])
            cL = sb(p2, "cL", [128, 8])
            cL2 = sb(p2, "cL2", [128, 8])
            onesT = sb(p2, "onesT", [128, 1])
            lruwb = sb(p2, "lruwb", [128, 16, 128], BF16)
            XP = sb(p2, "XP", [128, TT + 8])
            xcs = [sb(p2, f"xc{i}", [128, TT]) for i in range(2)]
            xcbs = [sb(p2, f"xcb{i}", [128, TT], BF16) for i in range(2)]
            Rs = [sb(p2, f"Rr{i}", [128, HL]) for i in range(2)]
            As = [sb(p2, f"A2_{i}", [128, HL]) for i in range(2)]
            Is = [sb(p2, f"Ii{i}", [128, HL]) for i in range(2)]
            Hf = sb(p2, "Hf", [128, TT])
            Hb = sb(p2, "Hb", [128, TT])
            gl = sb(p2, "gl", [128, SEQ], BF16)
            lst = sb(p2, "lst", [128, SEQ], BF16)
            S.dma(sp, lambda h: h.dma_start(out=convw[:], in_=convw_d), wr=[convw])
            S.dma(sp, lambda h: h.dma_start(out=convb[:], in_=convb_d), wr=[convb])
            S.dma(sp, lambda h: h.dma_start(out=lrub[:], in_=lrub_d), wr=[lrub])
            S.dma(sp, lambda h: h.dma_start(out=lrul[:], in_=lrul_d), wr=[lrul])
            S.dma(pool, lambda h: h.dma_start(out=lruwb[:], in_=lruw_d.rearrange("p (a n) -> p a n", n=128)),
                  wr=[lruwb])
            S.op(act, lambda h: h.activation(out=cL[:], in_=lrul[:], func=AF.Exp, scale=-1.0), rd=[lrul], wr=[cL])
            S.op(act, lambda h: h.activation(out=cL[:], in_=cL[:], func=AF.Ln, bias=1.0), rd=[cL], wr=[cL])
            S.op(dve, lambda h: h.tensor_scalar_mul(out=cL2[:], in0=cL[:], scalar1=-16.0), rd=[cL], wr=[cL2])
            S.op(dve, lambda h: h.tensor_scalar_mul(out=cL[:], in0=cL[:], scalar1=-8.0), rd=[cL, cL2], wr=[cL])
            S.op(dve, lambda h: h.memset(onesT[:], 1.0), wr=[onesT])
            S.op(dve, lambda h: h.memset(XP[:], 0.0), wr=[XP])
            segs = [(1, 0, CTX), (260, CTX, SEQ)]

            def conv(j):
                xc, xcb = xcs[j % 2], xcbs[j % 2]
                S.dma(sp, lambda h: h.dma_start(out=XP[:, 1:1 + CTX], in_=xl_d[j, :, 0:CTX]), rd=[R_xl[j]], wr=[XP])
                S.dma(sp, lambda h: h.dma_start(out=XP[:, 260:260 + SEQ], in_=xl_d[j, :, CTX:TT]),
                      rd=[R_xl[j]], wr=[XP])
                for (xo, to, ln) in segs:
                    S.op(dve, lambda h: h.tensor_scalar(out=xc[:, to:to + ln], in0=XP[:, xo - 1:xo - 1 + ln],
                                                         scalar1=convw[:, j * 4:j * 4 + 1], scalar2=convb[:, j:j + 1],
                                                         op0=ALU.mult, op1=ALU.add),
                         rd=[XP, convw, convb], wr=[xc])
                    for k in range(1, 4):
                        S.op(dve, lambda h: h.scalar_tensor_tensor(
                            out=xc[:, to:to + ln], in0=XP[:, xo - 1 + k:xo - 1 + k + ln],
                            scalar=convw[:, j * 4 + k:j * 4 + k + 1], in1=xc[:, to:to + ln],
                            op0=ALU.mult, op1=ALU.add), rd=[XP, convw, xc], wr=[xc])
                S.op(dve, lambda h: h.tensor_copy(out=xcb[:], in_=xc[:]), rd=[xc], wr=[xcb])

            pc = {"n": 0, "b": 0}

            def piece(j, d, t0, ln, first_col, init, rev):
                xc, xcb = xcs[j % 2], xcbs[j % 2]
                s_ = pc["n"] % 2
                pc["n"] += 1
                Rr, A2_, Ii = Rs[s_], As[s_], Is[s_]
                H = Hf if d == 0 else Hb
                for o in range(0, ln, 512):
                    w_ = min(512, ln - o)
                    for gi, dst in enumerate([Rr, Ii]):
                        b = 2 + pc["b"] % 4
                        pc["b"] += 1
                        S.op(pe, lambda h: h.matmul(bank(b)[:, 0:w_], lhsT=lruwb[:, (d * 2 + gi) * 4 + j, :],
                                                    rhs=xcb[:, t0 + o:t0 + o + w_], start=True, stop=True),
                             rd=[lruwb, xcb], wr=[bankR[b]], self_sync=False)
                        bi_ = (d * 2 + gi) * 4 + j
                        S.op(act, lambda h: h.activation(out=dst[:, o:o + w_], in_=bank(b)[:, 0:w_],
                                                         func=AF.Sigmoid, bias=lrub[:, bi_:bi_ + 1]),
                             rd=[bankR[b], lrub], wr=[dst])
                ci = d * 4 + j
                S.op(act, lambda h: h.activation(out=Rr[:, 0:ln], in_=Rr[:, 0:ln], func=AF.Exp, scale=cL[:, ci:ci + 1]),
                     rd=[Rr, cL], wr=[Rr])
                S.op(dve, lambda h: h.scalar_tensor_tensor(out=A2_[:, 0:ln], in0=Rr[:, 0:ln], scalar=1.0, in1=Rr[:, 0:ln],
                                                           op0=ALU.min, op1=ALU.mult), rd=[Rr], wr=[A2_])
                S.op(act, lambda h: h.activation(out=A2_[:, 0:ln], in_=A2_[:, 0:ln], func=AF.Sqrt, scale=-1.0,
                                                 bias=onesT[:, 0:1]), rd=[A2_, onesT], wr=[A2_])
                if first_col is not None:
                    S.op(dve, lambda h: h.memset(A2_[:, first_col:first_col + 1], 1.0), wr=[A2_])
                S.op(dve, lambda h: h.tensor_tensor(out=Ii[:, 0:ln], in0=Ii[:, 0:ln], in1=A2_[:, 0:ln], op=ALU.mult),
                     rd=[Ii, A2_], wr=[Ii])
                S.op(dve, lambda h: h.tensor_tensor(out=Ii[:, 0:ln], in0=Ii[:, 0:ln], in1=xc[:, t0:t0 + ln], op=ALU.mult),
                     rd=[Ii, xc], wr=[Ii])
                hv, av, uv = H[:, t0:t0 + ln], Rr[:, 0:ln], Ii[:, 0:ln]
                if rev:
                    hv, av, uv = hv[:, ::-1], av[:, ::-1], uv[:, ::-1]
                S.op(dve, lambda h: h.tensor_tensor_scan(out=hv, data0=av, data1=uv, initial=init,
                                                         op0=ALU.mult, op1=ALU.add), rd=[Rr, Ii, H], wr=[H])

            conv(0)
            for j in range(4):
                S.dma(sp, lambda h: h.dma_start(out=gl[:], in_=gel_d[j, :, :]), rd=[R_gel], wr=[gl])
                if j + 1 < 4:
                    conv(j + 1)
                piece(j, 0, 0, CTX, 0, 0.0, False)
                piece(j, 0, CTX, HL, None, Hf[:, CTX - 1:CTX], False)
                piece(j, 0, CTX + HL, HL, None, Hf[:, CTX + HL - 1:CTX + HL], False)
                piece(j, 1, 0, CTX, CTX - 1, 0.0, True)
                piece(j, 1, CTX + HL, HL, None, Hb[:, 0:1], True)
                piece(j, 1, CTX, HL, None, Hb[:, CTX + HL:CTX + HL + 1], True)
                S.op(dve, lambda h: h.tensor_tensor(out=Hf[:, CTX:TT], in0=Hf[:, CTX:TT], in1=Hb[:, CTX:TT], op=ALU.add),
                     rd=[Hf, Hb], wr=[Hf])
                S.op(dve, lambda h: h.tensor_tensor(out=lst[:], in0=Hf[:, CTX:TT], in1=gl[:], op=ALU.mult),
                     rd=[Hf, gl], wr=[lst])
                S.dma(sp, lambda h: h.dma_start(out=lru_d[j, :, :], in_=lst[:]), rd=[lst], wr=[R_lru])
        S.barrier()
        if stop_after < 3:
            S.finish()
            return nc


        with ExitStack() as p3:
            kT = sb(p3, "kT", [128, 4, CTX + SEQ], BF16)
            v1 = sb(p3, "v1", [128, NT, 520], BF16)
            woutb = sb(p3, "woutb", [128, 8, D], BF16)
            wrt = sb(p3, "wrt", [128, 8, NE])
            qz = [[sb(p3, f"qz{i}_{c}", [128, 4, 512], BF16) for c in range(2)] for i in range(2)]
            for i in range(2):
                for c in range(2):
                    S.op(pool, lambda h: h.memset(qz[i][c][:], 0.0), wr=[qz[i][c]])
            lruB = [sb(p3, f"lruB{i}", [128, 4, 512], BF16) for i in range(2)]
            Eb = [sb(p3, f"Eb{i}", [128, 1024], BF16) for i in range(3)]
            osb = [sb(p3, f"osb{i}", [128, 4, 128]) for i in range(2)]
            rl = sb(p3, "rl", [128, 8])
            ssn = sb(p3, "ssn", [128, 8])
            rsn = sb(p3, "rsn", [128, 8])
            junk3 = sb(p3, "junk3", [128, D], BF16)
            att = sb(p3, "att", [128, 4, 512], BF16)
            attT = sb(p3, "attT", [128, 4, 512], BF16)
            xres = [sb(p3, f"xres{i}", [128, D]) for i in range(2)]
            x1t = [sb(p3, f"x1t{i}", [128, D]) for i in range(2)]
            tmp3s = [sb(p3, f"tmp3{i}", [128, D]) for i in range(2)]
            h2fs = [sb(p3, f"h2f{i}", [128, D]) for i in range(2)]
            h2b = [sb(p3, f"h2b{i}", [128, D], BF16) for i in range(2)]
            h2Ts = [sb(p3, f"h2T{i}", [128, 8, 128]) for i in range(2)]
            lg = sb(p3, "lg", [128, NE])
            mx = sb(p3, "mx", [128, 4])
            S.dma(sp, lambda h: h.dma_start(out=kT[:], in_=kT_d.rearrange("c p t -> p c t")), rd=[R_kT], wr=[kT])
            S.dma(sp, lambda h: h.dma_start(out=v1[:], in_=v_d.rearrange("t p f -> p t f")), rd=[R_v], wr=[v1])
            S.dma(sp, lambda h: h.dma_start(out=wrt[:], in_=wr_d.rearrange("(kc p) n -> p kc n", p=128)), wr=[wrt])
            for kc in range(8):
                S.dma(pool, lambda h: h.dma_start(out=woutb[:, kc, :], in_=wout_d[kc * 128:(kc + 1) * 128, :]),
                      wr=[woutb])
            ssn2 = sb(p3, "ssn2", [128, 2])
            rsn2 = sb(p3, "rsn2", [128, 2])
            junk4 = sb(p3, "junk4", [128, D], BF16)

            def tail_steps(qb, lb, B0, B1):
                steps = []
                for s_ in range(4):
                    def st_t(s_=s_):
                        for hd in range(4):
                            S.op(pe, lambda h: h.transpose(out=bank_bf(B0)[:, hd * 128:(hd + 1) * 128],
                                                           in_=att[:, s_, hd * 128:(hd + 1) * 128], identity=ident_b),
                                 rd=[att, cstb], wr=[bankR[B0]], self_sync=False)
                        S.op(act, lambda h: h.copy(out=attT[:, :, s_ * 128:(s_ + 1) * 128],
                                                   in_=bank_bf(B0)[:, 0:512].rearrange("p (k t) -> p k t", t=128)),
                             rd=[bankR[B0]], wr=[attT])
                    steps.append(st_t)
                per_tile = []
                for s_ in range(4):
                    tok0 = qb * 512 + s_ * 128
                    tl = qb * 4 + s_
                    xr, x1 = xres[s_ % 2], x1t[s_ % 2]
                    hb_ = h2b[s_ % 2]
                    tmp3, h2f, h2T = tmp3s[s_ % 2], h2fs[s_ % 2], h2Ts[s_ % 2]

                    def st_w(fh, s_=s_, tok0=tok0, xr=xr, tmp3=tmp3):
                        bk = B0 if fh == 0 else B1
                        if fh == 0:
                            S.dma(sp, lambda h: h.dma_start(out=xr[:], in_=x_d[tok0:tok0 + 128, :]), wr=[xr])
                        for kc in range(8):
                            lhs = (lb[:, kc, s_ * 128:(s_ + 1) * 128] if kc < 4 else attT[:, kc - 4, s_ * 128:(s_ + 1) * 128])
                            S.op(pe, lambda h: h.matmul(bank(bk), lhsT=lhs, rhs=woutb[:, kc, fh * 512:(fh + 1) * 512],
                                                        start=(kc == 0), stop=(kc == 7)),
                                 rd=[lb, attT, woutb], wr=[bankR[bk]], self_sync=False)
                        S.op(dve, lambda h: h.tensor_tensor(out=tmp3[:, fh * 512:(fh + 1) * 512], in0=bank(bk),
                                                            in1=bcG[:, 0, fh * 512:(fh + 1) * 512], op=ALU.mult),
                             rd=[bankR[bk], bcG], wr=[tmp3])
                    tile_steps = {}
                    tile_steps["w0"] = (lambda st_w=st_w: st_w(0))

                    cc = s_ % 2

                    def st_c1(st_w=st_w, tok0=tok0, xr=xr, x1=x1, tmp3=tmp3, cc=cc):
                        st_w(1)
                        S.op(dve, lambda h: h.tensor_tensor(out=x1[:], in0=tmp3[:], in1=xr[:], op=ALU.add),
                             rd=[tmp3, xr], wr=[x1])
                        S.dma(sp, lambda h: h.dma_start(out=out_d[tok0:tok0 + 128, :], in_=x1[:]), rd=[x1], wr=[R_out])
                        S.op(act, lambda h: h.activation(out=junk4[:], in_=x1[:], func=AF.Square, accum_out=ssn2[:, cc:cc + 1]),
                             rd=[x1], wr=[junk4, ssn2])
                        S.op(act, lambda h: h.activation(out=rsn2[:, cc:cc + 1], in_=ssn2[:, cc:cc + 1], func=AF.Ln, scale=1.0 / D,
                                                         bias=epsT[:, 0:1]), rd=[ssn2, epsT], wr=[rsn2])
                        S.op(act, lambda h: h.activation(out=rsn2[:, cc:cc + 1], in_=rsn2[:, cc:cc + 1], func=AF.Exp, scale=-0.5),
                             rd=[rsn2], wr=[rsn2])
                    tile_steps["c1"] = st_c1

                    def st_c2(tok0=tok0, x1=x1, hb_=hb_, tmp3=tmp3, h2f=h2f, cc=cc):
                        S.op(dve, lambda h: h.scalar_tensor_tensor(out=tmp3[:], in0=x1[:], scalar=rsn2[:, cc:cc + 1], in1=bcG[:, 1, :],
                                                                   op0=ALU.mult, op1=ALU.mult), rd=[x1, rsn2, bcG], wr=[tmp3])
                        S.op(dve, lambda h: h.tensor_tensor(out=h2f[:], in0=tmp3[:], in1=bcG[:, 2, :], op=ALU.add),
                             rd=[tmp3, bcG], wr=[h2f])
                        S.op(act, lambda h: h.copy(out=hb_[:], in_=h2f[:]), rd=[h2f], wr=[hb_])
                        S.dma(sp, lambda h: h.dma_start(out=h2_d[tok0:tok0 + 128, :], in_=hb_[:]), rd=[hb_], wr=[R_h2])
                    tile_steps["c2"] = st_c2

                    def st_r(half, h2f=h2f, h2T=h2T):
                        for k4 in range(4):
                            kc = half * 4 + k4
                            S.op(pe, lambda h: h.transpose(out=bank(B0)[:, k4 * 128:(k4 + 1) * 128],
                                                           in_=h2f[:, kc * 128:(kc + 1) * 128], identity=ident_f),
                                 rd=[h2f, cst], wr=[bankR[B0]], self_sync=False)
                        S.op(dve, lambda h: h.tensor_copy(out=h2T[:, half * 4:half * 4 + 4, :],
                                                          in_=bank(B0).rearrange("p (k t) -> p k t", t=128)),
                             rd=[bankR[B0]], wr=[h2T])
                    tile_steps["r0"] = (lambda st_r=st_r: st_r(0))
                    tile_steps["r1"] = (lambda st_r=st_r: st_r(1))

                    def st_s(tl=tl, h2T=h2T):
                        for kc in range(8):
                            S.op(pe, lambda h: h.matmul(bank(B0)[:, 0:NE], lhsT=h2T[:, kc, :], rhs=wrt[:, kc, :],
                                                        start=(kc == 0), stop=(kc == 7)),
                                 rd=[h2T, wrt], wr=[bankR[B0]], self_sync=False)
                        S.op(dve, lambda h: h.reduce_max(out=mx[:, 0:1], in_=bank(B0)[:, 0:NE], axis=AX.X),
                             rd=[bankR[B0]], wr=[mx])
                        S.op(dve, lambda h: h.tensor_scalar_mul(out=mx[:, 1:2], in0=mx[:, 0:1], scalar1=-1.0), rd=[mx], wr=[mx])
                        S.op(act, lambda h: h.activation(out=lg[:], in_=bank(B0)[:, 0:NE], func=AF.Exp, bias=mx[:, 1:2],
                                                         accum_out=mx[:, 2:3]), rd=[bankR[B0], mx], wr=[lg, mx])
                        S.op(dve, lambda h: h.reciprocal(out=mx[:, 3:4], in_=mx[:, 2:3]), rd=[mx], wr=[mx])
                        S.op(dve, lambda h: h.tensor_scalar_mul(out=aff[:, tl, :], in0=lg[:], scalar1=mx[:, 3:4]),
                             rd=[lg, mx], wr=[aff])
                    tile_steps["sm"] = st_s
                    per_tile.append(tile_steps)
                order = [("w0", 0), ("c1", 0), ("w0", 1), ("c1", 1), ("c2", 0), ("r0", 0), ("c2", 1), ("r1", 0),
                         ("w0", 2), ("c1", 2), ("sm", 0), ("r0", 1), ("r1", 1), ("c2", 2), ("w0", 3), ("c1", 3), ("sm", 1),
                         ("r0", 2), ("r1", 2), ("c2", 3), ("sm", 2), ("r0", 3), ("r1", 3), ("sm", 3)]
                for (k_, t_) in order:
                    steps.append(per_tile[t_][k_])
                return steps

            pending = []
            deferred = []
            gcnt = {"g": 0}
            for qb in range(8):
                qt = qz[qb % 2]
                for c in range(2):
                    S.dma(sp, lambda h: h.dma_start(
                        out=qt[c][c * 64:(c + 1) * 64, :, :],
                        in_=qT_d.rearrange("c p t -> p c t")[c * 64:(c + 1) * 64, :, qb * 512:(qb + 1) * 512]),
                        rd=[R_qT], wr=[qt[c]])
                lb = lruB[qb % 2]
                S.dma(sp, lambda h: h.dma_start(out=lb[:], in_=lru_d.rearrange("c p t -> p c t")[:, :, qb * 512:(qb + 1) * 512]),
                      rd=[R_lru], wr=[lb])
                NP = NT // 2
                items = [(hd, c, kp) for hd in range(4) for c in range(2) for kp in range(NP)]

                def emit_S(i):
                    hd, c, kp = items[i]
                    pb = (i % 2) * 2
                    for u in range(2):
                        kt = kp * 2 + u
                        S.op(pe, lambda h: h.matmul(bank(pb + u), lhsT=kT[:, hd, kt * 128:(kt + 1) * 128],
                                                    rhs=qt[c][:, hd, :], start=True, stop=True),
                             rd=[kT, qt[c]], wr=[bankR[pb + u]], self_sync=False)

                emit_S(0)
                emit_S(1)
                for i, (hd, c, kp) in enumerate(items):
                    os_ = osb[hd % 2]
                    pb = (i % 2) * 2
                    ab = 4 + 2 * ((hd * 2 + c) % 2)
                    E = Eb[i % 3]
                    S.op(act, lambda h: h.activation(out=E[:], in_=PS[:, pb:pb + 2, :].rearrange("p a n -> p (a n)"),
                                                     func=AF.Exp, bias=sc2[:, 1:2]),
                         rd=[bankR[pb], bankR[pb + 1], sc2], wr=[E])
                    if i + 2 < len(items):
                        emit_S(i + 2)
                    for u in range(2):
                        kt = kp * 2 + u
                        for s_ in range(4):
                            bb = ab + s_ // 2
                            co = (s_ % 2) * 256
                            S.op(pe, lambda h: h.matmul(bank(bb)[:, co:co + 129],
                                                        lhsT=E[:, u * 512 + s_ * 128:u * 512 + (s_ + 1) * 128],
                                                        rhs=v1[:, kt, hd * 130:hd * 130 + 129],
                                                        start=(kt == 0 and s_ % 2 == 0), stop=(kt == NT - 1),
                                                        skip_group_check=True),
                                 rd=[E, v1], wr=[bankR[bb]], self_sync=False)
                    gcnt["g"] += 1
                    while deferred and deferred[0][0] <= gcnt["g"]:
                        deferred.pop(0)[1]()
                    if not deferred and i < NP - 1:
                        for _ in range(2):
                            if pending:
                                pending.pop(0)()
                    elif i == NP - 1:
                        while deferred:
                            deferred.pop(0)[1]()
                        while pending:
                            pending.pop(0)()
                    if kp < NP - 1:
                        continue
                    for s_ in range(4):
                        bb = ab + s_ // 2
                        co = (s_ % 2) * 256
                        S.op(dve, lambda h: h.reciprocal(out=rl[:, s_:s_ + 1], in_=bank(bb)[:, co + 128:co + 129]),
                             rd=[bankR[bb]], wr=[rl])
                        if c == 0:
                            S.op(dve, lambda h: h.tensor_scalar_mul(out=os_[:, s_, :], in0=bank(bb)[:, co:co + 128],
                                                                    scalar1=rl[:, s_:s_ + 1]),
                                 rd=[bankR[bb], rl], wr=[os_])
                        else:
                            S.op(dve, lambda h: h.tensor_tensor(out=rl[:, 4 + s_:5 + s_], in0=rl[:, s_:s_ + 1],
                                                                in1=sc2[:, 0:1], op=ALU.mult), rd=[rl, sc2], wr=[rl])
                            S.op(dve, lambda h: h.scalar_tensor_tensor(out=os_[:, s_, :], in0=bank(bb)[:, co:co + 128],
                                                                       scalar=rl[:, 4 + s_:5 + s_], in1=os_[:, s_, :],
                                                                       op0=ALU.mult, op1=ALU.add),
                                 rd=[bankR[bb], rl, os_], wr=[os_])
                    if c == 0:
                        continue
                    def sub_sq(s_, os_=os_):
                        S.op(act, lambda h: h.activation(out=junk3[:, 0:128], in_=os_[:, s_, :], func=AF.Square,
                                                         accum_out=ssn[:, s_:s_ + 1]), rd=[os_], wr=[junk3, ssn])

                    def sub_fin(hd=hd, os_=os_):
                        S.op(act, lambda h: h.activation(out=rsn[:, 0:4], in_=ssn[:, 0:4], func=AF.Ln, scale=1.0 / 128,
                                                         bias=epsT[:, 0:1]), rd=[ssn, epsT], wr=[rsn])
                        S.op(act, lambda h: h.activation(out=rsn[:, 0:4], in_=rsn[:, 0:4], func=AF.Exp, scale=-0.5),
                             rd=[rsn], wr=[rsn])
                        for s_ in range(4):
                            S.op(dve, lambda h: h.scalar_tensor_tensor(out=att[:, s_, hd * 128:(hd + 1) * 128], in0=os_[:, s_, :],
                                                                       scalar=rsn[:, s_:s_ + 1], in1=bcs[:, hd * 128:(hd + 1) * 128],
                                                                       op0=ALU.mult, op1=ALU.mult),
                                 rd=[os_, rsn, bcs], wr=[att])
                    for s_ in range(4):
                        deferred.append((gcnt["g"] + 2 + s_ // 2, (lambda sub_sq=sub_sq, s_=s_: sub_sq(s_))))
                    deferred.append((gcnt["g"] + 4, sub_fin))
                pending = tail_steps(qb, lb, 6, 7)
            while deferred:
                deferred.pop(0)[1]()
            for st in pending:
                st()
            if dbg:
                S.dma(sp, lambda h: h.dma_start(out=dbg_aff, in_=aff[:].rearrange("p t e -> p (t e)")), rd=[aff])
        p03.close()
        S.barrier()
        if stop_after < 4:
            S.finish()
            return nc

        p45 = top.enter_context(ExitStack())
        NGU = 4
        NB = 11
        wgb = [sb(p45, f"wgb{i}", [128, 8, 256], BF16) for i in range(NGU)]
        wub = [sb(p45, f"wub{i}", [128, 8, 256], BF16) for i in range(NGU)]
        wdb = [sb(p45, f"wdb{i}", [128, NJ, 512], BF16) for i in range(2)]

        def load_gu(e, b):
            i = (e * NB + b) % NGU
            for (dst, srcw) in ((wgb[i], wg_d), (wub[i], wu_d)):
                S.dma(pool, lambda h: h.dma_start(
                    out=dst[:], in_=srcw[e].rearrange("(kc p) n -> p kc n", p=128)[:, :, b * 256:(b + 1) * 256]),
                    wr=[dst])

        def load_d(e, fh):
            dst = wdb[fh]
            S.dma(pool, lambda h: h.dma_start(
                out=dst[:], in_=wd_d[e].rearrange("(j p) f -> p j f", p=128)[:, :, fh * 512:(fh + 1) * 512]),
                wr=[dst])

        if stop_after >= 5:
            for b_ in range(NGU):
                load_gu(0, b_)
            load_d(0, 0)
            load_d(0, 1)

        with ExitStack() as p4:
            lo = sb(p4, "lo", [128, NE])
            hi = sb(p4, "hi", [128, NE])
            mid = sb(p4, "mid", [128, NE])
            ta = sb(p4, "ta", [128, NE])
            cnt16 = sb(p4, "cnt16", [128, NE])
            cmpb = sb(p4, "cmpb", [128, 32, NE], BF16)
            maskb = sb(p4, "maskb", [128, 32, NE], BF16)
            cum = sb(p4, "cum", [128, 32, NE], BF16)
            posm = sb(p4, "posm", [128, 32, NE])
            TI = sb(p4, "TI", [128, NE, 32, 8], BF16)
            rres = sb(p4, "rres", [128, 32, NE])
            Sb = [sb(p4, f"Sb{i}", [128, 512], BF16) for i in range(4)]
            idxf = sb(p4, "idxf", [128, 4])
            pvs = sb(p4, "pvs", [128, 32])
            S.op(dve, lambda h: h.memset(lo[:], 0.0), wr=[lo])
            S.op(dve, lambda h: h.memset(hi[:], 1.0), wr=[hi])
            S.op(dve, lambda h: h.memset(mid[:], 0.5), wr=[mid])
            for it in range(28):
                S.op(dve, lambda h: h.tensor_tensor(out=cmpb[:], in0=aff[:],
                                                    in1=mid[:].unsqueeze(1).broadcast_to([128, 32, NE]), op=ALU.is_ge),
                     rd=[aff, mid], wr=[cmpb])
                S.op(pe, lambda h: h.matmul(bank(0), lhsT=ones_b, rhs=cmpb[:].rearrange("p t e -> p (t e)"),
                                            start=True, stop=True),
                     rd=[cmpb, cstb], wr=[bankR[0]], self_sync=False)
                S.op(dve, lambda h: h.tensor_reduce(out=cnt16[:], in_=bank(0).rearrange("p (t e) -> p e t", e=NE),
                                                    axis=AX.X, op=ALU.add), rd=[bankR[0]], wr=[cnt16])
                S.op(dve, lambda h: h.scalar_tensor_tensor(out=ta[:], in0=cnt16[:], scalar=float(CAP) - 0.5,
                                                           in1=mid[:], op0=ALU.is_ge, op1=ALU.mult),
                     rd=[cnt16, mid], wr=[ta])
                S.op(dve, lambda h: h.tensor_tensor(out=lo[:], in0=lo[:], in1=ta[:], op=ALU.max), rd=[lo, ta], wr=[lo])
                S.op(dve, lambda h: h.scalar_tensor_tensor(out=ta[:], in0=cnt16[:], scalar=float(CAP) - 0.5,
                                                           in1=mid[:], op0=ALU.is_ge, op1=ALU.add),
                     rd=[cnt16, mid], wr=[ta])
                S.op(dve, lambda h: h.tensor_tensor(out=hi[:], in0=hi[:], in1=ta[:], op=ALU.min), rd=[hi, ta], wr=[hi])
                S.op(dve, lambda h: h.tensor_tensor(out=mid[:], in0=lo[:], in1=hi[:], op=ALU.add), rd=[lo, hi], wr=[mid])
                S.op(dve, lambda h: h.tensor_scalar_mul(out=mid[:], in0=mid[:], scalar1=0.5), rd=[mid], wr=[mid])
            S.op(dve, lambda h: h.tensor_tensor(out=maskb[:], in0=aff[:],
                                                in1=lo[:].unsqueeze(1).broadcast_to([128, 32, NE]), op=ALU.is_ge),
                 rd=[aff, lo], wr=[maskb])
            S.op(dve, lambda h: h.memset(cum[:, 0, :], 0.0), wr=[cum])
            for t in range(1, 32):
                S.op(dve, lambda h: h.tensor_tensor(out=cum[:, t, :], in0=cum[:, t - 1, :], in1=maskb[:, t - 1, :],
                                                    op=ALU.add), rd=[cum, maskb], wr=[cum])
            S.op(pe, lambda h: h.matmul(bank(1), lhsT=ustr_b, rhs=maskb[:].rearrange("p t e -> p (t e)"),
                                        start=True, stop=False), rd=[maskb, cstb], wr=[bankR[1]], self_sync=False)
            S.op(pe, lambda h: h.matmul(bank(1), lhsT=ones_b, rhs=cum[:].rearrange("p t e -> p (t e)"),
                                        start=False, stop=True), rd=[cum, cstb], wr=[bankR[1]], self_sync=False)
            S.op(dve, lambda h: h.scalar_tensor_tensor(out=posm[:].rearrange("p t e -> p (t e)"), in0=bank(1), scalar=1.0,
                                                       in1=maskb[:].rearrange("p t e -> p (t e)"),
                                                       op0=ALU.add, op1=ALU.mult), rd=[bankR[1], maskb], wr=[posm])
            S.op(dve, lambda h: h.tensor_scalar_add(out=posm[:], in0=posm[:], scalar1=-1.0), rd=[posm], wr=[posm])
            S.op(pool, lambda h: h.memset(TI[:], 0.0), wr=[TI])
            S.op(dve, lambda h: h.tensor_copy(out=TI[:, :, :, 0],
                                              in_=cst[:, 897:929].unsqueeze(1).broadcast_to([128, NE, 32])),
                 rd=[cst, TI], wr=[TI])
            S.op(dve, lambda h: h.tensor_copy(out=TI[:, :, :, 1],
                                              in_=cst[:, 896:897].unsqueeze(1).broadcast_to([128, NE, 32])),
                 rd=[cst, TI], wr=[TI])
            affv = aff[:].rearrange("p t e -> p e t")
            rresv = rres[:].rearrange("p t e -> p e t")
            S.op(dve, lambda h: h.tensor_copy(out=TI[:, :, :, 2], in_=affv), rd=[aff, TI], wr=[TI])
            S.op(dve, lambda h: h.tensor_tensor(out=rresv, in0=affv, in1=TI[:, :, :, 2], op=ALU.subtract),
                 rd=[aff, TI], wr=[rres])
            S.op(dve, lambda h: h.tensor_copy(out=TI[:, :, :, 3], in_=rresv), rd=[rres, TI], wr=[TI])
            S.op(dve, lambda h: h.tensor_tensor(out=rresv, in0=rresv, in1=TI[:, :, :, 3], op=ALU.subtract),
                 rd=[rres, TI], wr=[rres])
            S.op(dve, lambda h: h.tensor_copy(out=TI[:, :, :, 4], in_=rresv), rd=[rres, TI], wr=[TI])
            scn = 0
            for e in range(NE):
                b = 2 + e % 2
                for t in range(32):
                    Sx = Sb[scn % 4]
                    scn += 1
                    S.op(dve, lambda h: h.tensor_scalar(out=Sx[:], in0=iota_f, scalar1=posm[:, t, e:e + 1], scalar2=None,
                                                        op0=ALU.is_equal), rd=[cst, posm], wr=[Sx])
                    for sc in range(4):
                        S.op(pe, lambda h: h.matmul(bank(b)[:, sc * 8:sc * 8 + 8], lhsT=Sx[:, sc * 128:(sc + 1) * 128],
                                                    rhs=TI[:, e, t, :], start=(t == 0 and sc == 0), stop=(t == 31),
                                                    skip_group_check=True),
                             rd=[Sx, TI], wr=[bankR[b]], self_sync=False)
                S.op(dve, lambda h: h.tensor_copy(out=pvs[:], in_=bank(b)[:, 0:32]), rd=[bankR[b]], wr=[pvs])
                pv = pvs[:].rearrange("p (s c) -> p s c", c=8)
                S.op(dve, lambda h: h.scalar_tensor_tensor(out=idxf[:], in0=pv[:, :, 0], scalar=128.0, in1=pv[:, :, 1],
                                                           op0=ALU.mult, op1=ALU.add), rd=[pvs], wr=[idxf])
                S.op(dve, lambda h: h.tensor_copy(out=idx_i[:, e, :], in_=idxf[:]), rd=[idxf], wr=[idx_i])
                S.op(dve, lambda h: h.tensor_tensor(out=gsl[:, e, :], in0=pv[:, :, 2], in1=pv[:, :, 3], op=ALU.add),
                     rd=[pvs], wr=[gsl])
                S.op(dve, lambda h: h.tensor_tensor(out=gsl[:, e, :], in0=gsl[:, e, :], in1=pv[:, :, 4], op=ALU.add),
                     rd=[pvs, gsl], wr=[gsl])
            if dbg:
                S.dma(sp, lambda h: h.dma_start(out=dbg_idx, in_=idx_i[:].rearrange("p e s -> p (e s)")), rd=[idx_i])
                S.dma(sp, lambda h: h.dma_start(out=dbg_g, in_=gsl[:].rearrange("p e s -> p (e s)")), rd=[gsl])
        S.barrier()
        if stop_after < 5:
            S.finish()
            return nc

        with ExitStack() as p5:
            xe = [sb(p5, f"xe{i}", [128, 4, D], BF16) for i in range(2)]
            xeT = [sb(p5, f"xeT{i}", [128, 8, 512], BF16) for i in range(2)]
            hT = [sb(p5, f"hTe{i}", [128, NJ, 512], BF16) for i in range(2)]
            old = [sb(p5, f"old{i}", [128, D]) for i in range(4)]
            sg = [sb(p5, f"sg{i}", [128, 512]) for i in range(2)]
            tmp5 = [sb(p5, f"tmp5{i}", [128, 512]) for i in range(2)]
            xe_r = [[Reg() for _ in range(4)] for _ in range(2)]
            R_osc = [Reg() for _ in range(4)]

            def gather_xe(e, scs=range(4)):
                xg = xe[e % 2]
                for sc in scs:
                    S.dma(pool, lambda h: h.indirect_dma_start(
                        out=xg[:, sc, :], out_offset=None, in_=h2_d[:, :],
                        in_offset=bass.IndirectOffsetOnAxis(ap=idx_i[:, e, sc:sc + 1], axis=0)),
                        rd=[R_h2, idx_i], wr=[xg])

            def gather_old(e, scs=range(4)):
                for sc in scs:
                    S.dma(pool, lambda h: h.indirect_dma_start(
                        out=old[sc][:], out_offset=None, in_=out_d[:, :],
                        in_offset=bass.IndirectOffsetOnAxis(ap=idx_i[:, e, sc:sc + 1], axis=0)),
                        rd=[R_out, idx_i], wr=[old[sc]])

            def make_xeT(e):
                xg, xt_ = xe[e % 2], xeT[e % 2]
                for kc in range(8):
                    for sc in range(4):
                        S.op(pe, lambda h: h.transpose(out=bank_bf(0)[:, sc * 128:(sc + 1) * 128],
                                                       in_=xg[:, sc, kc * 128:(kc + 1) * 128], identity=ident_b),
                             rd=[xg, cstb], wr=[bankR[0]], self_sync=False)
                    S.op(act, lambda h: h.copy(out=xt_[:, kc, :], in_=bank_bf(0)[:, 0:512]), rd=[bankR[0]], wr=[xt_])

            def scatter_new(e, scs=range(4)):
                for sc in scs:
                    S.dma(pool, lambda h: h.indirect_dma_start(
                        out=out_d[:, :], out_offset=bass.IndirectOffsetOnAxis(ap=idx_i[:, e, sc:sc + 1], axis=0),
                        in_=old[sc][:], in_offset=None), rd=[old[sc], idx_i], wr=[R_out])

            gather_xe(0)
            gather_old(0)
            make_xeT(0)
            pcnt = 0
            dcnt = 0
            for e in range(NE):
                xt_ = xeT[e % 2]
                hT_ = hT[e % 2]
                for b in range(NB):
                    i = (e * NB + b) % NGU
                    if b < 4 and e + 1 < NE:
                        gather_xe(e + 1, [b])
                    if 4 <= b < 8 and e > 0:
                        scatter_new(e - 1, [b - 4])
                    if b >= 8 and e > 0:
                        gather_old(e, [b - 8])
                    for jj in range(2):
                        j = b * 2 + jj
                        bg = 1 + (pcnt % 2) * 2
                        pcnt += 1
                        for (bk, wt) in ((bg, wgb[i]), (bg + 1, wub[i])):
                            for kc in range(8):
                                S.op(pe, lambda h: h.matmul(bank(bk), lhsT=wt[:, kc, jj * 128:(jj + 1) * 128], rhs=xt_[:, kc, :],
                                                            start=(kc == 0), stop=(kc == 7)),
                                     rd=[wt, xt_], wr=[bankR[bk]], self_sync=False)
                        sg_ = sg[j % 2]
                        S.op(act, lambda h: h.activation(out=sg_[:], in_=bank(bg), func=AF.Silu), rd=[bankR[bg]], wr=[sg_])
                        S.op(dve, lambda h: h.tensor_tensor(out=hT_[:, j, :], in0=sg_[:], in1=bank(bg + 1), op=ALU.mult),
                             rd=[sg_, bankR[bg + 1]], wr=[hT_])
                    nb_ = b + NGU
                    if nb_ < NB:
                        load_gu(e, nb_)
                    elif e + 1 < NE:
                        load_gu(e + 1, nb_ - NB)
                if e > 0:
                    gather_old(e, [3])
                if e + 1 < NE:
                    make_xeT(e + 1)
                for fh in range(2):
                    wd_ = wdb[fh]
                    for sc in range(4):
                        bk = 5 + dcnt % 3
                        dcnt += 1
                        for j in range(NJ):
                            S.op(pe, lambda h: h.matmul(bank(bk), lhsT=hT_[:, j, sc * 128:(sc + 1) * 128], rhs=wd_[:, j, :],
                                                        start=(j == 0), stop=(j == NJ - 1)),
                                 rd=[hT_, wd_], wr=[bankR[bk]], self_sync=False)
                        t5 = tmp5[sc % 2]
                        S.op(dve, lambda h: h.scalar_tensor_tensor(out=t5[:], in0=bank(bk), scalar=gsl[:, e, sc:sc + 1],
                                                                   in1=bcG[:, 3, fh * 512:(fh + 1) * 512],
                                                                   op0=ALU.mult, op1=ALU.mult),
                             rd=[bankR[bk], gsl, bcG], wr=[t5])
                        S.op(dve, lambda h: h.tensor_tensor(out=old[sc][:, fh * 512:(fh + 1) * 512],
                                                             in0=old[sc][:, fh * 512:(fh + 1) * 512], in1=t5[:], op=ALU.add),
                             rd=[old[sc], t5], wr=[old[sc]])
                    if e + 1 < NE:
                        load_d(e + 1, fh)
            scatter_new(NE - 1)
        S.finish()
    return nc


def _consts():
    c = np.zeros((128, NCONST), np.float32)
    c[:, 0:128] = np.eye(128, dtype=np.float32)
    p = np.arange(128)
    c[:, 128:256] = (p[:, None] < p[None, :]).astype(np.float32)
    c[:, 256:384] = 1.0
    c[:, 384:896] = np.arange(512, dtype=np.float32)[None, :]
    c[:, 896] = p.astype(np.float32)
    c[:, 897:929] = np.arange(32, dtype=np.float32)[None, :]
    return c


def _rope_table16():
    tab = np.zeros((128, NT, 2, 64), np.float32)
    tab[:, :, 0, :] = 1.0
    inv = (np.float32(10000.0) ** (-np.arange(16, dtype=np.float32) / np.float32(16))).astype(np.float32)
    for T in range(2, NT):
        tok = (T - 2) * 128 + np.arange(128)
        row = (tok // 64).astype(np.float32)
        col = (tok % 64).astype(np.float32)
        ar = (row[:, None] * inv[None, :]).astype(np.float32)
        ac = (col[:, None] * inv[None, :]).astype(np.float32)
        cr, sr, cc, sc_ = np.cos(ar), np.sin(ar), np.cos(ac), np.sin(ac)
        tab[:, T, 0, 0:16] = cr
        tab[:, T, 0, 16:32] = cr
        tab[:, T, 0, 32:48] = cc
        tab[:, T, 0, 48:64] = cc
        tab[:, T, 1, 0:16] = -sr
        tab[:, T, 1, 16:32] = sr
        tab[:, T, 1, 32:48] = -sc_
        tab[:, T, 1, 48:64] = sc_
    return tab.reshape(128, NT * 128)


def make_in_maps(inp, cores):
    f = lambda a: np.ascontiguousarray(np.asarray(a, dtype=np.float32))
    L = 0
    c_ctx = f(inp["c_ctx"])
    conv_w = f(inp["conv_w"][L])
    convw = np.zeros((128, 16), np.float32)
    for j in range(4):
        for k in range(4):
            convw[:, j * 4 + k] = conv_w[k, j * 128:(j + 1) * 128]
    convb = f(inp["conv_b"][L]).reshape(4, 128).T
    lruw = np.zeros((128, 2, 2, 4, 128), np.float32)
    lrub = np.zeros((128, 2, 2, 4), np.float32)
    for d in range(2):
        for gi, (wn, bn) in enumerate((("lru_wa", "lru_ba"), ("lru_wi", "lru_bi"))):
            w = f(inp[wn][L][d])
            bb = f(inp[bn][L][d])
            for j in range(4):
                for hh in range(2):
                    lruw[hh * 64:(hh + 1) * 64, d, gi, j, hh * 64:(hh + 1) * 64] = w[2 * j + hh]
                    lrub[hh * 64:(hh + 1) * 64, d, gi, j] = bb[2 * j + hh]
    lrul = np.zeros((128, 2, 4), np.float32)
    lam = f(inp["lru_lambda"][L])
    for d in range(2):
        lrul[:, d, :] = lam[d].reshape(4, 128).T
    shared = {
        "w_ada": f(inp["w_ada"][L]),
        "b_ada": f(inp["b_ada"][L]).reshape(1, -1),
        "normg": np.concatenate([f(inp["norm1_g"][L]), f(inp["norm2_g"][L])]).reshape(1, -1),
        "qkg": np.concatenate([np.tile(f(inp["q_norm_g"][L]), 8), np.tile(f(inp["k_norm_g"][L]), 8)]).reshape(1, -1),
        "lamv": np.concatenate([f(inp["lambda_q1"][L]), f(inp["lambda_k1"][L]),
                                f(inp["lambda_q2"][L]), f(inp["lambda_k2"][L])]).reshape(1, -1),
        "subg": np.tile(f(inp["subln_g"][L]), 4).reshape(1, -1),
        "w_in": f(inp["w_in"][L]),
        "convw": convw,
        "convb": np.ascontiguousarray(convb),
        "lruw": np.ascontiguousarray(lruw.reshape(128, 2048)),
        "lrub": np.ascontiguousarray(lrub.reshape(128, 16)),
        "lrul": np.ascontiguousarray(lrul.reshape(128, 8)),
        "w_out": f(inp["w_out"][L]),
        "w_router": f(inp["w_router"][L]),
        "w_gate": f(inp["w_gate"][L]),
        "w_up": f(inp["w_up"][L]),
        "w_down": f(inp["w_down"][L]),
        "rope": _rope_table16(),
        "consts": _consts(),
    }
    maps = []
    for b in cores:
        cvec = np.zeros((128, 16), np.float32)
        cvec[:, 0:8] = f(inp["c"][b]).reshape(8, 128).T
        cvec[:, 8:16] = c_ctx.reshape(8, 128).T
        m = dict(shared)
        m["x"] = f(inp["x"][b])
        m["ctx"] = f(inp["ctx"][b])
        m["cvec"] = cvec
        maps.append(m)
    return maps


def kernel(**inputs):
    nc = build()
    maps = make_in_maps(inputs, list(range(8)))
    res = run_bass_kernel_spmd(nc, maps, core_ids=list(range(8)))
    return np.stack([np.asarray(r["out"], dtype=np.float32) for r in res.results], axis=0)
```

```python
import math
from contextlib import ExitStack

import numpy as np
import concourse.bass as bass
import concourse.mybir as mybir
from concourse.bass_utils import run_bass_kernel_spmd

F32 = mybir.dt.float32
BF16 = mybir.dt.bfloat16
I32 = mybir.dt.int32
AF = mybir.ActivationFunctionType
ALU = mybir.AluOpType
AX = mybir.AxisListType

D = 1024
SEQ = 4096
CTX = 256
NT = 34
NE = 16
CAP = 512
DEXP = 2816
NJ = 22
EPS = 1e-6
LAM_INIT = 0.2
NCONST = 936


class Reg:
    __slots__ = ("w", "rs")

    def __init__(self):
        self.w = {}
        self.rs = {}


class Tile:
    def __init__(self, t):
        self.t = t
        self.r = Reg()

    def __getitem__(self, k):
        return self.t[k]


class Eng:
    def __init__(self, h, key, dkeys):
        self.h = h
        self.key = key
        self.n = 0
        self.seen = {}
        self.dkeys = dkeys
        self.dvals = [0] * len(dkeys)
        self.di = 0


def _reg(x):
    return x.r if isinstance(x, Tile) else x


class Sched:
    def __init__(self, nc, stack):
        self.nc = nc
        self.sems = []

        def mk(name):
            s = stack.enter_context(nc.semaphore(name))
            self.sems.append(s)
            return len(self.sems) - 1

        self.pe = Eng(nc.tensor, mk("s_pe"), [])
        self.act = Eng(nc.scalar, mk("s_act"), [])
        self.dve = Eng(nc.vector, mk("s_dve"), [])
        self.pool = Eng(nc.gpsimd, mk("s_pool"), [mk(f"d_pool{i}") for i in range(12)])
        self.sp = Eng(nc.sync, mk("s_sp"), [mk(f"d_sp{i}") for i in range(12)])
        self.engs = [self.pe, self.act, self.dve, self.pool, self.sp]

    def _deps(self, rd, wr):
        deps = {}

        def add(k, v):
            if deps.get(k, 0) < v:
                deps[k] = v

        for r in rd:
            r = _reg(r)
            for k, v in r.w.items():
                add(k, v)
        for r in wr:
            r = _reg(r)
            for k, v in r.w.items():
                add(k, v)
            for k, v in r.rs.items():
                add(k, v)
        return deps

    def _wait(self, e, deps, self_sync):
        for k, v in deps.items():
            if k == e.key and not self_sync:
                continue
            if e.seen.get(k, 0) >= v:
                continue
            e.h.wait_ge(self.sems[k], v)
            e.seen[k] = v

    def _mark(self, ev, rd, wr):
        k, v = ev
        for r in rd:
            r = _reg(r)
            if r.rs.get(k, 0) < v:
                r.rs[k] = v
        for r in wr:
            r = _reg(r)
            r.w[k] = v
            r.rs = {}

    def op(self, e, fn, rd=(), wr=(), self_sync=True):
        self._wait(e, self._deps(rd, wr), self_sync)
        ins = fn(e.h)
        e.n += 1
        ins.then_inc(self.sems[e.key], 1)
        self._mark((e.key, e.n), rd, wr)
        return ins

    def dma(self, q, fn, rd=(), wr=()):
        self._wait(q, self._deps(rd, wr), True)
        slot = q.di % len(q.dkeys)
        q.di += 1
        k = q.dkeys[slot]
        prev = q.dvals[slot]
        if q.seen.get(k, 0) < prev:
            q.h.wait_ge(self.sems[k], prev)
            q.seen[k] = prev
        ins = fn(q.h)
        q.dvals[slot] = prev + 16
        ins.then_inc(self.sems[k], 16)
        self._mark((k, prev + 16), rd, wr)
        return ins

    def barrier(self):
        for e in self.engs:
            for o in self.engs:
                if o.n > 0 and e.seen.get(o.key, 0) < o.n and o is not e:
                    e.h.wait_ge(self.sems[o.key], o.n)
                    e.seen[o.key] = o.n
                for k, v in zip(o.dkeys, o.dvals):
                    if v > 0 and e.seen.get(k, 0) < v:
                        e.h.wait_ge(self.sems[k], v)
                        e.seen[k] = v

    def finish(self):
        q = self.sp
        for e in self.engs:
            for k, v in zip(e.dkeys, e.dvals):
                if v > 0 and q.seen.get(k, 0) < v:
                    q.h.wait_ge(self.sems[k], v)
                    q.seen[k] = v
        for e in self.engs:
            if e is not q and e.n > 0:
                q.h.wait_ge(self.sems[e.key], e.n)


def build(dbg=False, stop_after=99):
    nc = bass.Bass("TRN2", target_bir_lowering=False)
    okind = "ExternalOutput" if dbg else "Internal"

    def din(name, shape, dt=F32):
        return nc.dram_tensor(name, list(shape), dt, kind="ExternalInput").ap()

    x_d = din("x", [SEQ, D])
    ctx_d = din("ctx", [CTX, D])
    cvec_d = din("cvec", [128, 16])
    wada_d = din("w_ada", [D, 6 * D])
    bada_d = din("b_ada", [1, 6 * D])
    normg_d = din("normg", [1, 2 * D])
    qkg_d = din("qkg", [1, 1024])
    lamv_d = din("lamv", [1, 256])
    subg_d = din("subg", [1, 512])
    win_d = din("w_in", [D, 2560])
    convw_d = din("convw", [128, 16])
    convb_d = din("convb", [128, 4])
    lruw_d = din("lruw", [128, 2048])
    lrub_d = din("lrub", [128, 16])
    lrul_d = din("lrul", [128, 8])
    wout_d = din("w_out", [D, D])
    wr_d = din("w_router", [D, NE])
    big = stop_after >= 5
    wg_d = din("w_gate", [NE, D, DEXP] if big else [1, 8, 8])
    wu_d = din("w_up", [NE, D, DEXP] if big else [1, 8, 8])
    wd_d = din("w_down", [NE, DEXP, D] if big else [1, 8, 8])
    rope_d = din("rope", [128, NT * 128])
    consts_d = din("consts", [128, NCONST])
    out_d = nc.dram_tensor("out", [SEQ, D], F32, kind="ExternalOutput").ap()

    xl_d = nc.dram_tensor("xl_s", [4, 128, CTX + SEQ], F32, kind=okind).ap()
    gel_d = nc.dram_tensor("gel_s", [4, 128, SEQ], BF16, kind=okind).ap()
    qT_d = nc.dram_tensor("qT_s", [4, 128, SEQ], BF16, kind=okind).ap()
    kT_d = nc.dram_tensor("kT_s", [4, 128, CTX + SEQ], BF16, kind=okind).ap()
    v_d = nc.dram_tensor("v_s", [NT, 128, 520], BF16, kind=okind).ap()
    h2_d = nc.dram_tensor("h2_s", [SEQ, D], BF16, kind=okind).ap()
    lru_d = nc.dram_tensor("lru_s", [4, 128, SEQ], BF16, kind=okind).ap()
    if dbg:
        dbg_mod = nc.dram_tensor("dbg_mod", [1, 8192], F32, kind="ExternalOutput").ap()
        dbg_aff = nc.dram_tensor("dbg_aff", [128, 32 * NE], F32, kind="ExternalOutput").ap()
        dbg_idx = nc.dram_tensor("dbg_idx", [128, NE * 4], I32, kind="ExternalOutput").ap()
        dbg_g = nc.dram_tensor("dbg_g", [128, NE * 4], F32, kind="ExternalOutput").ap()

    R_xl = [Reg() for _ in range(4)]
    R_gel, R_qT, R_kT, R_v, R_h2, R_out, R_lru = Reg(), Reg(), Reg(), Reg(), Reg(), Reg(), Reg()

    with ExitStack() as top:
        S = Sched(nc, top)
        pe, act, dve, pool, sp = S.pe, S.act, S.dve, S.pool, S.sp

        def sb(stack, name, shape, dt=F32):
            return Tile(stack.enter_context(nc.sbuf_tensor("t_" + name, list(shape), dt)))

        PS = top.enter_context(nc.psum_tensor("PS", [128, 8, 512], F32))
        bankR = [Reg() for _ in range(8)]

        def bank(b):
            return PS[:, b, :]

        def bank_bf(b):
            return PS[:, b, :].bitcast(BF16)

        cst = sb(top, "cst", [128, NCONST])
        cstb = sb(top, "cstb", [128, 384], BF16)
        bcG = sb(top, "bcG", [128, 4, D])
        aff = sb(top, "aff", [128, 32, NE])
        idx_i = sb(top, "idx_i", [128, NE, 4], I32)
        gsl = sb(top, "gsl", [128, NE, 4])
        A1, B1, A1C, B1C, G1, A2, B2, G2 = range(8)
        sc2 = sb(top, "sc2", [128, 2])
        epsT = sb(top, "epsT", [128, 1])
        S.dma(sp, lambda h: h.dma_start(out=cst[:], in_=consts_d), wr=[cst])
        S.op(dve, lambda h: h.tensor_copy(out=cstb[:], in_=cst[:, 0:384]), rd=[cst], wr=[cstb])
        S.op(dve, lambda h: h.memset(epsT[:], EPS), wr=[epsT])
        ident_f = cst[:, 0:128]
        ones_f = cst[:, 256:384]
        iota_f = cst[:, 384:896]
        ident_b = cstb[:, 0:128]
        ustr_b = cstb[:, 128:256]
        ones_b = cstb[:, 256:384]

        def rsqrt_ops(dst, src, scale, n, lo=0):
            S.op(act, lambda h: h.activation(out=dst[:, lo:n], in_=src[:, lo:n], func=AF.Sqrt,
                                             scale=scale, bias=epsT[:, 0:1]),
                 rd=[src, epsT], wr=[dst])
            S.op(dve, lambda h: h.reciprocal(out=dst[:, lo:n], in_=dst[:, lo:n]), rd=[dst], wr=[dst])

        p03 = top.enter_context(ExitStack())
        bcs = sb(p03, "bcs", [128, 512])
        p01 = p03.enter_context(ExitStack())
        bcq = sb(p01, "bcq", [128, 1024])
        bcA = sb(p01, "bcA", [128, 4, D])

        def bcr(i):
            return (bcA, i) if i < 4 else (bcG, i - 4)

        winb = sb(p01, "winb", [128, 8, 2560], BF16)
        ropeT = sb(p01, "ropeT", [128, NT, 2, 64])
        for kc in range(8):
            S.dma(pool, lambda h: h.dma_start(
                out=winb[:, kc, :].rearrange("p (a n) -> p a n", n=640),
                in_=win_d[kc * 128:(kc + 1) * 128, :].rearrange("p (a n) -> p a n", n=640)), wr=[winb])
        S.dma(sp, lambda h: h.dma_start(out=ropeT[:].rearrange("p t c d -> p (t c d)"), in_=rope_d), wr=[ropeT])

        with ExitStack() as p0:
            cv = sb(p0, "cv", [128, 16])
            scv = sb(p0, "scv", [128, 16])
            modrow = sb(p0, "modrow", [1, 8192])
            bada = sb(p0, "bada", [1, 6 * D])
            normg = sb(p0, "normg", [1, 2 * D])
            rowt = sb(p0, "rowt", [1, 3, D])
            qkg = sb(p0, "qkg", [1, 1024])
            lamv = sb(p0, "lamv", [1, 256])
            subg = sb(p0, "subg", [1, 512])
            lt = sb(p0, "lt", [1, 16])
            wb = [sb(p0, f"wadab{i}", [128, 8, 256]) for i in range(2)]
            S.dma(sp, lambda h: h.dma_start(out=cv[:], in_=cvec_d), wr=[cv])
            S.dma(sp, lambda h: h.dma_start(out=bada[:], in_=bada_d), wr=[bada])
            S.dma(sp, lambda h: h.dma_start(out=normg[:], in_=normg_d), wr=[normg])
            S.dma(sp, lambda h: h.dma_start(out=qkg[:], in_=qkg_d), wr=[qkg])
            S.dma(sp, lambda h: h.dma_start(out=lamv[:], in_=lamv_d), wr=[lamv])
            S.dma(sp, lambda h: h.dma_start(out=subg[:], in_=subg_d), wr=[subg])
            S.op(act, lambda h: h.activation(out=scv[:], in_=cv[:], func=AF.Silu), rd=[cv], wr=[scv])
            wada_v = wada_d.rearrange("(kc p) n -> p kc n", p=128)
            CW = 256
            for nb in range(6 * D // CW):
                w = wb[nb % 2]
                S.dma(sp, lambda h: h.dma_start(out=w[:], in_=wada_v[:, :, nb * CW:(nb + 1) * CW]), wr=[w])
                for kc in range(8):
                    S.op(pe, lambda h: h.matmul(bank(0)[0:1, 0:CW], lhsT=scv[:, kc:kc + 1], rhs=w[:, kc, :],
                                                start=(kc == 0), stop=(kc == 7)),
                         rd=[scv, w], wr=[bankR[0]], self_sync=False)
                S.op(dve, lambda h: h.tensor_tensor(out=modrow[0:1, nb * CW:(nb + 1) * CW], in0=bank(0)[0:1, 0:CW],
                                                    in1=bada[0:1, nb * CW:(nb + 1) * CW], op=ALU.add),
                     rd=[bankR[0], bada], wr=[modrow])
                if nb < 2 * D // CW:
                    for kc in range(8):
                        S.op(pe, lambda h: h.matmul(bank(1)[0:1, 0:CW], lhsT=scv[:, 8 + kc:9 + kc], rhs=w[:, kc, :],
                                                    start=(kc == 0), stop=(kc == 7)),
                             rd=[scv, w], wr=[bankR[1]], self_sync=False)
                    S.op(dve, lambda h: h.tensor_tensor(out=modrow[0:1, 6144 + nb * CW:6144 + (nb + 1) * CW],
                                                        in0=bank(1)[0:1, 0:CW], in1=bada[0:1, nb * CW:(nb + 1) * CW],
                                                        op=ALU.add),
                         rd=[bankR[1], bada], wr=[modrow])
            if dbg:
                S.dma(sp, lambda h: h.dma_start(out=dbg_mod, in_=modrow[:]), rd=[modrow])

            def mrow(i):
                return modrow[0:1, i * D:(i + 1) * D]

            for ti, (si, go) in enumerate([(1, 0), (7, 0), (4, D)]):
                S.op(dve, lambda h: h.scalar_tensor_tensor(out=rowt[0:1, ti, :], in0=mrow(si), scalar=1.0,
                                                           in1=normg[0:1, go:go + D], op0=ALU.add, op1=ALU.mult),
                     rd=[modrow, normg], wr=[rowt])
            rows = {A1: rowt[0:1, 0, :], B1: mrow(0), A1C: rowt[0:1, 1, :], B1C: mrow(6), G1: mrow(2),
                    A2: rowt[0:1, 2, :], B2: mrow(3), G2: mrow(5)}
            cnt = 0
            for bi, row in rows.items():
                for hf in range(2):
                    b = 2 + cnt % 2
                    cnt += 1
                    S.op(pe, lambda h: h.matmul(bank(b), lhsT=ones_f[0:1, :], rhs=row[0:1, hf * 512:(hf + 1) * 512],
                                                start=True, stop=True),
                         rd=[cst, modrow, rowt], wr=[bankR[b]], self_sync=False)
                    bt_, bi_ = bcr(bi)
                    S.op(act, lambda h: h.copy(out=bt_[:, bi_, hf * 512:(hf + 1) * 512], in_=bank(b)),
                         rd=[bankR[b]], wr=[bt_])
            for hf in range(2):
                b = 2 + hf
                S.op(pe, lambda h: h.matmul(bank(b), lhsT=ones_f[0:1, :], rhs=qkg[0:1, hf * 512:(hf + 1) * 512],
                                            start=True, stop=True), rd=[cst, qkg], wr=[bankR[b]], self_sync=False)
                S.op(act, lambda h: h.mul(out=bcq[:, hf * 512:(hf + 1) * 512], in_=bank(b),
                                          mul=(0.125 if hf == 0 else 1.0)), rd=[bankR[b]], wr=[bcq])
            S.op(pe, lambda h: h.matmul(bank(2), lhsT=ones_f[0:1, :], rhs=subg[0:1, :], start=True, stop=True),
                 rd=[cst, subg], wr=[bankR[2]], self_sync=False)
            S.op(act, lambda h: h.mul(out=bcs[:], in_=bank(2), mul=1.0 - LAM_INIT), rd=[bankR[2]], wr=[bcs])
            S.op(dve, lambda h: h.tensor_tensor(out=lamv[0:1, 0:64], in0=lamv[0:1, 0:64], in1=lamv[0:1, 64:128],
                                                op=ALU.mult), rd=[lamv], wr=[lamv])
            S.op(dve, lambda h: h.tensor_tensor(out=lamv[0:1, 128:192], in0=lamv[0:1, 128:192],
                                                in1=lamv[0:1, 192:256], op=ALU.mult), rd=[lamv], wr=[lamv])
            S.op(dve, lambda h: h.reduce_sum(out=lt[0:1, 0:1], in_=lamv[0:1, 0:64], axis=AX.X), rd=[lamv], wr=[lt])
            S.op(dve, lambda h: h.reduce_sum(out=lt[0:1, 1:2], in_=lamv[0:1, 128:192], axis=AX.X), rd=[lamv], wr=[lt])
            S.op(act, lambda h: h.activation(out=lt[0:1, 2:4], in_=lt[0:1, 0:2], func=AF.Exp), rd=[lt], wr=[lt])
            S.op(dve, lambda h: h.tensor_tensor(out=lt[0:1, 4:5], in0=lt[0:1, 3:4], in1=lt[0:1, 2:3],
                                                op=ALU.subtract), rd=[lt], wr=[lt])
            S.op(dve, lambda h: h.tensor_scalar_add(out=lt[0:1, 4:5], in0=lt[0:1, 4:5], scalar1=-LAM_INIT),
                 rd=[lt], wr=[lt])
            S.op(dve, lambda h: h.reduce_max(out=lt[0:1, 6:7], in_=qkg[0:1, 0:64], axis=AX.X,
                                             apply_absolute_value=True), rd=[qkg], wr=[lt])
            S.op(dve, lambda h: h.reduce_max(out=lt[0:1, 7:8], in_=qkg[0:1, 512:576], axis=AX.X,
                                             apply_absolute_value=True), rd=[qkg], wr=[lt])
            S.op(dve, lambda h: h.tensor_tensor(out=lt[0:1, 5:6], in0=lt[0:1, 6:7], in1=lt[0:1, 7:8], op=ALU.mult),
                 rd=[lt], wr=[lt])
            S.op(dve, lambda h: h.tensor_scalar_mul(out=lt[0:1, 5:6], in0=lt[0:1, 5:6], scalar1=-8.0),
                 rd=[lt], wr=[lt])
            S.op(pe, lambda h: h.matmul(bank(3)[:, 0:2], lhsT=ones_f[0:1, :], rhs=lt[0:1, 4:6], start=True, stop=True),
                 rd=[cst, lt], wr=[bankR[3]], self_sync=False)
            S.op(act, lambda h: h.copy(out=sc2[:], in_=bank(3)[:, 0:2]), rd=[bankR[3]], wr=[sc2])
        S.barrier()
        if stop_after < 1:
            S.finish()
            return nc

        with ExitStack() as p1:
            hlT = [sb(p1, f"hlT{i}", [128, 8, 512], BF16) for i in range(2)]
            xb = [sb(p1, f"xb{i}", [128, D]) for i in range(4)]
            junk = sb(p1, "junk", [128, D], BF16)
            ss4 = [sb(p1, f"ss4{i}", [128, 4]) for i in range(2)]
            rs4 = [sb(p1, f"rs4{i}", [128, 4]) for i in range(2)]
            t1 = sb(p1, "t1_0", [128, D])
            hl = [sb(p1, f"hl{i}", [128, D], BF16) for i in range(2)]
            xlst = [sb(p1, f"xlst{i}", [128, 512]) for i in range(2)]
            gst = sb(p1, "gst0", [128, 4, 512], BF16)
            vst = [sb(p1, f"vst{i}", [128, 4, 4, 130], BF16) for i in range(2)]
            sq = [sb(p1, f"sq{i}", [128, D]) for i in range(2)]
            ss16 = [sb(p1, f"ss16{i}", [128, 16]) for i in range(2)]
            rs16 = [sb(p1, f"rs16{i}", [128, 16]) for i in range(2)]
            tq = [sb(p1, f"tq{i}", [128, D]) for i in range(2)]
            r1 = [sb(p1, f"r1{i}", [128, D]) for i in range(2)]
            qkr = [sb(p1, f"qkr{i}", [128, D], BF16) for i in range(2)]
            qkst = [sb(p1, f"qkst{i}", [128, 8, 512], BF16) for i in range(2)]
            for v in vst:
                S.op(pool, lambda h: h.memset(v[:], 1.0), wr=[v])

            def blk_tiles(blk):
                return [0, 1] if blk == 0 else [2 + 4 * (blk - 1) + i for i in range(4)]

            cnts = {"x": 0, "f": 0, "t": 0}

            hl_state = {}

            def hl_chain(blk, i):
                xts, r4 = hl_state[blk]
                ai, bi = (A1C, B1C) if blk == 0 else (A1, B1)
                xt, hh = xts[i], hl[i % 2]
                S.op(dve, lambda h: h.scalar_tensor_tensor(out=t1[:], in0=xt[:], scalar=r4[:, i:i + 1],
                                                           in1=bcA[:, ai, :], op0=ALU.mult, op1=ALU.mult),
                     rd=[xt, r4, bcA], wr=[t1])
                S.op(dve, lambda h: h.tensor_tensor(out=hh[:], in0=t1[:], in1=bcA[:, bi, :], op=ALU.add),
                     rd=[t1, bcA], wr=[hh])

            def hl_T(blk, i):
                hT, hh = hlT[blk % 2], hl[i % 2]
                for kc in range(8):
                    S.op(pe, lambda h: h.transpose(out=bank_bf(0)[:, kc * 128:(kc + 1) * 128],
                                                   in_=hh[:, kc * 128:(kc + 1) * 128], identity=ident_b),
                         rd=[hh, cstb], wr=[bankR[0]], self_sync=False)
                S.op(act, lambda h: h.copy(out=hT[:, :, i * 128:(i + 1) * 128],
                                           in_=bank_bf(0).rearrange("p (k t) -> p k t", t=128)),
                     rd=[bankR[0]], wr=[hT])

            def hl_front(blk):
                tiles = blk_tiles(blk)
                nt = len(tiles)
                s4, r4 = ss4[blk % 2], rs4[blk % 2]
                xts = []
                for i, T in enumerate(tiles):
                    xt = xb[cnts["x"] % 4]
                    cnts["x"] += 1
                    src = ctx_d[T * 128:(T + 1) * 128, :] if T < 2 else x_d[(T - 2) * 128:(T - 1) * 128, :]
                    S.dma(sp, lambda h: h.dma_start(out=xt[:], in_=src), wr=[xt])
                    S.op(act, lambda h: h.activation(out=junk[:], in_=xt[:], func=AF.Square,
                                                     accum_out=s4[:, i:i + 1]), rd=[xt], wr=[junk, s4])
                    xts.append(xt)
                rsqrt_ops(r4, s4, 1.0 / D, nt)
                hl_state[blk] = (xts, r4)
                for i in range(min(2, nt)):
                    hl_chain(blk, i)

            def hl_back(blk):
                nt = len(blk_tiles(blk))
                for i in range(nt):
                    if i >= 2:
                        hl_chain(blk, i)
                    hl_T(blk, i)

            def hlstage(blk):
                hl_front(blk)
                hl_back(blk)

            def fmstage(blk):
                tiles = blk_tiles(blk)
                W = 128 * len(tiles)
                toff = 0 if blk == 0 else CTX + (blk - 1) * 512
                loff = (blk - 1) * 512
                hT = hlT[blk % 2]
                for oc in range(4 if blk == 0 else 8):
                    b = 2 + cnts["f"] % 2
                    cnts["f"] += 1
                    for kc in range(8):
                        S.op(pe, lambda h: h.matmul(bank(b)[:, 0:W], lhsT=winb[:, kc, oc * 128:(oc + 1) * 128],
                                                    rhs=hT[:, kc, 0:W], start=(kc == 0), stop=(kc == 7)),
                             rd=[winb, hT], wr=[bankR[b]], self_sync=False)
                    if oc < 4:
                        st = xlst[oc % 2]
                        S.op(dve, lambda h: h.tensor_copy(out=st[:, 0:W], in_=bank(b)[:, 0:W]),
                             rd=[bankR[b]], wr=[st])
                        S.dma(sp, lambda h: h.dma_start(out=xl_d[oc, :, toff:toff + W], in_=st[:, 0:W]),
                              rd=[st], wr=[R_xl[oc]])
                    else:
                        S.op(act, lambda h: h.activation(out=gst[:, oc - 4, :], in_=bank(b), func=AF.Gelu_apprx_tanh),
                             rd=[bankR[b]], wr=[gst])
                if blk > 0:
                    S.dma(sp, lambda h: h.dma_start(out=gel_d.rearrange("c p t -> p c t")[:, :, loff:loff + 512],
                                                    in_=gst[:]), rd=[gst], wr=[R_gel])

            def stage_a(blk, i):
                T = blk_tiles(blk)[i]
                hT = hlT[blk % 2]
                vs = vst[blk % 2]
                g0 = 8 if blk == 0 else 0
                c0 = g0 * 64
                u = cnts["t"] % 2
                qb_ = 4 + 2 * u
                for (b, col) in ([(qb_, 1024)] if blk > 0 else []) + [(qb_ + 1, 1536), (1, 2048)]:
                    for kc in range(8):
                        S.op(pe, lambda h: h.matmul(bank(b), lhsT=hT[:, kc, i * 128:(i + 1) * 128],
                                                    rhs=winb[:, kc, col:col + 512], start=(kc == 0), stop=(kc == 7)),
                             rd=[winb, hT], wr=[bankR[b]], self_sync=False)
                S.op(act, lambda h: h.copy(out=vs[:, i, :, 0:128],
                                           in_=bank(1).rearrange("p (a e) -> p a e", e=128)),
                     rd=[bankR[1]], wr=[vs])
                pqk = PS[:, qb_:qb_ + 2, :].rearrange("p a n -> p (a n)")
                S.op(act, lambda h: h.activation(out=sq[u][:, c0:], in_=pqk[:, c0:], func=AF.Square),
                     rd=[bankR[qb_], bankR[qb_ + 1]], wr=[sq[u]])
                cnts["t"] += 1
                return u

            def stage_a2(blk, i, u):
                g0 = 8 if blk == 0 else 0
                c0 = g0 * 64
                qb_ = 4 + 2 * u
                pqk = PS[:, qb_:qb_ + 2, :].rearrange("p a n -> p (a n)")
                S.op(dve, lambda h: h.tensor_reduce(out=ss16[u][:, g0:], in_=sq[u][:, c0:].rearrange("p (g d) -> p g d", d=64),
                                                    axis=AX.X, op=ALU.add), rd=[sq[u]], wr=[ss16[u]])
                rsqrt_ops(rs16[u], ss16[u], 1.0 / 64, 16, g0)
                S.op(dve, lambda h: h.tensor_tensor(
                    out=tq[u][:, c0:].rearrange("p (g d) -> p g d", d=64),
                    in0=pqk[:, c0:].rearrange("p (g d) -> p g d", d=64),
                    in1=rs16[u][:, g0:].unsqueeze(2).broadcast_to([128, 16 - g0, 64]), op=ALU.mult),
                    rd=[bankR[qb_], bankR[qb_ + 1], rs16[u]], wr=[tq[u]])
                S.op(dve, lambda h: h.tensor_tensor(out=tq[u][:, c0:], in0=tq[u][:, c0:], in1=bcq[:, c0:], op=ALU.mult),
                     rd=[tq[u], bcq], wr=[tq[u]])

            def stage_b(blk, i, u):
                T = blk_tiles(blk)[i]
                qs = qkst[blk % 2]
                g0 = 8 if blk == 0 else 0
                c0 = g0 * 64
                ng = 16 - g0
                r2 = sq[u]
                S.op(pool, lambda h: h.tensor_tensor(
                    out=r1[u][:, c0:].rearrange("p (g d) -> p g d", d=64),
                    in0=tq[u][:, c0:].rearrange("p (g d) -> p g d", d=64),
                    in1=ropeT[:, T, 0, :].unsqueeze(1).broadcast_to([128, ng, 64]), op=ALU.mult),
                    rd=[tq[u], ropeT], wr=[r1[u]])
                tq5 = tq[u][:, c0:].rearrange("p (g t h w) -> p g t h w", t=2, h=2, w=16)
                r25 = r2[:, c0:].rearrange("p (g t h w) -> p g t h w", t=2, h=2, w=16)
                sn4 = ropeT[:, T, 1, :].rearrange("p (t h w) -> p t h w", t=2, h=2)
                for hv in range(2):
                    S.op(pool, lambda h: h.tensor_tensor(
                        out=r25[:, :, :, hv, :], in0=tq5[:, :, :, 1 - hv, :],
                        in1=sn4[:, :, hv, :].unsqueeze(1).broadcast_to([128, ng, 2, 16]), op=ALU.mult),
                        rd=[tq[u], ropeT], wr=[r2])

            def stage_b2(blk, i, u):
                qs = qkst[blk % 2]
                g0 = 8 if blk == 0 else 0
                c0 = g0 * 64
                r2 = sq[u]
                qq = qkr[u]
                S.op(pool, lambda h: h.tensor_tensor(out=qq[:, c0:], in0=r1[u][:, c0:], in1=r2[:, c0:], op=ALU.add),
                     rd=[r1[u], r2], wr=[qq])
                k0 = g0 // 2
                for kc in range(k0, 8):
                    S.op(pe, lambda h: h.transpose(out=bank_bf(0)[:, kc * 128:(kc + 1) * 128],
                                                   in_=qq[:, kc * 128:(kc + 1) * 128], identity=ident_b),
                         rd=[qq, cstb], wr=[bankR[0]], self_sync=False)
                S.op(act, lambda h: h.copy(out=qs[:, k0:8, i * 128:(i + 1) * 128],
                                           in_=bank_bf(0).rearrange("p (k t) -> p k t", t=128)[:, k0:8, :]),
                     rd=[bankR[0]], wr=[qs])

            def outstage(blk):
                tiles = blk_tiles(blk)
                nt = len(tiles)
                W = 128 * nt
                toff = 0 if blk == 0 else CTX + (blk - 1) * 512
                loff = (blk - 1) * 512
                qs = qkst[blk % 2]
                vs = vst[blk % 2]
                if blk > 0:
                    S.dma(sp, lambda h: h.dma_start(out=qT_d.rearrange("c p t -> p c t")[:, :, loff:loff + 512],
                                                    in_=qs[:, 0:4, :]), rd=[qs], wr=[R_qT])
                S.dma(sp, lambda h: h.dma_start(out=kT_d.rearrange("c p t -> p c t")[:, :, toff:toff + W],
                                                in_=qs[:, 4:8, 0:W]), rd=[qs], wr=[R_kT])
                S.dma(sp, lambda h: h.dma_start(
                    out=v_d[tiles[0]:tiles[0] + nt, :, :].rearrange("t p f -> p t f"),
                    in_=vs[:, 0:nt, :, :].rearrange("p t a e -> p t (a e)")), rd=[vs], wr=[R_v])

            flat = [(blk, i) for blk in range(9) for i in range(len(blk_tiles(blk)))]
            hlstage(0)
            fmstage(0)
            hlstage(1)
            prev = None
            for (blk, i) in flat:
                u = stage_a(blk, i)
                last = (i == len(blk_tiles(blk)) - 1)
                if i == len(blk_tiles(blk)) - 2 and blk + 2 < 9:
                    hl_front(blk + 2)
                if last and blk + 1 < 9:
                    fmstage(blk + 1)
                    if blk + 2 < 9:
                        hl_back(blk + 2)
                if prev is not None:
                    stage_b(*prev)
                stage_a2(blk, i, u)
                if prev is not None:
                    stage_b2(*prev)
                    if prev[1] == len(blk_tiles(prev[0])) - 1:
                        outstage(prev[0])
                prev = (blk, i, u)
            stage_b(*prev)
            stage_b2(*prev)
            outstage(prev[0])
        p01.close()
        S.barrier()
        if stop_after < 2:
            S.finish()
            return nc


        with ExitStack() as p2:
            TT = CTX + SEQ
            HL = SEQ // 2
            convw = sb(p2, "convw", [128, 16])
            convb = sb(p2, "convb", [128, 4])
            lrub = sb(p2, "lrub", [128, 16])
            lrul = sb(p2, "lrul", [128, 8])
            cL = sb(p2, "cL", [128, 8])
            cL2 = sb(p2, "cL2", [128, 8])
            onesT = sb(p2, "onesT", [128, 1])
            lruwb = sb(p2, "lruwb", [128, 16, 128], BF16)
            XP = sb(p2, "XP", [128, TT + 8])
            xcs = [sb(p2, f"xc{i}", [128, TT]) for i in range(2)]
            xcbs = [sb(p2, f"xcb{i}", [128, TT], BF16) for i in range(2)]
            Rs = [sb(p2, f"Rr{i}", [128, HL]) for i in range(2)]
            As = [sb(p2, f"A2_{i}", [128, HL]) for i in range(2)]
            Is = [sb(p2, f"Ii{i}", [128, HL]) for i in range(2)]
            Hf = sb(p2, "Hf", [128, TT])
            Hb = sb(p2, "Hb", [128, TT])
            gl = sb(p2, "gl", [128, SEQ], BF16)
            lst = sb(p2, "lst", [128, SEQ], BF16)
            S.dma(sp, lambda h: h.dma_start(out=convw[:], in_=convw_d), wr=[convw])
            S.dma(sp, lambda h: h.dma_start(out=convb[:], in_=convb_d), wr=[convb])
            S.dma(sp, lambda h: h.dma_start(out=lrub[:], in_=lrub_d), wr=[lrub])
            S.dma(sp, lambda h: h.dma_start(out=lrul[:], in_=lrul_d), wr=[lrul])
            S.dma(pool, lambda h: h.dma_start(out=lruwb[:], in_=lruw_d.rearrange("p (a n) -> p a n", n=128)),
                  wr=[lruwb])
            S.op(act, lambda h: h.activation(out=cL[:], in_=lrul[:], func=AF.Exp, scale=-1.0), rd=[lrul], wr=[cL])
            S.op(act, lambda h: h.activation(out=cL[:], in_=cL[:], func=AF.Ln, bias=1.0), rd=[cL], wr=[cL])
            S.op(dve, lambda h: h.tensor_scalar_mul(out=cL2[:], in0=cL[:], scalar1=-16.0), rd=[cL], wr=[cL2])
            S.op(dve, lambda h: h.tensor_scalar_mul(out=cL[:], in0=cL[:], scalar1=-8.0), rd=[cL, cL2], wr=[cL])
            S.op(dve, lambda h: h.memset(onesT[:], 1.0), wr=[onesT])
            S.op(dve, lambda h: h.memset(XP[:], 0.0), wr=[XP])
            segs = [(1, 0, CTX), (260, CTX, SEQ)]

            def conv(j):
                xc, xcb = xcs[j % 2], xcbs[j % 2]
                S.dma(sp, lambda h: h.dma_start(out=XP[:, 1:1 + CTX], in_=xl_d[j, :, 0:CTX]), rd=[R_xl[j]], wr=[XP])
                S.dma(sp, lambda h: h.dma_start(out=XP[:, 260:260 + SEQ], in_=xl_d[j, :, CTX:TT]),
                      rd=[R_xl[j]], wr=[XP])
                for (xo, to, ln) in segs:
                    S.op(dve, lambda h: h.tensor_scalar(out=xc[:, to:to + ln], in0=XP[:, xo - 1:xo - 1 + ln],
                                                         scalar1=convw[:, j * 4:j * 4 + 1], scalar2=convb[:, j:j + 1],
                                                         op0=ALU.mult, op1=ALU.add),
                         rd=[XP, convw, convb], wr=[xc])
                    for k in range(1, 4):
                        S.op(dve, lambda h: h.scalar_tensor_tensor(
                            out=xc[:, to:to + ln], in0=XP[:, xo - 1 + k:xo - 1 + k + ln],
                            scalar=convw[:, j * 4 + k:j * 4 + k + 1], in1=xc[:, to:to + ln],
                            op0=ALU.mult, op1=ALU.add), rd=[XP, convw, xc], wr=[xc])
                S.op(dve, lambda h: h.tensor_copy(out=xcb[:], in_=xc[:]), rd=[xc], wr=[xcb])

            pc = {"n": 0, "b": 0}

            def piece(j, d, t0, ln, first_col, init, rev):
                xc, xcb = xcs[j % 2], xcbs[j % 2]
                s_ = pc["n"] % 2
                pc["n"] += 1
                Rr, A2_, Ii = Rs[s_], As[s_], Is[s_]
                H = Hf if d == 0 else Hb
                for o in range(0, ln, 512):
                    w_ = min(512, ln - o)
                    for gi, dst in enumerate([Rr, Ii]):
                        b = 2 + pc["b"] % 4
                        pc["b"] += 1
                        S.op(pe, lambda h: h.matmul(bank(b)[:, 0:w_], lhsT=lruwb[:, (d * 2 + gi) * 4 + j, :],
                                                    rhs=xcb[:, t0 + o:t0 + o + w_], start=True, stop=True),
                             rd=[lruwb, xcb], wr=[bankR[b]], self_sync=False)
                        bi_ = (d * 2 + gi) * 4 + j
                        S.op(act, lambda h: h.activation(out=dst[:, o:o + w_], in_=bank(b)[:, 0:w_],
                                                         func=AF.Sigmoid, bias=lrub[:, bi_:bi_ + 1]),
                             rd=[bankR[b], lrub], wr=[dst])
                ci = d * 4 + j
                S.op(act, lambda h: h.activation(out=Rr[:, 0:ln], in_=Rr[:, 0:ln], func=AF.Exp, scale=cL[:, ci:ci + 1]),
                     rd=[Rr, cL], wr=[Rr])
                S.op(dve, lambda h: h.scalar_tensor_tensor(out=A2_[:, 0:ln], in0=Rr[:, 0:ln], scalar=1.0, in1=Rr[:, 0:ln],
                                                           op0=ALU.min, op1=ALU.mult), rd=[Rr], wr=[A2_])
                S.op(act, lambda h: h.activation(out=A2_[:, 0:ln], in_=A2_[:, 0:ln], func=AF.Sqrt, scale=-1.0,
                                                 bias=onesT[:, 0:1]), rd=[A2_, onesT], wr=[A2_])
                if first_col is not None:
                    S.op(dve, lambda h: h.memset(A2_[:, first_col:first_col + 1], 1.0), wr=[A2_])
                S.op(dve, lambda h: h.tensor_tensor(out=Ii[:, 0:ln], in0=Ii[:, 0:ln], in1=A2_[:, 0:ln], op=ALU.mult),
                     rd=[Ii, A2_], wr=[Ii])
                S.op(dve, lambda h: h.tensor_tensor(out=Ii[:, 0:ln], in0=Ii[:, 0:ln], in1=xc[:, t0:t0 + ln], op=ALU.mult),
                     rd=[Ii, xc], wr=[Ii])
                hv, av, uv = H[:, t0:t0 + ln], Rr[:, 0:ln], Ii[:, 0:ln]
                if rev:
                    hv, av, uv = hv[:, ::-1], av[:, ::-1], uv[:, ::-1]
                S.op(dve, lambda h: h.tensor_tensor_scan(out=hv, data0=av, data1=uv, initial=init,
                                                         op0=ALU.mult, op1=ALU.add), rd=[Rr, Ii, H], wr=[H])

            conv(0)
            for j in range(4):
                S.dma(sp, lambda h: h.dma_start(out=gl[:], in_=gel_d[j, :, :]), rd=[R_gel], wr=[gl])
                if j + 1 < 4:
                    conv(j + 1)
                piece(j, 0, 0, CTX, 0, 0.0, False)
                piece(j, 0, CTX, HL, None, Hf[:, CTX - 1:CTX], False)
                piece(j, 0, CTX + HL, HL, None, Hf[:, CTX + HL - 1:CTX + HL], False)
                piece(j, 1, 0, CTX, CTX - 1, 0.0, True)
                piece(j, 1, CTX + HL, HL, None, Hb[:, 0:1], True)
                piece(j, 1, CTX, HL, None, Hb[:, CTX + HL:CTX + HL + 1], True)
                S.op(dve, lambda h: h.tensor_tensor(out=Hf[:, CTX:TT], in0=Hf[:, CTX:TT], in1=Hb[:, CTX:TT], op=ALU.add),
                     rd=[Hf, Hb], wr=[Hf])
                S.op(dve, lambda h: h.tensor_tensor(out=lst[:], in0=Hf[:, CTX:TT], in1=gl[:], op=ALU.mult),
                     rd=[Hf, gl], wr=[lst])
                S.dma(sp, lambda h: h.dma_start(out=lru_d[j, :, :], in_=lst[:]), rd=[lst], wr=[R_lru])
        S.barrier()
        if stop_after < 3:
            S.finish()
            return nc


        with ExitStack() as p3:
            kT = sb(p3, "kT", [128, 4, CTX + SEQ], BF16)
            v1 = sb(p3, "v1", [128, NT, 520], BF16)
            woutb = sb(p3, "woutb", [128, 8, D], BF16)
            wrt = sb(p3, "wrt", [128, 8, NE])
            qz = [[sb(p3, f"qz{i}_{c}", [128, 4, 512], BF16) for c in range(2)] for i in range(2)]
            for i in range(2):
                for c in range(2):
                    S.op(pool, lambda h: h.memset(qz[i][c][:], 0.0), wr=[qz[i][c]])
            lruB = [sb(p3, f"lruB{i}", [128, 4, 512], BF16) for i in range(2)]
            Eb = [sb(p3, f"Eb{i}", [128, 1024], BF16) for i in range(3)]
            osb = [sb(p3, f"osb{i}", [128, 4, 128]) for i in range(2)]
            rl = sb(p3, "rl", [128, 8])
            ssn = sb(p3, "ssn", [128, 8])
            rsn = sb(p3, "rsn", [128, 8])
            junk3 = sb(p3, "junk3", [128, D], BF16)
            att = sb(p3, "att", [128, 4, 512], BF16)
            attT = sb(p3, "attT", [128, 4, 512], BF16)
            xres = [sb(p3, f"xres{i}", [128, D]) for i in range(2)]
            x1t = [sb(p3, f"x1t{i}", [128, D]) for i in range(2)]
            tmp3s = [sb(p3, f"tmp3{i}", [128, D]) for i in range(2)]
            h2fs = [sb(p3, f"h2f{i}", [128, D]) for i in range(2)]
            h2b = [sb(p3, f"h2b{i}", [128, D], BF16) for i in range(2)]
            h2Ts = [sb(p3, f"h2T{i}", [128, 8, 128]) for i in range(2)]
            lg = sb(p3, "lg", [128, NE])
            mx = sb(p3, "mx", [128, 4])
            S.dma(sp, lambda h: h.dma_start(out=kT[:], in_=kT_d.rearrange("c p t -> p c t")), rd=[R_kT], wr=[kT])
            S.dma(sp, lambda h: h.dma_start(out=v1[:], in_=v_d.rearrange("t p f -> p t f")), rd=[R_v], wr=[v1])
            S.dma(sp, lambda h: h.dma_start(out=wrt[:], in_=wr_d.rearrange("(kc p) n -> p kc n", p=128)), wr=[wrt])
            for kc in range(8):
                S.dma(pool, lambda h: h.dma_start(out=woutb[:, kc, :], in_=wout_d[kc * 128:(kc + 1) * 128, :]),
                      wr=[woutb])
            ssn2 = sb(p3, "ssn2", [128, 2])
            rsn2 = sb(p3, "rsn2", [128, 2])
            junk4 = sb(p3, "junk4", [128, D], BF16)

            def tail_steps(qb, lb, B0, B1):
                steps = []
                for s_ in range(4):
                    def st_t(s_=s_):
                        for hd in range(4):
                            S.op(pe, lambda h: h.transpose(out=bank_bf(B0)[:, hd * 128:(hd + 1) * 128],
                                                           in_=att[:, s_, hd * 128:(hd + 1) * 128], identity=ident_b),
                                 rd=[att, cstb], wr=[bankR[B0]], self_sync=False)
                        S.op(act, lambda h: h.copy(out=attT[:, :, s_ * 128:(s_ + 1) * 128],
                                                   in_=bank_bf(B0)[:, 0:512].rearrange("p (k t) -> p k t", t=128)),
                             rd=[bankR[B0]], wr=[attT])
                    steps.append(st_t)
                per_tile = []
                for s_ in range(4):
                    tok0 = qb * 512 + s_ * 128
                    tl = qb * 4 + s_
                    xr, x1 = xres[s_ % 2], x1t[s_ % 2]
                    hb_ = h2b[s_ % 2]
                    tmp3, h2f, h2T = tmp3s[s_ % 2], h2fs[s_ % 2], h2Ts[s_ % 2]

                    def st_w(fh, s_=s_, tok0=tok0, xr=xr, tmp3=tmp3):
                        bk = B0 if fh == 0 else B1
                        if fh == 0:
                            S.dma(sp, lambda h: h.dma_start(out=xr[:], in_=x_d[tok0:tok0 + 128, :]), wr=[xr])
                        for kc in range(8):
                            lhs = (lb[:, kc, s_ * 128:(s_ + 1) * 128] if kc < 4 else attT[:, kc - 4, s_ * 128:(s_ + 1) * 128])
                            S.op(pe, lambda h: h.matmul(bank(bk), lhsT=lhs, rhs=woutb[:, kc, fh * 512:(fh + 1) * 512],
                                                        start=(kc == 0), stop=(kc == 7)),
                                 rd=[lb, attT, woutb], wr=[bankR[bk]], self_sync=False)
                        S.op(dve, lambda h: h.tensor_tensor(out=tmp3[:, fh * 512:(fh + 1) * 512], in0=bank(bk),
                                                            in1=bcG[:, 0, fh * 512:(fh + 1) * 512], op=ALU.mult),
                             rd=[bankR[bk], bcG], wr=[tmp3])
                    tile_steps = {}
                    tile_steps["w0"] = (lambda st_w=st_w: st_w(0))

                    cc = s_ % 2

                    def st_c1(st_w=st_w, tok0=tok0, xr=xr, x1=x1, tmp3=tmp3, cc=cc):
                        st_w(1)
                        S.op(dve, lambda h: h.tensor_tensor(out=x1[:], in0=tmp3[:], in1=xr[:], op=ALU.add),
                             rd=[tmp3, xr], wr=[x1])
                        S.dma(pool, lambda h: h.dma_start(out=out_d[tok0:tok0 + 128, :], in_=x1[:]), rd=[x1], wr=[R_out])
                        S.op(act, lambda h: h.activation(out=junk4[:], in_=x1[:], func=AF.Square, accum_out=ssn2[:, cc:cc + 1]),
                             rd=[x1], wr=[junk4, ssn2])
                        S.op(act, lambda h: h.activation(out=rsn2[:, cc:cc + 1], in_=ssn2[:, cc:cc + 1], func=AF.Ln, scale=1.0 / D,
                                                         bias=epsT[:, 0:1]), rd=[ssn2, epsT], wr=[rsn2])
                        S.op(act, lambda h: h.activation(out=rsn2[:, cc:cc + 1], in_=rsn2[:, cc:cc + 1], func=AF.Exp, scale=-0.5),
                             rd=[rsn2], wr=[rsn2])
                    tile_steps["c1"] = st_c1

                    def st_c2(tok0=tok0, x1=x1, hb_=hb_, tmp3=tmp3, h2f=h2f, cc=cc):
                        S.op(dve, lambda h: h.scalar_tensor_tensor(out=tmp3[:], in0=x1[:], scalar=rsn2[:, cc:cc + 1], in1=bcG[:, 1, :],
                                                                   op0=ALU.mult, op1=ALU.mult), rd=[x1, rsn2, bcG], wr=[tmp3])
                        S.op(dve, lambda h: h.tensor_tensor(out=h2f[:], in0=tmp3[:], in1=bcG[:, 2, :], op=ALU.add),
                             rd=[tmp3, bcG], wr=[h2f])
                        S.op(act, lambda h: h.copy(out=hb_[:], in_=h2f[:]), rd=[h2f], wr=[hb_])
                        S.dma(pool, lambda h: h.dma_start(out=h2_d[tok0:tok0 + 128, :], in_=hb_[:]), rd=[hb_], wr=[R_h2])
                    tile_steps["c2"] = st_c2

                    def st_r(half, h2f=h2f, h2T=h2T):
                        for k4 in range(4):
                            kc = half * 4 + k4
                            S.op(pe, lambda h: h.transpose(out=bank(B0)[:, k4 * 128:(k4 + 1) * 128],
                                                           in_=h2f[:, kc * 128:(kc + 1) * 128], identity=ident_f),
                                 rd=[h2f, cst], wr=[bankR[B0]], self_sync=False)
                        S.op(dve, lambda h: h.tensor_copy(out=h2T[:, half * 4:half * 4 + 4, :],
                                                          in_=bank(B0).rearrange("p (k t) -> p k t", t=128)),
                             rd=[bankR[B0]], wr=[h2T])
                    tile_steps["r0"] = (lambda st_r=st_r: st_r(0))
                    tile_steps["r1"] = (lambda st_r=st_r: st_r(1))

                    def st_s(tl=tl, h2T=h2T):
                        for kc in range(8):
                            S.op(pe, lambda h: h.matmul(bank(B0)[:, 0:NE], lhsT=h2T[:, kc, :], rhs=wrt[:, kc, :],
                                                        start=(kc == 0), stop=(kc == 7)),
                                 rd=[h2T, wrt], wr=[bankR[B0]], self_sync=False)
                        S.op(dve, lambda h: h.reduce_max(out=mx[:, 0:1], in_=bank(B0)[:, 0:NE], axis=AX.X),
                             rd=[bankR[B0]], wr=[mx])
                        S.op(dve, lambda h: h.tensor_scalar_mul(out=mx[:, 1:2], in0=mx[:, 0:1], scalar1=-1.0), rd=[mx], wr=[mx])
                        S.op(act, lambda h: h.activation(out=lg[:], in_=bank(B0)[:, 0:NE], func=AF.Exp, bias=mx[:, 1:2],
                                                         accum_out=mx[:, 2:3]), rd=[bankR[B0], mx], wr=[lg, mx])
                        S.op(dve, lambda h: h.reciprocal(out=mx[:, 3:4], in_=mx[:, 2:3]), rd=[mx], wr=[mx])
                        S.op(dve, lambda h: h.tensor_scalar_mul(out=aff[:, tl, :], in0=lg[:], scalar1=mx[:, 3:4]),
                             rd=[lg, mx], wr=[aff])
                    tile_steps["sm"] = st_s
                    per_tile.append(tile_steps)
                order = [("w0", 0), ("c1", 0), ("w0", 1), ("c1", 1), ("c2", 0), ("r0", 0), ("c2", 1), ("r1", 0),
                         ("w0", 2), ("c1", 2), ("sm", 0), ("r0", 1), ("r1", 1), ("c2", 2), ("w0", 3), ("c1", 3), ("sm", 1),
                         ("r0", 2), ("r1", 2), ("c2", 3), ("sm", 2), ("r0", 3), ("r1", 3), ("sm", 3)]
                for (k_, t_) in order:
                    steps.append(per_tile[t_][k_])
                return steps

            pending = []
            deferred = []
            gcnt = {"g": 0}
            for qb in range(8):
                qt = qz[qb % 2]
                for c in range(2):
                    S.dma(sp, lambda h: h.dma_start(
                        out=qt[c][c * 64:(c + 1) * 64, :, :],
                        in_=qT_d.rearrange("c p t -> p c t")[c * 64:(c + 1) * 64, :, qb * 512:(qb + 1) * 512]),
                        rd=[R_qT], wr=[qt[c]])
                lb = lruB[qb % 2]
                S.dma(sp, lambda h: h.dma_start(out=lb[:], in_=lru_d.rearrange("c p t -> p c t")[:, :, qb * 512:(qb + 1) * 512]),
                      rd=[R_lru], wr=[lb])
                NP = NT // 2
                items = [(hd, c, kp) for hd in range(4) for c in range(2) for kp in range(NP)]

                def emit_S(i):
                    hd, c, kp = items[i]
                    pb = (i % 2) * 2
                    for u in range(2):
                        kt = kp * 2 + u
                        S.op(pe, lambda h: h.matmul(bank(pb + u), lhsT=kT[:, hd, kt * 128:(kt + 1) * 128],
                                                    rhs=qt[c][:, hd, :], start=True, stop=True),
                             rd=[kT, qt[c]], wr=[bankR[pb + u]], self_sync=False)

                emit_S(0)
                emit_S(1)
                for i, (hd, c, kp) in enumerate(items):
                    os_ = osb[hd % 2]
                    pb = (i % 2) * 2
                    ab = 4 + 2 * ((hd * 2 + c) % 2)
                    E = Eb[i % 3]
                    S.op(act, lambda h: h.activation(out=E[:], in_=PS[:, pb:pb + 2, :].rearrange("p a n -> p (a n)"),
                                                     func=AF.Exp, bias=sc2[:, 1:2]),
                         rd=[bankR[pb], bankR[pb + 1], sc2], wr=[E])
                    if i + 2 < len(items):
                        emit_S(i + 2)
                    for u in range(2):
                        kt = kp * 2 + u
                        for s_ in range(4):
                            bb = ab + s_ // 2
                            co = (s_ % 2) * 256
                            S.op(pe, lambda h: h.matmul(bank(bb)[:, co:co + 129],
                                                        lhsT=E[:, u * 512 + s_ * 128:u * 512 + (s_ + 1) * 128],
                                                        rhs=v1[:, kt, hd * 130:hd * 130 + 129],
                                                        start=(kt == 0 and s_ % 2 == 0), stop=(kt == NT - 1),
                                                        skip_group_check=True),
                                 rd=[E, v1], wr=[bankR[bb]], self_sync=False)
                    gcnt["g"] += 1
                    while deferred and deferred[0][0] <= gcnt["g"]:
                        deferred.pop(0)[1]()
                    if not deferred and i < NP - 1:
                        for _ in range(2):
                            if pending:
                                pending.pop(0)()
                    elif i == NP - 1:
                        while deferred:
                            deferred.pop(0)[1]()
                        while pending:
                            pending.pop(0)()
                    if kp < NP - 1:
                        continue
                    for s_ in range(4):
                        bb = ab + s_ // 2
                        co = (s_ % 2) * 256
                        S.op(dve, lambda h: h.reciprocal(out=rl[:, s_:s_ + 1], in_=bank(bb)[:, co + 128:co + 129]),
                             rd=[bankR[bb]], wr=[rl])
                        if c == 0:
                            S.op(dve, lambda h: h.tensor_scalar_mul(out=os_[:, s_, :], in0=bank(bb)[:, co:co + 128],
                                                                    scalar1=rl[:, s_:s_ + 1]),
                                 rd=[bankR[bb], rl], wr=[os_])
                        else:
                            S.op(dve, lambda h: h.tensor_tensor(out=rl[:, 4 + s_:5 + s_], in0=rl[:, s_:s_ + 1],
                                                                in1=sc2[:, 0:1], op=ALU.mult), rd=[rl, sc2], wr=[rl])
                            S.op(dve, lambda h: h.scalar_tensor_tensor(out=os_[:, s_, :], in0=bank(bb)[:, co:co + 128],
                                                                       scalar=rl[:, 4 + s_:5 + s_], in1=os_[:, s_, :],
                                                                       op0=ALU.mult, op1=ALU.add),
                                 rd=[bankR[bb], rl, os_], wr=[os_])
                    if c == 0:
                        continue
                    def sub_sq(s_, os_=os_):
                        S.op(act, lambda h: h.activation(out=junk3[:, 0:128], in_=os_[:, s_, :], func=AF.Square,
                                                         accum_out=ssn[:, s_:s_ + 1]), rd=[os_], wr=[junk3, ssn])

                    def sub_fin(hd=hd, os_=os_):
                        S.op(act, lambda h: h.activation(out=rsn[:, 0:4], in_=ssn[:, 0:4], func=AF.Ln, scale=1.0 / 128,
                                                         bias=epsT[:, 0:1]), rd=[ssn, epsT], wr=[rsn])
                        S.op(act, lambda h: h.activation(out=rsn[:, 0:4], in_=rsn[:, 0:4], func=AF.Exp, scale=-0.5),
                             rd=[rsn], wr=[rsn])
                        for s_ in range(4):
                            S.op(dve, lambda h: h.scalar_tensor_tensor(out=att[:, s_, hd * 128:(hd + 1) * 128], in0=os_[:, s_, :],
                                                                       scalar=rsn[:, s_:s_ + 1], in1=bcs[:, hd * 128:(hd + 1) * 128],
                                                                       op0=ALU.mult, op1=ALU.mult),
                                 rd=[os_, rsn, bcs], wr=[att])
                    for s_ in range(4):
                        deferred.append((gcnt["g"] + 2 + s_ // 2, (lambda sub_sq=sub_sq, s_=s_: sub_sq(s_))))
                    deferred.append((gcnt["g"] + 4, sub_fin))
                pending = tail_steps(qb, lb, 6, 7)
            while deferred:
                deferred.pop(0)[1]()
            for st in pending:
                st()
            if dbg:
                S.dma(sp, lambda h: h.dma_start(out=dbg_aff, in_=aff[:].rearrange("p t e -> p (t e)")), rd=[aff])
        p03.close()
        S.barrier()
        if stop_after < 4:
            S.finish()
            return nc

        p45 = top.enter_context(ExitStack())
        NGU = 4
        NB = 11
        wgb = [sb(p45, f"wgb{i}", [128, 8, 256], BF16) for i in range(NGU)]
        wub = [sb(p45, f"wub{i}", [128, 8, 256], BF16) for i in range(NGU)]
        wdb = [sb(p45, f"wdb{i}", [128, NJ, 512], BF16) for i in range(2)]

        def load_gu(e, b):
            i = (e * NB + b) % NGU
            for (dst, srcw) in ((wgb[i], wg_d), (wub[i], wu_d)):
                S.dma(pool, lambda h: h.dma_start(
                    out=dst[:], in_=srcw[e].rearrange("(kc p) n -> p kc n", p=128)[:, :, b * 256:(b + 1) * 256]),
                    wr=[dst])

        def load_d(e, fh):
            dst = wdb[fh]
            S.dma(pool, lambda h: h.dma_start(
                out=dst[:], in_=wd_d[e].rearrange("(j p) f -> p j f", p=128)[:, :, fh * 512:(fh + 1) * 512]),
                wr=[dst])

        if stop_after >= 5:
            for b_ in range(NGU):
                load_gu(0, b_)
            load_d(0, 0)
            load_d(0, 1)

        with ExitStack() as p4:
            lo = sb(p4, "lo", [128, NE])
            hi = sb(p4, "hi", [128, NE])
            mid = sb(p4, "mid", [128, NE])
            ta = sb(p4, "ta", [128, NE])
            cmpb = sb(p4, "cmpb", [128, 32, NE], BF16)
            maskb = sb(p4, "maskb", [128, 32, NE], BF16)
            cum = sb(p4, "cum", [128, 32, NE], BF16)
            posm = sb(p4, "posm", [128, 32, NE])
            TI = sb(p4, "TI", [128, NE, 32, 8], BF16)
            rres = sb(p4, "rres", [128, 32, NE])
            Sb = [sb(p4, f"Sb{i}", [128, 512], BF16) for i in range(4)]
            idxf = sb(p4, "idxf", [128, 4])
            pvs = sb(p4, "pvs", [128, 32])
            S.op(dve, lambda h: h.memset(lo[:], 0.0), wr=[lo])
            S.op(dve, lambda h: h.memset(hi[:], 1.0), wr=[hi])
            S.op(dve, lambda h: h.memset(mid[:], 0.5), wr=[mid])
            for it in range(32):
                S.op(dve, lambda h: h.tensor_tensor(out=cmpb[:], in0=aff[:],
                                                    in1=mid[:].unsqueeze(1).broadcast_to([128, 32, NE]), op=ALU.is_ge),
                     rd=[aff, mid], wr=[cmpb])
                for t in range(32):
                    S.op(pe, lambda h: h.matmul(bank(0)[:, 0:NE], lhsT=ones_b, rhs=cmpb[:, t, :],
                                                start=(t == 0), stop=(t == 31)),
                         rd=[cmpb, cstb], wr=[bankR[0]], self_sync=False)
                S.op(dve, lambda h: h.scalar_tensor_tensor(out=ta[:], in0=bank(0)[:, 0:NE], scalar=float(CAP) - 0.5,
                                                           in1=mid[:], op0=ALU.is_ge, op1=ALU.mult),
                     rd=[bankR[0], mid], wr=[ta])
                S.op(dve, lambda h: h.tensor_tensor(out=lo[:], in0=lo[:], in1=ta[:], op=ALU.max), rd=[lo, ta], wr=[lo])
                S.op(dve, lambda h: h.scalar_tensor_tensor(out=ta[:], in0=bank(0)[:, 0:NE], scalar=float(CAP) - 0.5,
                                                           in1=mid[:], op0=ALU.is_ge, op1=ALU.add),
                     rd=[bankR[0], mid], wr=[ta])
                S.op(dve, lambda h: h.tensor_tensor(out=hi[:], in0=hi[:], in1=ta[:], op=ALU.min), rd=[hi, ta], wr=[hi])
                S.op(dve, lambda h: h.tensor_tensor(out=mid[:], in0=lo[:], in1=hi[:], op=ALU.add), rd=[lo, hi], wr=[mid])
                S.op(dve, lambda h: h.tensor_scalar_mul(out=mid[:], in0=mid[:], scalar1=0.5), rd=[mid], wr=[mid])
            S.op(dve, lambda h: h.tensor_tensor(out=maskb[:], in0=aff[:],
                                                in1=lo[:].unsqueeze(1).broadcast_to([128, 32, NE]), op=ALU.is_ge),
                 rd=[aff, lo], wr=[maskb])
            S.op(dve, lambda h: h.memset(cum[:, 0, :], 0.0), wr=[cum])
            for t in range(1, 32):
                S.op(dve, lambda h: h.tensor_tensor(out=cum[:, t, :], in0=cum[:, t - 1, :], in1=maskb[:, t - 1, :],
                                                    op=ALU.add), rd=[cum, maskb], wr=[cum])
            S.op(pe, lambda h: h.matmul(bank(1), lhsT=ustr_b, rhs=maskb[:].rearrange("p t e -> p (t e)"),
                                        start=True, stop=False), rd=[maskb, cstb], wr=[bankR[1]], self_sync=False)
            S.op(pe, lambda h: h.matmul(bank(1), lhsT=ones_b, rhs=cum[:].rearrange("p t e -> p (t e)"),
                                        start=False, stop=True), rd=[cum, cstb], wr=[bankR[1]], self_sync=False)
            S.op(dve, lambda h: h.scalar_tensor_tensor(out=posm[:].rearrange("p t e -> p (t e)"), in0=bank(1), scalar=1.0,
                                                       in1=maskb[:].rearrange("p t e -> p (t e)"),
                                                       op0=ALU.add, op1=ALU.mult), rd=[bankR[1], maskb], wr=[posm])
            S.op(dve, lambda h: h.tensor_scalar_add(out=posm[:], in0=posm[:], scalar1=-1.0), rd=[posm], wr=[posm])
            S.op(pool, lambda h: h.memset(TI[:], 0.0), wr=[TI])
            S.op(dve, lambda h: h.tensor_copy(out=TI[:, :, :, 0],
                                              in_=cst[:, 897:929].unsqueeze(1).broadcast_to([128, NE, 32])),
                 rd=[cst, TI], wr=[TI])
            S.op(dve, lambda h: h.tensor_copy(out=TI[:, :, :, 1],
                                              in_=cst[:, 896:897].unsqueeze(1).broadcast_to([128, NE, 32])),
                 rd=[cst, TI], wr=[TI])
            affv = aff[:].rearrange("p t e -> p e t")
            rresv = rres[:].rearrange("p t e -> p e t")
            S.op(dve, lambda h: h.tensor_copy(out=TI[:, :, :, 2], in_=affv), rd=[aff, TI], wr=[TI])
            S.op(dve, lambda h: h.tensor_tensor(out=rresv, in0=affv, in1=TI[:, :, :, 2], op=ALU.subtract),
                 rd=[aff, TI], wr=[rres])
            S.op(dve, lambda h: h.tensor_copy(out=TI[:, :, :, 3], in_=rresv), rd=[rres, TI], wr=[TI])
            S.op(dve, lambda h: h.tensor_tensor(out=rresv, in0=rresv, in1=TI[:, :, :, 3], op=ALU.subtract),
                 rd=[rres, TI], wr=[rres])
            S.op(dve, lambda h: h.tensor_copy(out=TI[:, :, :, 4], in_=rresv), rd=[rres, TI], wr=[TI])
            scn = 0
            for e in range(NE):
                b = 2 + e % 2
                for t in range(32):
                    Sx = Sb[scn % 4]
                    scn += 1
                    S.op(dve, lambda h: h.tensor_scalar(out=Sx[:], in0=iota_f, scalar1=posm[:, t, e:e + 1], scalar2=None,
                                                        op0=ALU.is_equal), rd=[cst, posm], wr=[Sx])
                    for sc in range(4):
                        S.op(pe, lambda h: h.matmul(bank(b)[:, sc * 8:sc * 8 + 8], lhsT=Sx[:, sc * 128:(sc + 1) * 128],
                                                    rhs=TI[:, e, t, :], start=(t == 0 and sc == 0), stop=(t == 31),
                                                    skip_group_check=True),
                             rd=[Sx, TI], wr=[bankR[b]], self_sync=False)
                S.op(dve, lambda h: h.tensor_copy(out=pvs[:], in_=bank(b)[:, 0:32]), rd=[bankR[b]], wr=[pvs])
                pv = pvs[:].rearrange("p (s c) -> p s c", c=8)
                S.op(dve, lambda h: h.scalar_tensor_tensor(out=idxf[:], in0=pv[:, :, 0], scalar=128.0, in1=pv[:, :, 1],
                                                           op0=ALU.mult, op1=ALU.add), rd=[pvs], wr=[idxf])
                S.op(dve, lambda h: h.tensor_copy(out=idx_i[:, e, :], in_=idxf[:]), rd=[idxf], wr=[idx_i])
                S.op(dve, lambda h: h.tensor_tensor(out=gsl[:, e, :], in0=pv[:, :, 2], in1=pv[:, :, 3], op=ALU.add),
                     rd=[pvs], wr=[gsl])
                S.op(dve, lambda h: h.tensor_tensor(out=gsl[:, e, :], in0=gsl[:, e, :], in1=pv[:, :, 4], op=ALU.add),
                     rd=[pvs, gsl], wr=[gsl])
            if dbg:
                S.dma(sp, lambda h: h.dma_start(out=dbg_idx, in_=idx_i[:].rearrange("p e s -> p (e s)")), rd=[idx_i])
                S.dma(sp, lambda h: h.dma_start(out=dbg_g, in_=gsl[:].rearrange("p e s -> p (e s)")), rd=[gsl])
        S.barrier()
        if stop_after < 5:
            S.finish()
            return nc

        with ExitStack() as p5:
            xe = [sb(p5, f"xe{i}", [128, 4, D], BF16) for i in range(2)]
            xeT = [sb(p5, f"xeT{i}", [128, 8, 512], BF16) for i in range(2)]
            hT = [sb(p5, f"hTe{i}", [128, NJ, 512], BF16) for i in range(2)]
            old = [sb(p5, f"old{i}", [128, D]) for i in range(4)]
            sg = [sb(p5, f"sg{i}", [128, 512]) for i in range(2)]
            tmp5 = [sb(p5, f"tmp5{i}", [128, 512]) for i in range(2)]
            xe_r = [[Reg() for _ in range(4)] for _ in range(2)]
            R_osc = [Reg() for _ in range(4)]

            def gather_xe(e, scs=range(4)):
                xg = xe[e % 2]
                for sc in scs:
                    S.dma(pool, lambda h: h.indirect_dma_start(
                        out=xg[:, sc, :], out_offset=None, in_=h2_d[:, :],
                        in_offset=bass.IndirectOffsetOnAxis(ap=idx_i[:, e, sc:sc + 1], axis=0)),
                        rd=[R_h2, idx_i], wr=[xg])

            def gather_old(e, scs=range(4)):
                for sc in scs:
                    S.dma(pool, lambda h: h.indirect_dma_start(
                        out=old[sc][:], out_offset=None, in_=out_d[:, :],
                        in_offset=bass.IndirectOffsetOnAxis(ap=idx_i[:, e, sc:sc + 1], axis=0)),
                        rd=[R_out, idx_i], wr=[old[sc]])

            def make_xeT(e):
                xg, xt_ = xe[e % 2], xeT[e % 2]
                for kc in range(8):
                    for sc in range(4):
                        S.op(pe, lambda h: h.transpose(out=bank_bf(0)[:, sc * 128:(sc + 1) * 128],
                                                       in_=xg[:, sc, kc * 128:(kc + 1) * 128], identity=ident_b),
                             rd=[xg, cstb], wr=[bankR[0]], self_sync=False)
                    S.op(act, lambda h: h.copy(out=xt_[:, kc, :], in_=bank_bf(0)[:, 0:512]), rd=[bankR[0]], wr=[xt_])

            def scatter_new(e, scs=range(4)):
                for sc in scs:
                    S.dma(pool, lambda h: h.indirect_dma_start(
                        out=out_d[:, :], out_offset=bass.IndirectOffsetOnAxis(ap=idx_i[:, e, sc:sc + 1], axis=0),
                        in_=old[sc][:], in_offset=None), rd=[old[sc], idx_i], wr=[R_out])

            gather_xe(0)
            gather_old(0)
            make_xeT(0)
            pcnt = 0
            dcnt = 0
            for e in range(NE):
                xt_ = xeT[e % 2]
                hT_ = hT[e % 2]
                for b in range(NB):
                    i = (e * NB + b) % NGU
                    if b < 4 and e + 1 < NE:
                        gather_xe(e + 1, [b])
                    if 4 <= b < 8 and e > 0:
                        scatter_new(e - 1, [b - 4])
                    if b >= 8 and e > 0:
                        gather_old(e, [b - 8])
                    for jj in range(2):
                        j = b * 2 + jj
                        bg = 1 + (pcnt % 2) * 2
                        pcnt += 1
                        for (bk, wt) in ((bg, wgb[i]), (bg + 1, wub[i])):
                            for kc in range(8):
                                S.op(pe, lambda h: h.matmul(bank(bk), lhsT=wt[:, kc, jj * 128:(jj + 1) * 128], rhs=xt_[:, kc, :],
                                                            start=(kc == 0), stop=(kc == 7)),
                                     rd=[wt, xt_], wr=[bankR[bk]], self_sync=False)
                        sg_ = sg[j % 2]
                        S.op(act, lambda h: h.activation(out=sg_[:], in_=bank(bg), func=AF.Silu), rd=[bankR[bg]], wr=[sg_])
                        S.op(dve, lambda h: h.tensor_tensor(out=hT_[:, j, :], in0=sg_[:], in1=bank(bg + 1), op=ALU.mult),
                             rd=[sg_, bankR[bg + 1]], wr=[hT_])
                    nb_ = b + NGU
                    if nb_ < NB:
                        load_gu(e, nb_)
                    elif e + 1 < NE:
                        load_gu(e + 1, nb_ - NB)
                if e > 0:
                    gather_old(e, [3])
                if e + 1 < NE:
                    make_xeT(e + 1)
                for fh in range(2):
                    wd_ = wdb[fh]
                    for sc in range(4):
                        bk = 5 + dcnt % 3
                        dcnt += 1
                        for j in range(NJ):
                            S.op(pe, lambda h: h.matmul(bank(bk), lhsT=hT_[:, j, sc * 128:(sc + 1) * 128], rhs=wd_[:, j, :],
                                                        start=(j == 0), stop=(j == NJ - 1)),
                                 rd=[hT_, wd_], wr=[bankR[bk]], self_sync=False)
                        t5 = tmp5[sc % 2]
                        S.op(dve, lambda h: h.scalar_tensor_tensor(out=t5[:], in0=bank(bk), scalar=gsl[:, e, sc:sc + 1],
                                                                   in1=bcG[:, 3, fh * 512:(fh + 1) * 512],
                                                                   op0=ALU.mult, op1=ALU.mult),
                             rd=[bankR[bk], gsl, bcG], wr=[t5])
                        S.op(dve, lambda h: h.tensor_tensor(out=old[sc][:, fh * 512:(fh + 1) * 512],
                                                             in0=old[sc][:, fh * 512:(fh + 1) * 512], in1=t5[:], op=ALU.add),
                             rd=[old[sc], t5], wr=[old[sc]])
                    if e + 1 < NE:
                        load_d(e + 1, fh)
            scatter_new(NE - 1)
        S.finish()
    return nc


def _consts():
    c = np.zeros((128, NCONST), np.float32)
    c[:, 0:128] = np.eye(128, dtype=np.float32)
    p = np.arange(128)
    c[:, 128:256] = (p[:, None] < p[None, :]).astype(np.float32)
    c[:, 256:384] = 1.0
    c[:, 384:896] = np.arange(512, dtype=np.float32)[None, :]
    c[:, 896] = p.astype(np.float32)
    c[:, 897:929] = np.arange(32, dtype=np.float32)[None, :]
    return c


def _rope_table16():
    tab = np.zeros((128, NT, 2, 64), np.float32)
    tab[:, :, 0, :] = 1.0
    inv = (np.float32(10000.0) ** (-np.arange(16, dtype=np.float32) / np.float32(16))).astype(np.float32)
    for T in range(2, NT):
        tok = (T - 2) * 128 + np.arange(128)
        row = (tok // 64).astype(np.float32)
        col = (tok % 64).astype(np.float32)
        ar = (row[:, None] * inv[None, :]).astype(np.float32)
        ac = (col[:, None] * inv[None, :]).astype(np.float32)
        cr, sr, cc, sc_ = np.cos(ar), np.sin(ar), np.cos(ac), np.sin(ac)
        tab[:, T, 0, 0:16] = cr
        tab[:, T, 0, 16:32] = cr
        tab[:, T, 0, 32:48] = cc
        tab[:, T, 0, 48:64] = cc
        tab[:, T, 1, 0:16] = -sr
        tab[:, T, 1, 16:32] = sr
        tab[:, T, 1, 32:48] = -sc_
        tab[:, T, 1, 48:64] = sc_
    return tab.reshape(128, NT * 128)


def make_in_maps(inp, cores):
    f = lambda a: np.ascontiguousarray(np.asarray(a, dtype=np.float32))
    L = 0
    c_ctx = f(inp["c_ctx"])
    conv_w = f(inp["conv_w"][L])
    convw = np.zeros((128, 16), np.float32)
    for j in range(4):
        for k in range(4):
            convw[:, j * 4 + k] = conv_w[k, j * 128:(j + 1) * 128]
    convb = f(inp["conv_b"][L]).reshape(4, 128).T
    lruw = np.zeros((128, 2, 2, 4, 128), np.float32)
    lrub = np.zeros((128, 2, 2, 4), np.float32)
    for d in range(2):
        for gi, (wn, bn) in enumerate((("lru_wa", "lru_ba"), ("lru_wi", "lru_bi"))):
            w = f(inp[wn][L][d])
            bb = f(inp[bn][L][d])
            for j in range(4):
                for hh in range(2):
                    lruw[hh * 64:(hh + 1) * 64, d, gi, j, hh * 64:(hh + 1) * 64] = w[2 * j + hh]
                    lrub[hh * 64:(hh + 1) * 64, d, gi, j] = bb[2 * j + hh]
    lrul = np.zeros((128, 2, 4), np.float32)
    lam = f(inp["lru_lambda"][L])
    for d in range(2):
        lrul[:, d, :] = lam[d].reshape(4, 128).T
    shared = {
        "w_ada": f(inp["w_ada"][L]),
        "b_ada": f(inp["b_ada"][L]).reshape(1, -1),
        "normg": np.concatenate([f(inp["norm1_g"][L]), f(inp["norm2_g"][L])]).reshape(1, -1),
        "qkg": np.concatenate([np.tile(f(inp["q_norm_g"][L]), 8), np.tile(f(inp["k_norm_g"][L]), 8)]).reshape(1, -1),
        "lamv": np.concatenate([f(inp["lambda_q1"][L]), f(inp["lambda_k1"][L]),
                                f(inp["lambda_q2"][L]), f(inp["lambda_k2"][L])]).reshape(1, -1),
        "subg": np.tile(f(inp["subln_g"][L]), 4).reshape(1, -1),
        "w_in": f(inp["w_in"][L]),
        "convw": convw,
        "convb": np.ascontiguousarray(convb),
        "lruw": np.ascontiguousarray(lruw.reshape(128, 2048)),
        "lrub": np.ascontiguousarray(lrub.reshape(128, 16)),
        "lrul": np.ascontiguousarray(lrul.reshape(128, 8)),
        "w_out": f(inp["w_out"][L]),
        "w_router": f(inp["w_router"][L]),
        "w_gate": f(inp["w_gate"][L]),
        "w_up": f(inp["w_up"][L]),
        "w_down": f(inp["w_down"][L]),
        "rope": _rope_table16(),
        "consts": _consts(),
    }
    maps = []
    for b in cores:
        cvec = np.zeros((128, 16), np.float32)
        cvec[:, 0:8] = f(inp["c"][b]).reshape(8, 128).T
        cvec[:, 8:16] = c_ctx.reshape(8, 128).T
        m = dict(shared)
        m["x"] = f(inp["x"][b])
        m["ctx"] = f(inp["ctx"][b])
        m["cvec"] = cvec
        maps.append(m)
    return maps


def kernel(**inputs):
    nc = build()
    maps = make_in_maps(inputs, list(range(8)))
    res = run_bass_kernel_spmd(nc, maps, core_ids=list(range(8)))
    return np.stack([np.asarray(r["out"], dtype=np.float32) for r in res.results], axis=0)
```
